# Optimizing a Trainium2 kernel written in Bass

```python
import jax
import jax.numpy as jnp
from jax import lax
import numpy as np

D_MODEL = 1024
BATCH = 4
SEQ = 4096
DEPTH = 4

HEAD_DIM = 64
N_HEADS = D_MODEL // HEAD_DIM
D_MIX = N_HEADS * HEAD_DIM
GLA_HEADS = N_HEADS // 4
NSA_HEADS = N_HEADS // 2
NSA_KV_HEADS = NSA_HEADS // 4
RET_HEADS = N_HEADS - GLA_HEADS - NSA_HEADS
GLA_W = GLA_HEADS * HEAD_DIM
NSA_W = NSA_HEADS * HEAD_DIM
NSA_KV_W = NSA_KV_HEADS * HEAD_DIM
RET_W = RET_HEADS * HEAD_DIM
GLA_LOWRANK = 16
GLA_TAU = 16.0
CHUNK = 64
CMP_LEN = 32
CMP_STRIDE = 16
CMP_HIDDEN = 128
SLC_LEN = 64
SLC_TOPK = 16
WINDOW = 512
Q_BLOCK = 128
ROPE_THETA = 10000.0
D_FF = 2816
FFN_HALF = 0.5
NORM_EPS = 1e-6
NEG_INF = -1e30
FORCED_SCORE = 1e4
N_ADA = 9

IN_SIZES = (GLA_W, GLA_W, GLA_W, GLA_W, GLA_LOWRANK,
            NSA_W, NSA_KV_W, NSA_KV_W, NSA_KV_W, NSA_KV_W, NSA_KV_W, NSA_KV_W, 3 * NSA_HEADS,
            RET_W, RET_W, RET_W, RET_W)
IN_W = sum(IN_SIZES)
IN_SPLITS = tuple(int(s) for s in np.cumsum(IN_SIZES)[:-1])

kernel_name = 'hybrid_gla_nsa_retnet_macaron_adaln'


def rms_norm(x, g):
    xf = x.astype(jnp.float32)
    y = xf * lax.rsqrt(jnp.mean(xf * xf, axis=-1, keepdims=True) + NORM_EPS) * g
    return y.astype(x.dtype)


def group_norm(x, g):
    xf = x.astype(jnp.float32)
    xc = xf - jnp.mean(xf, axis=-1, keepdims=True)
    return xc * lax.rsqrt(jnp.mean(xc * xc, axis=-1, keepdims=True) + NORM_EPS) * g


def rotary(x, pos):
    half = x.shape[-1] // 2
    inv_freq = ROPE_THETA ** (-jnp.arange(half, dtype=jnp.float32) / half)
    ang = pos.astype(jnp.float32)[:, None] * inv_freq[None, :]
    cos, sin = jnp.cos(ang), jnp.sin(ang)
    xf = x.astype(jnp.float32)
    x1, x2 = xf[..., :half], xf[..., half:]
    return jnp.concatenate([x1 * cos - x2 * sin, x1 * sin + x2 * cos], axis=-1)


def modulate(h, shift, scale):
    return h * (1.0 + scale[:, None, :]) + shift[:, None, :]


def swiglu(h, w_in, w_out):
    gate, up = jnp.split(h @ w_in, 2, axis=-1)
    return (jax.nn.silu(gate) * up) @ w_out


def gla_chunked(q, k, v, log_a):
    B, H, T, dk = q.shape
    dv = v.shape[-1]
    n = T // CHUNK
    f32 = jnp.float32
    q = (q.astype(f32) * dk ** -0.5).reshape(B, H, n, CHUNK, dk)
    k = k.astype(f32).reshape(B, H, n, CHUNK, dk)
    v = v.astype(f32).reshape(B, H, n, CHUNK, dv)
    b = jnp.cumsum(log_a.astype(f32).reshape(B, H, n, CHUNK, dk), axis=3)
    b_last = b[:, :, :, -1:, :]
    q_dec = q * jnp.exp(b)
    causal = jnp.tril(jnp.ones((CHUNK, CHUNK), dtype=bool))
    attn = jnp.einsum('bhnid,bhnjd->bhnij', q_dec, k * jnp.exp(-b))
    attn = jnp.where(causal, attn, 0.0)
    o_intra = jnp.einsum('bhnij,bhnjv->bhniv', attn, v)
    kv = jnp.einsum('bhnjd,bhnjv->bhndv', k * jnp.exp(b_last - b), v)
    chunk_decay = jnp.exp(b_last[:, :, :, 0, :])

    def step(state, inp):
        kv_n, dec_n = inp
        return state * dec_n[..., None] + kv_n, state

    _, s_prev = lax.scan(step, jnp.zeros((B, H, dk, dv), f32),
                         (jnp.moveaxis(kv, 2, 0), jnp.moveaxis(chunk_decay, 2, 0)))
    o_inter = jnp.einsum('bhnid,bhndv->bhniv', q_dec, jnp.moveaxis(s_prev, 0, 2))
    return (o_intra + o_inter).reshape(B, H, T, dv)


def retention_chunked(q, k, v, log_gamma):
    B, H, T, dk = q.shape
    dv = v.shape[-1]
    n = T // CHUNK
    f32 = jnp.float32
    q = q.astype(f32).reshape(B, H, n, CHUNK, dk)
    k = (k.astype(f32) * dk ** -0.5).reshape(B, H, n, CHUNK, dk)
    v = v.astype(f32).reshape(B, H, n, CHUNK, dv)
    idx = jnp.arange(CHUNK, dtype=f32)
    rel = idx[:, None] - idx[None, :]
    decay = jnp.where(rel >= 0, jnp.exp(log_gamma[:, None, None] * rel), 0.0)
    attn = jnp.einsum('bhnid,bhnjd->bhnij', q, k) * decay[None, :, None]
    o_intra = jnp.einsum('bhnij,bhnjv->bhniv', attn, v)
    q_dec = q * jnp.exp(log_gamma[:, None] * (idx + 1.0))[None, :, None, :, None]
    k_dec = k * jnp.exp(log_gamma[:, None] * (CHUNK - 1.0 - idx))[None, :, None, :, None]
    kv = jnp.einsum('bhnjd,bhnjv->bhndv', k_dec, v)
    chunk_decay = jnp.exp(log_gamma * CHUNK)[None, :, None, None]

    def step(state, kv_n):
        return state * chunk_decay + kv_n, state

    _, s_prev = lax.scan(step, jnp.zeros((B, H, dk, dv), f32), jnp.moveaxis(kv, 2, 0))
    o_inter = jnp.einsum('bhnid,bhndv->bhniv', q_dec, jnp.moveaxis(s_prev, 0, 2))
    return (o_intra + o_inter).reshape(B, H, T, dv)


def compress_blocks(blocks, pe, w1, w2):
    h = (blocks + pe).reshape(blocks.shape[:-2] + (CMP_LEN * blocks.shape[-1],))
    return jax.nn.silu(h @ w1) @ w2


def nsa_attention(q, k_cmp, v_cmp, k_slc, v_slc, k_win, v_win, gate_logits,
                  pe_k, pe_v, w1_k, w2_k, w1_v, w2_v, pos):
    f32 = jnp.float32
    B, H, T, dh = q.shape
    G = k_cmp.shape[1]
    R = H // G
    scale = dh ** -0.5
    t = jnp.arange(T)
    q_raw = q.astype(f32).reshape(B, G, R, T, dh) * scale
    q_rot = rotary(q, pos).reshape(B, G, R, T, dh) * scale
    k_cmp, v_cmp, v_slc, v_win = (a.astype(f32) for a in (k_cmp, v_cmp, v_slc, v_win))
    k_slc_r = rotary(k_slc, pos)
    k_win_r = rotary(k_win, pos)

    n_cmp = (T - CMP_LEN) // CMP_STRIDE + 1
    c_start = np.arange(n_cmp) * CMP_STRIDE
    c_idx = c_start[:, None] + np.arange(CMP_LEN)[None, :]
    k_c = compress_blocks(k_cmp[:, :, c_idx], pe_k, w1_k, w2_k)
    v_c = compress_blocks(v_cmp[:, :, c_idx], pe_v, w1_v, w2_v)
    s_c = jnp.einsum('bgrtd,bgnd->bgrtn', q_raw, k_c)
    valid_c = (c_start + CMP_LEN - 1)[None, :] <= t[:, None]
    p_c = jax.nn.softmax(jnp.where(valid_c, s_c, NEG_INF), axis=-1) * valid_c
    o_cmp = jnp.einsum('bgrtn,bgnd->bgrtd', p_c, v_c)

    n_slc = T // SLC_LEN
    s_start = np.arange(n_slc) * SLC_LEN
    overlap = ((c_start[:, None] < s_start[None, :] + SLC_LEN)
               & (c_start[:, None] + CMP_LEN > s_start[None, :])).astype(np.float32)
    imp = jnp.einsum('bgrtn,ns->bgts', p_c, jnp.asarray(overlap))
    cur = t // SLC_LEN
    blk = jnp.arange(n_slc)
    forced = (blk[None, :] == 0) | (blk[None, :] == cur[:, None]) | (blk[None, :] == cur[:, None] - 1)
    causal_s = blk[None, :] <= cur[:, None]
    imp = jnp.where(forced, FORCED_SCORE, jnp.where(causal_s, imp, -1.0))
    n_sel = min(SLC_TOPK, n_slc)
    _, sel = lax.top_k(imp, n_sel)

    k_sb = k_slc_r.reshape(B, G, n_slc, SLC_LEN, dh)
    v_sb = v_slc.reshape(B, G, n_slc, SLC_LEN, dh)
    nqb = T // Q_BLOCK
    b_ix = jnp.arange(B)[:, None, None, None]
    g_ix = jnp.arange(G)[None, :, None, None]

    def select_block(args):
        qb, selb, tb = args
        kg = k_sb[b_ix, g_ix, selb]
        vg = v_sb[b_ix, g_ix, selb]
        s = jnp.einsum('bgrqd,bgqkld->bgrqkl', qb, kg)
        kpos = selb[..., None] * SLC_LEN + jnp.arange(SLC_LEN)
        ok = (kpos <= tb[:, None, None])[:, :, None]
        s = jnp.where(ok, s, NEG_INF).reshape(B, G, R, Q_BLOCK, n_sel * SLC_LEN)
        p = jax.nn.softmax(s, axis=-1).reshape(B, G, R, Q_BLOCK, n_sel, SLC_LEN)
        return jnp.einsum('bgrqkl,bgqkld->bgrqd', p, vg)

    q_blocks = jnp.moveaxis(q_rot.reshape(B, G, R, nqb, Q_BLOCK, dh), 3, 0)
    sel_blocks = jnp.moveaxis(sel.reshape(B, G, nqb, Q_BLOCK, n_sel), 2, 0)
    o_slc = lax.map(select_block, (q_blocks, sel_blocks, t.reshape(nqb, Q_BLOCK)))
    o_slc = jnp.moveaxis(o_slc, 0, 3).reshape(B, G, R, T, dh)

    n_band = WINDOW // Q_BLOCK + 1

    def band(a):
        ap = jnp.pad(a, ((0, 0), (0, 0), (WINDOW, 0), (0, 0))).reshape(B, G, nqb + n_band - 1, Q_BLOCK, dh)
        return jnp.concatenate([ap[:, :, i:i + nqb] for i in range(n_band)], axis=3)

    k_w, v_w = band(k_win_r), band(v_win)
    s_w = jnp.einsum('bgrnid,bgnjd->bgrnij', q_rot.reshape(B, G, R, nqb, Q_BLOCK, dh), k_w)
    qpos = t.reshape(nqb, Q_BLOCK)[:, :, None]
    kpos = ((jnp.arange(nqb)[:, None] - (n_band - 1)) * Q_BLOCK + jnp.arange(n_band * Q_BLOCK)[None, :])[:, None, :]
    ok_w = (kpos <= qpos) & (kpos > qpos - WINDOW) & (kpos >= 0)
    p_w = jax.nn.softmax(jnp.where(ok_w, s_w, NEG_INF), axis=-1)
    o_win = jnp.einsum('bgrnij,bgnjd->bgrnid', p_w, v_w).reshape(B, G, R, T, dh)

    gate = jax.nn.sigmoid(gate_logits.astype(f32)).reshape(B, T, H, 3)
    gate = gate.transpose(0, 2, 1, 3).reshape(B, G, R, T, 3)
    o = gate[..., 0:1] * o_cmp + gate[..., 1:2] * o_slc + gate[..., 2:3] * o_win
    return o.reshape(B, H, T, dh)


def token_mixer(h, w_in, gla_a2, gla_a_bias, gla_norm_g, nsa_pe_k, nsa_pe_v, nsa_w1_k, nsa_w2_k,
                nsa_w1_v, nsa_w2_v, nsa_gate_bias, ret_norm_g, w_out):
    B, T, _ = h.shape
    pos = jnp.arange(T)

    def heads(a, n):
        return a.reshape(B, T, n, HEAD_DIM).transpose(0, 2, 1, 3)

    def merge(a):
        return a.transpose(0, 2, 1, 3).reshape(B, T, -1).astype(h.dtype)

    proj = h @ w_in
    (g_q, g_k, g_v, g_g, g_lr,
     n_q, n_kc, n_vc, n_ks, n_vs, n_kw, n_vw, n_gate,
     r_q, r_k, r_v, r_g) = jnp.split(proj, IN_SPLITS, axis=-1)

    log_a = jax.nn.log_sigmoid((g_lr @ gla_a2 + gla_a_bias).astype(jnp.float32)) / GLA_TAU
    o_gla = gla_chunked(heads(g_q, GLA_HEADS), heads(g_k, GLA_HEADS), heads(g_v, GLA_HEADS),
                        heads(log_a, GLA_HEADS))
    o_gla = merge(rms_norm(o_gla, gla_norm_g)) * jax.nn.silu(g_g)

    o_nsa = nsa_attention(heads(n_q, NSA_HEADS), heads(n_kc, NSA_KV_HEADS), heads(n_vc, NSA_KV_HEADS),
                          heads(n_ks, NSA_KV_HEADS), heads(n_vs, NSA_KV_HEADS),
                          heads(n_kw, NSA_KV_HEADS), heads(n_vw, NSA_KV_HEADS),
                          n_gate + nsa_gate_bias, nsa_pe_k, nsa_pe_v, nsa_w1_k, nsa_w2_k,
                          nsa_w1_v, nsa_w2_v, pos)
    o_nsa = merge(o_nsa)

    log_gamma = jnp.log1p(-jnp.exp2(-5.0 - jnp.arange(RET_HEADS, dtype=jnp.float32)))
    o_ret = retention_chunked(rotary(heads(r_q, RET_HEADS), pos), rotary(heads(r_k, RET_HEADS), pos),
                              heads(r_v, RET_HEADS), log_gamma)
    o_ret = merge(group_norm(o_ret, ret_norm_g)) * jax.nn.silu(r_g)

    return jnp.concatenate([o_gla, o_nsa, o_ret], axis=-1) @ w_out


def setup_inputs(seed: int = 0) -> dict:
    key = jax.random.key(seed)
    ks = jax.random.split(key, 23)
    f32 = jnp.float32

    def nrm(k, shape, scale):
        return jax.random.normal(k, shape, f32) * scale

    L, D = DEPTH, D_MODEL
    cmp_in = CMP_LEN * HEAD_DIM
    return {
        'x': nrm(ks[0], (BATCH, SEQ, D), 1.0),
        'c': nrm(ks[1], (BATCH, D), 1.0),
        'w_ada': nrm(ks[2], (L, D, N_ADA * D), 0.5 * D ** -0.5),
        'b_ada': nrm(ks[3], (L, N_ADA * D), 0.02),
        'norm_g': 1.0 + nrm(ks[4], (L, 3, D), 0.05),
        'ffn1_in': nrm(ks[5], (L, D, 2 * D_FF), D ** -0.5),
        'ffn1_out': nrm(ks[6], (L, D_FF, D), D_FF ** -0.5),
        'w_in': nrm(ks[7], (L, D, IN_W), D ** -0.5),
        'gla_a2': nrm(ks[8], (L, GLA_LOWRANK, GLA_W), GLA_LOWRANK ** -0.5),
        'gla_a_bias': nrm(ks[9], (L, GLA_W), 0.1),
        'gla_norm_g': 1.0 + nrm(ks[10], (L, HEAD_DIM), 0.05),
        'nsa_pe_k': nrm(ks[11], (L, CMP_LEN, HEAD_DIM), 0.1),
        'nsa_pe_v': nrm(ks[12], (L, CMP_LEN, HEAD_DIM), 0.1),
        'nsa_w1_k': nrm(ks[13], (L, cmp_in, CMP_HIDDEN), cmp_in ** -0.5),
        'nsa_w2_k': nrm(ks[14], (L, CMP_HIDDEN, HEAD_DIM), CMP_HIDDEN ** -0.5),
        'nsa_w1_v': nrm(ks[15], (L, cmp_in, CMP_HIDDEN), cmp_in ** -0.5),
        'nsa_w2_v': nrm(ks[16], (L, CMP_HIDDEN, HEAD_DIM), CMP_HIDDEN ** -0.5),
        'nsa_gate_bias': nrm(ks[17], (L, 3 * NSA_HEADS), 0.1),
        'ret_norm_g': 1.0 + nrm(ks[18], (L, HEAD_DIM), 0.05),
        'w_out': nrm(ks[19], (L, D_MIX, D), D_MIX ** -0.5),
        'ffn2_in': nrm(ks[20], (L, D, 2 * D_FF), D ** -0.5),
        'ffn2_out': nrm(ks[21], (L, D_FF, D), D_FF ** -0.5),
        'final_norm_g': 1.0 + nrm(ks[22], (D,), 0.05),
    }


def reference(x, c, w_ada, b_ada, norm_g, ffn1_in, ffn1_out, w_in, gla_a2, gla_a_bias, gla_norm_g,
              nsa_pe_k, nsa_pe_v, nsa_w1_k, nsa_w2_k, nsa_w1_v, nsa_w2_v, nsa_gate_bias, ret_norm_g,
              w_out, ffn2_in, ffn2_out, final_norm_g):
    c_act = jax.nn.silu(c)
    for l in range(DEPTH):
        mod = c_act @ w_ada[l] + b_ada[l]
        sh1, sc1, gt1, shm, scm, gtm, sh2, sc2, gt2 = jnp.split(mod, N_ADA, axis=-1)
        h = modulate(rms_norm(x, norm_g[l, 0]), sh1, sc1)
        x = x + FFN_HALF * gt1[:, None, :] * swiglu(h, ffn1_in[l], ffn1_out[l])
        h = modulate(rms_norm(x, norm_g[l, 1]), shm, scm)
        x = x + gtm[:, None, :] * token_mixer(h, w_in[l], gla_a2[l], gla_a_bias[l], gla_norm_g[l],
                                              nsa_pe_k[l], nsa_pe_v[l], nsa_w1_k[l], nsa_w2_k[l],
                                              nsa_w1_v[l], nsa_w2_v[l], nsa_gate_bias[l],
                                              ret_norm_g[l], w_out[l])
        h = modulate(rms_norm(x, norm_g[l, 2]), sh2, sc2)
        x = x + FFN_HALF * gt2[:, None, :] * swiglu(h, ffn2_in[l], ffn2_out[l])
    return rms_norm(x, final_norm_g)
```

```python
import math
from contextlib import ExitStack

import numpy as np
import concourse.bass as bass
import concourse.mybir as mybir
from concourse.bass_utils import run_bass_kernel_spmd

F32 = mybir.dt.float32
BF16 = mybir.dt.bfloat16
AF = mybir.ActivationFunctionType
ALU = mybir.AluOpType

D = 1024
T = 4096
L = 4
TT = 512
NTILE = T // TT
FF = 2816
NJ = FF // 128
EPS = 1e-6
NEG = -30000.0
NFM = 35
TMA1 = NFM * 128
TMA2 = TMA1 + 256
TMB = TMA2 + 256
NEXT = TMB + 280
FW = 2176


class V:
    def __init__(self, tl, ap):
        self.tl = tl
        self.ap = ap


class Tl:
    def __init__(self, t=None):
        self.t = t
        self.w = None
        self.r = {}

    def __getitem__(self, idx):
        return V(self, self.t[idx])


class Eng:
    def __init__(self, name, h):
        self.name = name
        self.h = h
        self.sem = None
        self.cnt = 0
        self.waited = {}
        self.pending = []


class KB:
    LIMIT = 20000

    def __init__(self, nc, es):
        self.nc = nc
        self.es = es
        self.sems = []
        self.eng = {
            "pe": Eng("pe", nc.tensor),
            "act": Eng("act", nc.scalar),
            "dve": Eng("dve", nc.vector),
            "pool": Eng("pool", nc.gpsimd),
            "sp": Eng("sp", nc.sync),
        }
        self.dq = {}
        for q in ("sp", "pool"):
            self.dq[q] = {"i": 0, "sems": [self.newsem("d%s%d" % (q, i)) for i in range(12)], "cnt": [0] * 12}
        self.nuniq = 0

    def newsem(self, name):
        s = self.es.enter_context(self.nc.semaphore(name))
        self.sems.append(s)
        return len(self.sems) - 1

    def sb(self, name, shape, dt, es=None):
        self.nuniq += 1
        t = (es or self.es).enter_context(self.nc.sbuf_tensor("%s_%d" % (name, self.nuniq), list(shape), dt))
        return Tl(t)

    def ps(self, name, shape, dt=F32):
        t = self.es.enter_context(self.nc.psum_tensor(name, list(shape), dt))
        return Tl(t)

    def _deps(self, reads, writes):
        deps = {}

        def add(ev):
            if ev is not None:
                if deps.get(ev[0], 0) < ev[1]:
                    deps[ev[0]] = ev[1]

        for t in reads:
            add(t.w)
        for t in writes:
            add(t.w)
            for s, v in t.r.items():
                add((s, v))
        return deps

    def _wait(self, E, deps):
        for s, v in deps.items():
            if E.waited.get(s, 0) < v:
                E.h.wait_ge(self.sems[s], v)
                E.waited[s] = v

    def op(self, eng, fn, reads=(), writes=(), last=True):
        E = self.eng[eng]
        reads = [x.tl if isinstance(x, V) else x for x in reads]
        writes = [x.tl if isinstance(x, V) else x for x in writes]
        if E.sem is None or (E.cnt >= self.LIMIT and not E.pending):
            E.sem = self.newsem("e%s%d" % (eng, len(self.sems)))
            E.cnt = 0
        self._wait(E, self._deps(reads, writes))
        ins = fn(E.h)
        E.pending.append((reads, writes))
        if last:
            E.cnt += 1
            ins.then_inc(self.sems[E.sem], 1)
            ev = (E.sem, E.cnt)
            for rd, wr in E.pending:
                for t in rd:
                    if t.r.get(ev[0], 0) < ev[1]:
                        t.r[ev[0]] = ev[1]
                for t in wr:
                    t.w = ev
                    t.r = {}
            E.pending = []
        return ins

    def dma(self, q, out, in_, reads=(), writes=()):
        Q = self.eng[q]
        reads = [x.tl if isinstance(x, V) else x for x in reads]
        writes = [x.tl if isinstance(x, V) else x for x in writes]
        deps = self._deps(reads, writes)
        pool = self.dq[q]
        i = pool["i"] % len(pool["sems"])
        pool["i"] += 1
        sem, cnt = pool["sems"][i], pool["cnt"][i]
        if cnt > 0 and deps.get(sem, 0) < cnt:
            deps[sem] = cnt
        self._wait(Q, deps)
        Q.h.dma_start(out=out, in_=in_).then_inc(self.sems[sem], 16)
        pool["cnt"][i] = cnt + 16
        ev = (sem, cnt + 16)
        for t in reads:
            if t.r.get(ev[0], 0) < ev[1]:
                t.r[ev[0]] = ev[1]
        for t in writes:
            t.w = ev
            t.r = {}
        return ev

    def barrier(self, names=("pe", "act", "dve", "sp", "pool")):
        for a in names:
            A = self.eng[a]
            deps = {}
            for b in names:
                B = self.eng[b]
                if b != a and B.sem is not None and B.cnt > 0:
                    deps[B.sem] = B.cnt
            self._wait(A, deps)

    def mm(self, out, lhsT, rhs, start=True, stop=True, last=True, sgc=False):
        return self.op("pe", lambda e: e.matmul(out.ap, lhsT.ap, rhs.ap, start=start, stop=stop,
                                                skip_group_check=sgc),
                       reads=[lhsT, rhs], writes=[out], last=last)

    def act(self, out, in_, func, bias=None, scale=None, extra=()):
        kw = {}
        rd = [in_] + list(extra)
        if bias is not None:
            if isinstance(bias, V):
                kw["bias"] = bias.ap
                rd.append(bias)
            else:
                kw["bias"] = bias
        if scale is not None:
            if isinstance(scale, V):
                kw["scale"] = scale.ap
                rd.append(scale)
            else:
                kw["scale"] = scale
        return self.op("act", lambda e: e.activation(out=out.ap, in_=in_.ap, func=func, **kw),
                       reads=rd, writes=[out])

    def tt(self, out, in0, in1, op, eng="dve"):
        return self.op(eng, lambda e: e.tensor_tensor(out=out.ap, in0=in0.ap, in1=in1.ap, op=op),
                       reads=[in0, in1], writes=[out])

    def ts(self, out, in0, s1, s2, op0, op1=None, eng="dve"):
        rd = [in0]
        a1 = s1
        a2 = s2
        if isinstance(s1, V):
            rd.append(s1)
            a1 = s1.ap
        if isinstance(s2, V):
            rd.append(s2)
            a2 = s2.ap
        if op1 is None:
            return self.op(eng, lambda e: e.tensor_scalar(out=out.ap, in0=in0.ap, scalar1=a1, scalar2=None, op0=op0),
                           reads=rd, writes=[out])
        return self.op(eng, lambda e: e.tensor_scalar(out=out.ap, in0=in0.ap, scalar1=a1, scalar2=a2, op0=op0, op1=op1),
                       reads=rd, writes=[out])

    def stt(self, out, in0, scalar, in1, op0, op1, eng="dve"):
        rd = [in0, in1]
        a = scalar
        if isinstance(scalar, V):
            rd.append(scalar)
            a = scalar.ap
        return self.op(eng, lambda e: e.scalar_tensor_tensor(out=out.ap, in0=in0.ap, scalar=a, in1=in1.ap, op0=op0, op1=op1),
                       reads=rd, writes=[out])

    def rsqrt(self, out, in_):
        self.act(out, in_, AF.Sqrt, bias=EPS)
        return self.op("dve", lambda e: e.reciprocal(out=out.ap, in_=out.ap), reads=[out], writes=[out])

    def sigmoid(self, out, in_):
        self.act(out, in_, AF.Exp, scale=-1.0)
        self.ts(out, out, 1.0, None, ALU.add)
        return self.op("dve", lambda e: e.reciprocal(out=out.ap, in_=out.ap), reads=[out], writes=[out])

    def cp(self, out, in_, eng="dve"):
        if eng == "act":
            return self.op(eng, lambda e: e.activation(out=out.ap, in_=in_.ap, func=AF.Copy), reads=[in_], writes=[out])
        return self.op(eng, lambda e: e.tensor_copy(out=out.ap, in_=in_.ap), reads=[in_], writes=[out])

    def memset(self, out, val, eng="dve"):
        return self.op(eng, lambda e: e.memset(out.ap, val), reads=[], writes=[out])


def _colidx():
    def rng(a, n):
        return list(range(a, a + n))

    def swp(a, n):
        o = []
        for h in range(n // 64):
            o += rng(a + 64 * h + 32, 32) + rng(a + 64 * h, 32)
        return o

    b = []
    b.append(rng(0, 128)); b.append(rng(128, 128))
    b.append(rng(256, 128)); b.append(rng(384, 128))
    b.append(rng(768, 128)); b.append(rng(896, 128))
    b.append(rng(1024, 16) + [1024] * 112)
    for p in range(4):
        b.append(rng(1040 + 128 * p, 128))
    for p in range(4):
        b.append(swp(1040 + 128 * p, 128))
    b.append(rng(1552, 128)); b.append(rng(1680, 128))
    for g in range(2):
        b.append(rng(1808 + 64 * g, 64) * 2)
    for g in range(2):
        b.append(swp(1808 + 64 * g, 64) * 2)
    for g in range(2):
        b.append(rng(2064 + 64 * g, 64) * 2)
    for g in range(2):
        b.append(swp(2064 + 64 * g, 64) * 2)
    for p in range(2):
        b.append(rng(2344 + 128 * p, 128))
    for p in range(2):
        b.append(swp(2344 + 128 * p, 128))
    for p in range(2):
        b.append(rng(2600 + 128 * p, 128))
    for p in range(2):
        b.append(swp(2600 + 128 * p, 128))
    b.append(rng(3112, 128)); b.append(rng(3240, 128))
    assert len(b) == NFM
    idx = []
    for x in b:
        assert len(x) == 128
        idx += x
    idx += rng(512, 256) + rng(2856, 256)
    idx += rng(1936, 128) + rng(2192, 128) + rng(2320, 24)
    assert len(idx) == NEXT
    return np.array(idx, dtype=np.int64)


def _consts():
    c = {}
    p = np.arange(128)
    t = np.arange(T)
    invf = (10000.0 ** (-np.arange(32, dtype=np.float64) / 32.0))
    ang = t[None, :].astype(np.float64) * invf[(p % 32)][:, None]
    cos = np.cos(ang)
    sin = np.sin(ang)
    sgn = np.where((p % 64) < 32, -1.0, 1.0)[:, None]
    c["cosk"] = cos.astype(np.float32)
    c["sink"] = (sin * sgn).astype(np.float32)
    c["cosq"] = (cos * 0.125).astype(np.float32)
    c["sinq"] = (sin * sgn * 0.125).astype(np.float32)
    lg = np.log1p(-np.exp2(-5.0 - np.arange(4, dtype=np.float64)))
    i = np.arange(128, dtype=np.float64)
    decq = np.zeros((128, 2, 128)); deck = np.zeros((128, 2, 128)); rets = np.zeros((128, 2))
    for pair in range(2):
        for half in range(2):
            h = pair * 2 + half
            dq = np.exp(lg[h] * (i + 1.0))
            dk = np.exp(-lg[h] * (i + 1.0)) * 0.125
            decq[half * 64:(half + 1) * 64, pair, :] = dq[None, :]
            deck[half * 64:(half + 1) * 64, pair, :] = dk[None, :]
            rets[half * 64:(half + 1) * 64, pair] = np.exp(lg[h] * 128.0)
    c["decq"] = decq.reshape(128, 256).astype(np.float32)
    c["deck"] = deck.reshape(128, 256).astype(np.float32)
    c["rets"] = rets.astype(np.float32)
    j = np.arange(128)[:, None]
    ii = np.arange(128)[None, :]
    c["ident"] = np.eye(128, dtype=np.float32)
    c["ident4"] = np.tile(np.eye(128, dtype=np.float32), (1, 4))
    c["negc4"] = np.tile(np.where(j > ii, NEG, 0.0).astype(np.float32), (1, 4))
    c["negw4"] = np.tile(np.where(j <= ii, NEG, 0.0).astype(np.float32), (1, 4))
    c["caus4"] = np.tile((j <= ii).astype(np.float32), (1, 4))
    c["tri"] = np.where(j <= ii, -1.0 / 16.0, 0.0).astype(np.float32)
    y = np.arange(FW)[None, :]
    c["fneg"] = np.where(16 * j + 15 <= y, 0.0, NEG).astype(np.float32)
    x = np.arange(126)[None, :]
    m = x - 62
    cur = (np.arange(128)[:, None] >= 64).astype(np.int64)
    c["mulu"] = (m < cur - 1).astype(np.float32)
    addu = np.zeros((128, 126), dtype=np.float32)
    addu[np.broadcast_to(m == cur - 1, addu.shape)] = 1.2e4
    addu[np.broadcast_to(m == cur, addu.shape)] = 1.1e4
    addu[np.broadcast_to(m > cur, addu.shape)] = -1.0
    c["addu"] = addu
    ova = np.zeros((128, 2, 65), dtype=np.float32)
    for slot in range(1, 256):
        cc, sl = divmod(slot, 128)
        ova[sl, cc, 0] = 1.0
        for s in range(64):
            if 4 * s <= slot <= 4 * s + 4:
                ova[sl, cc, 1 + s] = 1.0
    c["ovaug"] = ova.reshape(128, 130)
    return c


_CONST_SHAPES = None


def _const_shapes():
    global _CONST_SHAPES
    if _CONST_SHAPES is None:
        _CONST_SHAPES = {k: v.shape for k, v in _consts().items()}
    return _CONST_SHAPES


def build(nlayers=L, ntiles=NTILE, stages=('ffn1', 'mixer', 'pall', 'lin', 'nsa', 'wout', 'ffn2')):
    nc = bass.Bass("TRN2", target_bir_lowering=False)
    dr = {}

    def din(name, shape):
        dr[name] = nc.dram_tensor(name, list(shape), F32, kind="ExternalInput").ap()
        return dr[name]

    x_d = din("x", [T, D])
    cT_d = din("cT", [128, 8])
    wada_d = din("w_ada", [L * D, 9 * D])
    bada_d = din("b_adaT", [128, L * 72])
    ng_d = din("norm_gT", [128, L * 24])
    fg_d = din("final_gT", [128, 8])
    fin_d = [din("ffn1_in", [L * D, 2 * FF]), din("ffn2_in", [L * D, 2 * FF])]
    fout_d = [din("ffn1_out", [L * FF, D]), din("ffn2_out", [L * FF, D])]
    winx_d = din("w_in_ext", [L * D, NEXT])
    a2b_d = din("a2b", [L * 17, 256])
    glag_d = din("gla_gT", [64, L])
    retg_d = din("ret_gT", [64, L])
    pek_d = din("pekT", [128, L * 32])
    pev_d = din("pevT", [128, L * 32])
    w1k_d = din("w1k", [L * 2048, 128])
    w1v_d = din("w1v", [L * 2048, 128])
    w2k_d = din("w2k", [L * 128, 64])
    w2v_d = din("w2v", [L * 128, 64])
    gb_d = din("gbias", [128, L * 24])
    wout_d = din("w_out", [L * D, D])
    cd = {k: din("c_" + k, list(s)) for k, s in _const_shapes().items()}
    out_d = nc.dram_tensor("out", [T, D], F32, kind="ExternalOutput").ap()
    xs_d = nc.dram_tensor("xscr", [D, T], F32, kind="Internal").ap()

    with ExitStack() as es:
        K = KB(nc, es)
        P = [K.ps("ps%d" % i, [128, 512]) for i in range(8)]
        xs_trk = [Tl() for _ in range(NTILE)]
        out_trk = Tl()

        def cload(name, dt, q=None):
            shp = _const_shapes()[name]
            t = K.sb("c_" + name, shp, dt)
            K.dma("pool" if dt == BF16 else "sp", t.t[:], cd[name][:, :], writes=[t])
            return t

        ident_bf = cload("ident", BF16)
        ident_f = cload("ident", F32)
        ident4 = cload("ident4", BF16)
        negc4 = cload("negc4", BF16)
        negw4 = cload("negw4", BF16)
        caus4 = cload("caus4", BF16)
        tri = cload("tri", F32)
        fneg = cload("fneg", BF16)
        mulu = cload("mulu", F32)
        addu = cload("addu", F32)
        rets = cload("rets", F32)
        decq = cload("decq", F32)
        deck = cload("deck", F32)
        ones128 = K.sb("ones128", [128, 128], BF16)
        K.memset(ones128[:, :], 1.0 / 1024.0)
        ones64 = K.sb("ones64", [64, 64], BF16)
        K.memset(ones64[:, :], 1.0 / 64.0)

        xT = K.sb("xT", [128, 8, TT], F32)
        hT = K.sb("hT", [128, 8, TT], BF16)
        rstd = K.sb("rstd", [128, TT], F32)
        tmp = [K.sb("tmp%d" % i, [128, TT], F32) for i in range(3)]
        tmpi = [0]

        def ntmp():
            tmpi[0] += 1
            return tmp[tmpi[0] % 3]

        modall = K.sb("modall", [128, L * 72], F32)
        Amod = K.sb("Amod", [128, L * 24], F32)
        Gmod = K.sb("Gmod", [128, L * 24], F32)
        ngT = K.sb("ngT", [128, L * 24], F32)
        K.dma("sp", ngT.t[:], ng_d[:, :], writes=[ngT])
        fgT = K.sb("fgT", [128, 8], F32)
        K.dma("sp", fgT.t[:], fg_d[:, :], writes=[fgT])
        glag = K.sb("glag", [64, L], F32)
        K.dma("sp", glag.t[:], glag_d[:, :], writes=[glag])
        retg = K.sb("retg", [64, L], F32)
        K.dma("sp", retg.t[:], retg_d[:, :], writes=[retg])
        gbrow = K.sb("gbrow", [1, L * 24], BF16)
        K.dma("pool", gbrow.t[:], gb_d[0:1, :], writes=[gbrow])
        onesrow = K.sb("onesrow", [1, 128], BF16)
        K.memset(onesrow[:, :], 1.0)

        NFB = 3
        fwi = [0]
        NOB = 4
        foi = [0]
        NWB = 4
        wii = [0]
        wti = [0]

        class NS:
            pass

        B = NS()

        def alloc_ffn_bufs(pes):
            B.fwb = [K.sb("fwb%d" % i, [128, 8, 256], BF16, es=pes) for i in range(NFB)]
            B.fob = [K.sb("fob%d" % i, [128, 512], BF16, es=pes) for i in range(NOB)]

        a2b = K.sb("a2b", [17, 256], BF16)
        w2k = K.sb("w2k", [128, 128], BF16)
        w2v = K.sb("w2v", [128, 64], BF16)
        pek = K.sb("pek", [128, 32], BF16)
        pev = K.sb("pev", [128, 32], BF16)
        pebk = K.sb("pebk", [128, 1], F32)
        pebv = K.sb("pebv", [128, 1], F32)

        ksc = K.sb("ksc", [128, 2, T], BF16)
        kwc = K.sb("kwc", [128, 2, 1024], BF16)
        vsc = K.sb("vsc", [128, 32, 2, 80], BF16)
        vwc = K.sb("vwc", [128, 8, 2, 80], BF16)
        kcc = K.sb("kcc", [128, 2, 256], BF16)
        vca = K.sb("vca", [128, 2, 2, 136], BF16)
        kcb = K.sb("kcb", [128, 16 + TT], BF16)
        vcb = K.sb("vcb", [128, 16 + TT], BF16)
        hvp = K.sb("hvp", [128, 2, 128], BF16)
        K.memset(ksc[:, :, :], 0.0)
        K.memset(kwc[:, :, :], 0.0)
        K.memset(vsc[:, :, :, :], 1.0)
        K.memset(vwc[:, :, :, :], 1.0)
        K.memset(kcc[:, :, :], 0.0)
        K.memset(vca[:, :, :, :], 0.0)
        K.memset(kcb[:, :], 0.0)
        K.memset(vcb[:, :], 0.0)
        K.memset(hvp[:, :, :], 0.0)
        for g in range(2):
            for cc in range(2):
                K.dma("pool", vca.t[:, g, cc, 64:129], cd["ovaug"][:, cc * 65:(cc + 1) * 65], writes=[vca])

        S_f = {"g": K.sb("Sg", [128, 2, 128], F32), "r": K.sb("Sr", [128, 2, 128], F32)}
        S_b = {"g": K.sb("Sgb", [128, 2, 128], BF16), "r": K.sb("Srb", [128, 2, 128], BF16)}

        cact = K.sb("cact", [128, 8], BF16)
        ctmp = K.sb("ctmp", [128, 8], F32)
        K.dma("sp", ctmp.t[:], cT_d[:, :], writes=[ctmp])
        K.act(cact[:, :], ctmp[:, :], AF.Silu)
        badaT = K.sb("badaT", [128, L * 72], F32)
        K.dma("sp", badaT.t[:], bada_d[:, :], writes=[badaT])
        PM = P[7]
        pes0 = ExitStack()
        alloc_ffn_bufs(pes0)
        for l in range(nlayers):
            for cg in range(36):
                buf = B.fwb[fwi[0] % NFB]
                fwi[0] += 1
                K.dma("pool", buf.t[:, :, :],
                      wada_d[l * D:(l + 1) * D, cg * 256:(cg + 1) * 256].rearrange("(k p) c -> p k c", p=128),
                      writes=[buf])
                for jj in range(2):
                    col = (l * 72 + cg * 2 + jj) % 512
                    for k in range(8):
                        K.mm(PM[:, col:col + 1], buf[:, k, jj * 128:(jj + 1) * 128], cact[:, k:k + 1],
                             start=(k == 0), stop=(k == 7), last=(k == 7))
            K.tt(modall[:, l * 72:(l + 1) * 72], PM[:, (l * 72) % 512:(l * 72) % 512 + 72],
                 badaT[:, l * 72:(l + 1) * 72], ALU.add)
            for i in range(3):
                sc = modall[:, l * 72 + (3 * i + 1) * 8: l * 72 + (3 * i + 1) * 8 + 8]
                gt = modall[:, l * 72 + (3 * i + 2) * 8: l * 72 + (3 * i + 2) * 8 + 8]
                K.stt(Amod[:, (l * 3 + i) * 8:(l * 3 + i) * 8 + 8], sc, 1.0,
                      ngT[:, (l * 3 + i) * 8:(l * 3 + i) * 8 + 8], ALU.add, ALU.mult)
                K.ts(Gmod[:, (l * 3 + i) * 8:(l * 3 + i) * 8 + 8], gt, 1.0 if i == 1 else 0.5, None, ALU.mult)

        K.barrier()
        pes0.close()

        def Bmod(l, i, k):
            c0 = l * 72 + (3 * i) * 8 + k
            return modall[:, c0:c0 + 1]

        def norm_mod(l, i):
            for k in range(8):
                K.act(hT[:, k, :], xT[:, k, :], AF.Square)
            for k in range(8):
                K.mm(P[7][:, :], ones128[:, :], hT[:, k, :], start=(k == 0), stop=(k == 7), last=(k == 7))
            K.rsqrt(rstd[:, :], P[7][:, :])
            for k in range(8):
                t1 = ntmp()
                K.tt(t1[:, :], xT[:, k, :], rstd[:, :], ALU.mult)
                c0 = (l * 3 + i) * 8 + k
                K.act(hT[:, k, :], t1[:, :], AF.Identity, bias=Bmod(l, i, k), scale=Amod[:, c0:c0 + 1])

        def ffn(l, w, aT):
            i = 0 if w == 0 else 2
            norm_mod(l, i)
            win = fin_d[w]
            wout = fout_d[w]
            for j in range(NJ):
                buf = B.fwb[fwi[0] % NFB]
                fwi[0] += 1
                for gu in range(2):
                    c0 = gu * FF + j * 128
                    K.dma("pool", buf.t[:, :, gu * 128:(gu + 1) * 128],
                          win[l * D:(l + 1) * D, c0:c0 + 128].rearrange("(k p) c -> p k c", p=128), writes=[buf])
                pg = P[(j % 2) * 2]
                pu = P[(j % 2) * 2 + 1]
                for k in range(8):
                    K.mm(pg[:, :], buf[:, k, 0:128], hT[:, k, :], start=(k == 0), stop=(k == 7), last=(k == 7))
                for k in range(8):
                    K.mm(pu[:, :], buf[:, k, 128:256], hT[:, k, :], start=(k == 0), stop=(k == 7), last=(k == 7))
                t1 = ntmp()
                K.act(t1[:, :], pg[:, :], AF.Silu)
                K.tt(aT[:, j, :], t1[:, :], pu[:, :], ALU.mult)
            for mg in range(2):
                for j in range(NJ):
                    ob = B.fob[foi[0] % NOB]
                    foi[0] += 1
                    K.dma("pool", ob.t[:, :], wout[l * FF + j * 128: l * FF + (j + 1) * 128, mg * 512:(mg + 1) * 512],
                          writes=[ob])
                    for m in range(4):
                        K.mm(P[4 + m][:, :], ob[:, m * 128:(m + 1) * 128], aT[:, j, :],
                             start=(j == 0), stop=(j == NJ - 1))
                for m in range(4):
                    k = mg * 4 + m
                    c0 = (l * 3 + i) * 8 + k
                    K.stt(xT[:, k, :], P[4 + m][:, :], Gmod[:, c0:c0 + 1], xT[:, k, :], ALU.mult, ALU.add)

        def load_w1(l, pl):
            B.w1k = K.sb("w1k", [128, 32, 128], BF16, es=pl)
            B.w1v = K.sb("w1v", [128, 32, 128], BF16, es=pl)
            for half in range(2):
                K.dma("pool", B.w1k.t[half * 64:(half + 1) * 64, :, :],
                      w1k_d[l * 2048:(l + 1) * 2048, :].rearrange("(l d) h -> d l h", d=64), writes=[B.w1k])
                K.dma("pool", B.w1v.t[half * 64:(half + 1) * 64, :, :],
                      w1v_d[l * 2048:(l + 1) * 2048, :].rearrange("(l d) h -> d l h", d=64), writes=[B.w1v])

        def layer_setup(l):
            pl = ExitStack()
            load_w1(l, pl)
            K.dma("pool", a2b.t[:, :], a2b_d[l * 17:(l + 1) * 17, :], writes=[a2b])
            for half in range(2):
                K.dma("pool", w2k.t[:, half * 64:(half + 1) * 64], w2k_d[l * 128:(l + 1) * 128, :], writes=[w2k])
            K.dma("pool", w2v.t[:, :], w2v_d[l * 128:(l + 1) * 128, :], writes=[w2v])
            K.dma("pool", pek.t[:, :], pek_d[:, l * 32:(l + 1) * 32], writes=[pek])
            K.dma("pool", pev.t[:, :], pev_d[:, l * 32:(l + 1) * 32], writes=[pev])
            for (w1, pe, peb) in ((B.w1k, pek, pebk), (B.w1v, pev, pebv)):
                for ll in range(32):
                    K.mm(P[6][:, 0:1], w1[0:64, ll, :], pe[0:64, ll:ll + 1], start=(ll == 0), stop=(ll == 31),
                         last=(ll == 31))
                K.cp(peb[:, :], P[6][:, 0:1])
            for kind in ("g", "r"):
                K.memset(S_f[kind][:, :, :], 0.0)
                K.memset(S_b[kind][:, :, :], 0.0)
            K.memset(hvp[:, :, :], 0.0)
            K.barrier()
            pl.close()

        def mixer(l, tt, pes):
            cur = [pes]

            def sbl(name, shape, dt):
                return K.sb(name, shape, dt, es=cur[0])

            gqT = sbl("gqT", [128, 2, TT], BF16)
            gkT = sbl("gkT", [128, 2, TT], BF16)
            ggT = sbl("ggT", [64, 4, TT], BF16)
            glrT = sbl("glrT", [17, TT], BF16)
            nqr = sbl("nqr", [128, 4, TT], BF16)
            nqo = sbl("nqo", [128, 4, TT], BF16)
            rqT = sbl("rqT", [128, 2, TT], BF16)
            rkT = sbl("rkT", [128, 2, TT], BF16)
            rgT = sbl("rgT", [64, 4, TT], BF16)
            gv = sbl("gv", [128, 4, 256], BF16)
            rv = sbl("rv", [128, 4, 256], BF16)
            sig = sbl("sig", [128, 4, 24], F32)
            ogT = sbl("ogT", [64, 4, TT], BF16)
            orT = sbl("orT", [64, 4, TT], BF16)
            onT = sbl("onT", [128, 4, TT], BF16)
            pa = ExitStack()
            cur[0] = pa
            B.wib = [sbl("wib%d" % i, [128, 8, 128], BF16) for i in range(NWB)]
            B.wtb = [sbl("wtb%d" % i, [128, 8, 280], BF16) for i in range(2)]
            rot = {}
            for nm in ("cosq", "sinq", "cosk", "sink"):
                rot[nm] = sbl(nm, [128, TT], F32)
                K.dma("sp", rot[nm].t[:, :], cd[nm][:, tt * TT:(tt + 1) * TT], writes=[rot[nm]])
            K.memset(glrT[:, :], 1.0)

            pcur = [0]

            def fm(b, M=128, col0=0, cache={}):
                if cache.get("b") != b:
                    buf = B.wib[wii[0] % NWB]
                    wii[0] += 1
                    K.dma("pool", buf.t[:, :, :],
                          winx_d[l * D:(l + 1) * D, b * 128:(b + 1) * 128].rearrange("(k p) c -> p k c", p=128),
                          writes=[buf])
                    cache["b"] = b
                    cache["buf"] = buf
                buf = cache["buf"]
                ps = P[pcur[0] % 4]
                pcur[0] += 1
                for k in range(8):
                    K.mm(ps[0:M, :], buf[:, k, col0:col0 + M], hT[:, k, :], start=(k == 0), stop=(k == 7),
                         last=(k == 7))
                return ps

            fmc = {}
            G = lambda nm, n: (n if (nm in stages or 'pall' in stages) else 0)
            for p in range(G('pA', 2)):
                K.cp(gqT[:, p, :], fm(0 + p, cache=fmc)[:, :], eng="act")
                K.cp(gkT[:, p, :], fm(2 + p, cache=fmc)[:, :], eng="act")
            for p in range(G('pB', 2)):
                for hh in range(2):
                    ps = fm(4 + p, 64, hh * 64, cache=fmc)
                    K.act(ggT[:, 2 * p + hh, :], ps[0:64, :], AF.Silu)
            for p in range(G('pB', 2)):
                for hh in range(2):
                    ps = fm(33 + p, 64, hh * 64, cache=fmc)
                    K.act(rgT[:, 2 * p + hh, :], ps[0:64, :], AF.Silu)
            for _ in range(G('pC', 1)):
                ps = fm(6, 16, 0, cache=fmc)
                K.cp(glrT[0:16, :], ps[0:16, :], eng="act")
            for p in range(G('pD', 4)):
                K.act(nqr[:, p, :], fm(7 + p, cache=fmc)[:, :], AF.Copy, scale=0.125)

            def rotj(braw, bsw, cosn, sinn, dest, dec=None):
                t1 = ntmp()
                K.tt(t1[:, :], fm(braw, cache=fmc)[:, :], rot[cosn][:, :], ALU.mult)
                t2 = ntmp()
                K.tt(t2[:, :], fm(bsw, cache=fmc)[:, :], rot[sinn][:, :], ALU.mult)
                if dec is None:
                    K.tt(dest, t1[:, :], t2[:, :], ALU.add)
                else:
                    K.tt(t1[:, :], t1[:, :], t2[:, :], ALU.add)
                    dtile, dp, dtab = dec
                    for s4 in range(4):
                        K.tt(dtile[:, dp, s4 * 128:(s4 + 1) * 128], t1[:, s4 * 128:(s4 + 1) * 128],
                             dtab[:, dp * 128:(dp + 1) * 128], ALU.mult)

            for p in range(G('pE', 4)):
                rotj(7 + p, 11 + p, "cosq", "sinq", nqo[:, p, :])
            for _ in range(G('pF', 1)):
                K.cp(kcb[:, 0:16], kcb[:, TT:TT + 16])
                K.cp(vcb[:, 0:16], vcb[:, TT:TT + 16])
                K.cp(kcb[:, 16:16 + TT], fm(15, cache=fmc)[:, :], eng="act")
                K.cp(vcb[:, 16:16 + TT], fm(16, cache=fmc)[:, :], eng="act")
            for g in range(G('pG', 2)):
                rotj(17 + g, 19 + g, "cosk", "sink", ksc[:, g, tt * TT:(tt + 1) * TT])
                w0 = (tt % 2) * TT
                rotj(21 + g, 23 + g, "cosk", "sink", kwc[:, g, w0:w0 + TT])
            for p in range(G('pH', 2)):
                rotj(25 + p, 27 + p, "cosk", "sink", None, dec=(rqT, p, decq))
                rotj(29 + p, 31 + p, "cosk", "sink", None, dec=(rkT, p, deck))

            for (c0, n, kindtm) in ((TMA1, 256, "gv"), (TMA2, 256, "rv"), (TMB, 280, "b"))[:max(G('pT', 3), 2 if 'pT2' in stages else 0)]:
                buf = B.wtb[wti[0] % 2]
                wti[0] += 1
                K.dma("pool", buf.t[:, :, 0:n],
                      winx_d[l * D:(l + 1) * D, c0:c0 + n].rearrange("(k p) c -> p k c", p=128), writes=[buf])
                for s in range(4):
                    ps = P[pcur[0] % 4]
                    pcur[0] += 1
                    for k in range(8):
                        K.mm(ps[:, 0:n], hT[:, k, s * 128:(s + 1) * 128], buf[:, k, 0:n], start=(k == 0),
                             stop=(k == 7), last=(k == 7 and kindtm != "b"), sgc=(kindtm == "b"))
                    if kindtm == "b":
                        K.mm(ps[:, 256:280], onesrow[0:1, :], gbrow[0:1, l * 24:(l + 1) * 24], start=False, stop=True,
                             sgc=True)
                    if kindtm == "gv":
                        K.cp(gv[:, s, :], ps[:, 0:256], eng="act")
                    elif kindtm == "rv":
                        K.cp(rv[:, s, :], ps[:, 0:256], eng="act")
                    else:
                        ca = tt * 4 + s
                        for g in range(0 if 'nob2' in stages else 2):
                            K.cp(vsc[:, ca, g, 0:64], ps[:, g * 64:(g + 1) * 64], eng="act")
                            K.cp(vwc[:, ca % 8, g, 0:64], ps[:, 128 + g * 64:128 + (g + 1) * 64], eng="act")
                        if 'nob3' not in stages:
                            K.sigmoid(sig[:, s, :], ps[:, 256:280])

            K.barrier()
            pa.close()
            pb_ = ExitStack()
            cur[0] = pb_
            lt = {}
            for nm, shp, dt in (("L1", [128, 256], F32), ("eb", [128, 256], F32), ("enb", [128, 256], F32),
                                ("qd", [128, 2, 128], BF16), ("kd", [128, 2, 128], BF16),
                                ("am", [128, 512], BF16), ("ktok", [128, 256], BF16), ("tS", [128, 128], F32),
                                ("sq", [64, 512], BF16), ("osb", [64, 512], F32), ("obf", [64, 512], BF16),
                                ("rs", [64, 512], F32), ("xc", [64, 512], F32)):
                lt[nm] = sbl("lt_" + nm, shp, dt)

            def linattn(kind, s):
                PA, PK, PO, PV, PN, PB = P[0], P[1], P[2], P[3], P[4], P[5]
                cs = slice(s * 128, (s + 1) * 128)
                if kind == "g":
                    K.mm(PB[:, 0:256], glrT[0:17, cs], a2b[0:17, :])
                    K.act(lt["L1"][:, :], PB[:, 0:256], AF.Exp, scale=-1.0)
                    K.act(lt["L1"][:, :], lt["L1"][:, :], AF.Ln, bias=1.0)
                    for p in range(2):
                        K.mm(PB[:, 256 + p * 128:256 + (p + 1) * 128], lt["L1"][:, p * 128:(p + 1) * 128], tri[:, :])
                    K.act(lt["eb"][:, :], PB[:, 256:512], AF.Exp)
                    K.act(lt["enb"][:, :], PB[:, 256:512], AF.Exp, scale=-1.0, bias=math.log(0.125))
                    for p in range(2):
                        K.tt(lt["qd"][:, p, :], gqT[:, p, cs], lt["eb"][:, p * 128:(p + 1) * 128], ALU.mult)
                        K.tt(lt["kd"][:, p, :], gkT[:, p, cs], lt["enb"][:, p * 128:(p + 1) * 128], ALU.mult)
                    qd = lambda p, a, b: lt["qd"][a:b, p, :]
                    kd = lambda p, a, b: lt["kd"][a:b, p, :]
                    dec = lambda p: lt["eb"][:, p * 128 + 127:p * 128 + 128]
                    vt = gv
                    gate = ggT
                    dst = ogT
                    gn = glag
                else:
                    qd = lambda p, a, b: rqT[a:b, p, cs]
                    kd = lambda p, a, b: rkT[a:b, p, cs]
                    dec = lambda p: rets[:, p:p + 1]
                    vt = rv
                    gate = rgT
                    dst = orT
                    gn = retg
                Sf, Sb = S_f[kind], S_b[kind]
                for h in range(4):
                    p, hb = h // 2, (h % 2) * 64
                    K.mm(PA[:, h * 128:(h + 1) * 128], kd(p, hb, hb + 64), qd(p, hb, hb + 64))
                K.tt(lt["am"][:, :], PA[:, :], caus4[:, :], ALU.mult)
                for p in range(2):
                    K.mm(PK[:, p * 128:(p + 1) * 128], kd(p, 0, 128), ident_bf[:, :])
                K.cp(lt["ktok"][:, :], PK[:, 0:256], eng="act")
                for h in range(4):
                    p, hb = h // 2, (h % 2) * 64
                    K.mm(PO[0:64, h * 128:(h + 1) * 128], vt[:, s, h * 64:(h + 1) * 64],
                         lt["am"][:, h * 128:(h + 1) * 128], start=True, stop=False, last=False)
                    K.mm(PO[0:64, h * 128:(h + 1) * 128], Sb[hb:hb + 64, p, hb:hb + 64], qd(p, hb, hb + 64),
                         start=False, stop=True)
                for p in range(2):
                    K.mm(PV[:, p * 128:(p + 1) * 128], lt["ktok"][:, p * 128:(p + 1) * 128],
                         vt[:, s, p * 128:(p + 1) * 128])
                for p in range(2):
                    K.ts(lt["tS"][:, :], PV[:, p * 128:(p + 1) * 128], dec(p), None, ALU.mult)
                    K.stt(Sf[:, p, :], Sf[:, p, :], dec(p), lt["tS"][:, :], ALU.mult, ALU.add)
                    K.cp(Sb[:, p, :], Sf[:, p, :], eng="act")
                if kind == "g":
                    K.act(lt["sq"][:, :], PO[0:64, :], AF.Square)
                    K.mm(PN[0:64, :], ones64[:, :], lt["sq"][:, :])
                    K.rsqrt(lt["rs"][:, :], PN[0:64, :])
                    K.tt(lt["xc"][:, :], PO[0:64, :], lt["rs"][:, :], ALU.mult)
                else:
                    K.cp(lt["osb"][:, :], PO[0:64, :], eng="act")
                    K.cp(lt["obf"][:, :], PO[0:64, :], eng="act")
                    K.mm(PN[0:64, :], ones64[:, :], lt["obf"][:, :])
                    K.tt(lt["xc"][:, :], lt["osb"][:, :], PN[0:64, :], ALU.subtract)
                    K.act(lt["sq"][:, :], lt["xc"][:, :], AF.Square)
                    K.mm(PN[0:64, :], ones64[:, :], lt["sq"][:, :])
                    K.rsqrt(lt["rs"][:, :], PN[0:64, :])
                    K.tt(lt["xc"][:, :], lt["xc"][:, :], lt["rs"][:, :], ALU.mult)
                for h in range(4):
                    K.stt(dst[:, h, cs], lt["xc"][:, h * 128:(h + 1) * 128], gn[:, l:l + 1], gate[:, h, cs],
                          ALU.mult, ALU.mult)

            for s in range(4 if 'lin' in stages else 0):
                linattn("g", s)
                linattn("r", s)

            K.barrier()
            pb_.close()
            pc_ = ExitStack()
            cur[0] = pc_
            load_w1(l, pc_)
            hk = sbl("hk", [128, 32], F32)
            hs = sbl("hs", [128, 32], F32)
            hkb = sbl("hkb", [128, 32], BF16)
            m0 = 1 if tt == 0 else 0
            nm_ = 32 - m0
            cchunk = (32 * tt) // 128
            soff = (32 * tt) % 128
            for g in range(2 if 'nsa' in stages else 0):
                gb = g * 64
                for (w1, buf, peb, isk) in ((B.w1k, kcb, pebk, True), (B.w1v, vcb, pebv, False)):
                    for ll in range(32):
                        K.mm(P[6][:, 0:nm_], w1[gb:gb + 64, ll, :],
                             V(buf, buf.t[gb:gb + 64, 16 * m0 + ll: 16 * m0 + ll + 16 * (nm_ - 1) + 1: 16]),
                             start=(ll == 0), stop=(ll == 31), last=(ll == 31))
                    K.act(hk[:, 0:nm_], P[6][:, 0:nm_], AF.Identity, bias=peb[:, 0:1])
                    K.sigmoid(hs[:, 0:nm_], hk[:, 0:nm_])
                    if isk:
                        K.tt(hkb[:, 0:nm_], hk[:, 0:nm_], hs[:, 0:nm_], ALU.mult)
                        K.mm(P[6][:, 64:64 + nm_], w2k[:, :], hkb[:, 0:nm_])
                        K.cp(kcc[:, g, 32 * tt + m0: 32 * tt + 32], P[6][:, 64:64 + nm_], eng="act")
                    else:
                        K.tt(hvp[:, g, soff + m0: soff + 32], hk[:, 0:nm_], hs[:, 0:nm_], ALU.mult)
                        K.mm(P[6][:, 128:192], hvp[:, g, :], w2v[:, :])
                        K.cp(vca[:, g, cchunk, 0:64], P[6][:, 128:192], eng="act")
            if soff == 96:
                K.memset(hvp[:, :, :], 0.0)

            nt = {}
            for nm, shp, dt in (("e0", [128, 512], BF16), ("e1", [128, 512], BF16), ("e2", [128, 512], BF16),
                                ("zt", [128, 12], F32), ("cf", [128, 12], F32), ("imp", [128, 64], F32),
                                ("imp2", [128, 64], F32), ("w1", [128, 64], F32), ("w2", [128, 64], F32),
                                ("m8", [128, 8], F32), ("nsb", [128, 64], BF16), ("nse", [128, 64, 64], BF16),
                                ("ont", [128, 512], BF16)):
                nt[nm] = sbl("nt_" + nm, shp, dt)
            ei = [0]

            def branch(kq, kcache_fn, vfn, chunks, acc_fn, maskfn, g, s, first_r=(0,)):
                cs = slice(s * 128, (s + 1) * 128)
                for ci, c in enumerate(chunks):
                    ps = P[ei[0] % 2]
                    ms = maskfn(c)
                    for r in range(4):
                        pr, hb = 2 * g + r // 2, (r % 2) * 64
                        K.mm(ps[:, r * 128:(r + 1) * 128], kcache_fn(c, hb), kq[hb:hb + 64, pr, cs],
                             start=(r == 0), stop=(len(ms) == 0), sgc=True)
                    for mi, (ml, mr, full) in enumerate(ms):
                        if full:
                            K.mm(ps[:, :], ml, mr, start=False, stop=(mi == len(ms) - 1), sgc=True)
                        else:
                            for r in range(4):
                                K.mm(ps[:, r * 128:(r + 1) * 128], ml, mr, start=False, stop=(mi == len(ms) - 1), sgc=True)
                    e = nt["e%d" % (ei[0] % 3)]
                    ei[0] += 1
                    K.act(e[:, :], ps[:, :], AF.Exp)
                    for r in range(4):
                        K.mm(acc_fn(r), e[:, r * 128:(r + 1) * 128], vfn(c), start=(ci == 0 and r in first_r),
                             stop=(ci == len(chunks) - 1), sgc=True)

            for s in range(4 if 'nsa' in stages else 0):
                qa = tt * 4 + s
                for g in range(2):
                    cch = [0] if qa < 16 else [0, 1]

                    def cmask(c):
                        u = qa - 16 * c
                        if u > 16:
                            return []
                        return [(ident_bf[:, :], fneg[:, 128 * u:128 * u + 128], False)]

                    branch(nqr, lambda c, hb: kcc[hb:hb + 64, g, c * 128:(c + 1) * 128],
                           lambda c: vca[:, g, c, 0:130], cch,
                           lambda r: P[2 + r // 2][:, (r % 2) * 136:(r % 2) * 136 + 130], cmask, g, s, first_r=(0, 2))
                    wch = list(range(max(0, qa - 4), qa + 1))

                    def wmask(c):
                        m = []
                        if c == qa:
                            m.append((ident_bf[:, :], negc4[:, :], True))
                        if c == qa - 4:
                            m.append((ident_bf[:, :], negw4[:, :], True))
                        return m

                    branch(nqo, lambda c, hb: kwc[hb:hb + 64, g, (c % 8) * 128:(c % 8 + 1) * 128],
                           lambda c: vwc[:, c % 8, g, 0:66], wch,
                           lambda r: P[5][:, r * 72:r * 72 + 66], wmask, g, s)
                    zt, cf = nt["zt"], nt["cf"]
                    for bk in range(2):
                        K.cp(V(zt, zt.t[:, 6 * bk:6 * bk + 6:3]), V(P[2 + bk], P[2 + bk].t[:, 64:64 + 272:136]))
                    K.ts(zt[:, 0:12:3], zt[:, 0:12:3], 1e-30, None, ALU.add)
                    K.op("dve", lambda e: e.reciprocal(out=cf.t[:, 0:12:3], in_=zt.t[:, 0:12:3]), reads=[zt], writes=[cf])
                    for r in range(4):
                        src = P[2 + r // 2][:, (r % 2) * 136 + 65:(r % 2) * 136 + 129]
                        if r == 0:
                            K.ts(nt["imp"][:, :], src, cf[:, 0:1], None, ALU.mult)
                        else:
                            K.stt(nt["imp"][:, :], src, cf[:, 3 * r:3 * r + 1], nt["imp"][:, :], ALU.mult, ALU.add)
                    x0 = 62 - 2 * qa
                    K.tt(nt["imp2"][:, :], nt["imp"][:, :], mulu[:, x0:x0 + 64], ALU.mult)
                    K.tt(nt["imp2"][:, :], nt["imp2"][:, :], addu[:, x0:x0 + 64], ALU.add)
                    K.memset(nt["imp2"][:, 0:1], 1.0e4)
                    K.op("dve", lambda e: e.max(out=nt["m8"].t[:, :], in_=nt["imp2"].t[:, :]),
                         reads=[nt["imp2"]], writes=[nt["m8"]])
                    K.op("dve", lambda e: e.match_replace(out=nt["w1"].t[:, :], in_to_replace=nt["m8"].t[:, :],
                                                          in_values=nt["imp2"].t[:, :], imm_value=-1.0e9),
                         reads=[nt["m8"], nt["imp2"]], writes=[nt["w1"]])
                    K.op("dve", lambda e: e.max(out=nt["m8"].t[:, :], in_=nt["w1"].t[:, :]),
                         reads=[nt["w1"]], writes=[nt["m8"]])
                    K.op("dve", lambda e: e.match_replace(out=nt["w2"].t[:, :], in_to_replace=nt["m8"].t[:, :],
                                                          in_values=nt["w1"].t[:, :], imm_value=-1.0e9),
                         reads=[nt["m8"], nt["w1"]], writes=[nt["w2"]])
                    K.tt(nt["w1"][:, :], nt["imp2"][:, :], nt["w2"][:, :], ALU.is_gt)
                    K.ts(nt["nsb"][:, :], nt["w1"][:, :], -1.0, -NEG, ALU.add, ALU.mult)
                    K.op("dve", lambda e: e.tensor_copy(
                        out=nt["nse"].t[:, :, :],
                        in_=nt["nsb"].t[:, :].unsqueeze(2).to_broadcast([128, 64, 64])),
                        reads=[nt["nsb"]], writes=[nt["nse"]])
                    sch = list(range(0, qa + 1))

                    def smask(c):
                        m = [(V(nt["nse"], nt["nse"].t[:, 2 * c:2 * c + 2, :]), ident4[:, :], True)]
                        if c == qa:
                            m.append((ident_bf[:, :], negc4[:, :], True))
                        return m

                    branch(nqo, lambda c, hb: ksc[hb:hb + 64, g, c * 128:(c + 1) * 128],
                           lambda c: vsc[:, c, g, 0:66], sch,
                           lambda r: P[4][:, r * 72:r * 72 + 66], smask, g, s)
                    K.cp(V(zt, zt.t[:, 1:12:3]), V(P[4], P[4].t[:, 64:288:72]))
                    K.cp(V(zt, zt.t[:, 2:12:3]), V(P[5], P[5].t[:, 64:288:72]))
                    K.ts(zt[:, :], zt[:, :], 1e-30, None, ALU.add)
                    K.op("dve", lambda e: e.reciprocal(out=cf.t[:, :], in_=zt.t[:, :]), reads=[zt], writes=[cf])
                    K.tt(cf[:, :], cf[:, :], sig[:, s, 12 * g:12 * g + 12], ALU.mult)
                    for r in range(4):
                        dst = nt["ont"][:, (4 * g + r) * 64:(4 * g + r + 1) * 64]
                        K.ts(dst, P[2 + r // 2][:, (r % 2) * 136:(r % 2) * 136 + 64], cf[:, 3 * r:3 * r + 1], None, ALU.mult)
                        K.stt(dst, P[4][:, r * 72:r * 72 + 64], cf[:, 3 * r + 1:3 * r + 2], dst, ALU.mult, ALU.add)
                        K.stt(dst, P[5][:, r * 72:r * 72 + 64], cf[:, 3 * r + 2:3 * r + 3], dst, ALU.mult, ALU.add)
                for p in range(4):
                    K.mm(P[6][:, p * 128:(p + 1) * 128], nt["ont"][:, p * 128:(p + 1) * 128], ident_bf[:, :])
                for p in range(4):
                    K.cp(onT[:, p, s * 128:(s + 1) * 128], P[6][:, p * 128:(p + 1) * 128], eng="act")

            K.barrier()
            pc_.close()
            cur[0] = pes
            B.wogr = sbl("wogr", [64, 8, 512], BF16)
            B.wons = sbl("wons", [128, 4, 512], BF16)
            for mg in range(2 if 'wout' in stages else 0):
                K.dma("pool", B.wogr.t[:, 0:4, :],
                      wout_d[l * D:l * D + 256, mg * 512:(mg + 1) * 512].rearrange("(h p) c -> p h c", p=64),
                      writes=[B.wogr])
                K.dma("pool", B.wogr.t[:, 4:8, :],
                      wout_d[l * D + 768:l * D + 1024, mg * 512:(mg + 1) * 512].rearrange("(h p) c -> p h c", p=64),
                      writes=[B.wogr])
                K.dma("pool", B.wons.t[:, :, :],
                      wout_d[l * D + 256:l * D + 768, mg * 512:(mg + 1) * 512].rearrange("(h p) c -> p h c", p=128),
                      writes=[B.wons])
                for m in range(4):
                    ps = P[m % 4]
                    ms = slice(m * 128, (m + 1) * 128)
                    for h in range(4):
                        K.mm(ps[:, :], B.wogr[0:64, h, ms], ogT[0:64, h, :], start=(h == 0), stop=False, last=False)
                    for h in range(4):
                        K.mm(ps[:, :], B.wogr[0:64, 4 + h, ms], orT[0:64, h, :], start=False, stop=False, last=False)
                    for p in range(4):
                        K.mm(ps[:, :], B.wons[:, p, ms], onT[:, p, :], start=False, stop=(p == 3), last=(p == 3))
                    k = mg * 4 + m
                    c0 = (l * 3 + 1) * 8 + k
                    K.stt(xT[:, k, :], ps[:, :], Gmod[:, c0:c0 + 1], xT[:, k, :], ALU.mult, ALU.add)

        B.xtok = K.sb("xtok", [128, D], F32)
        for l in range(nlayers):
            layer_setup(l)
            for tt in range(ntiles):
                if l == 0:
                    for s in range(4):
                        r0 = tt * TT + s * 128
                        K.dma("sp", B.xtok.t[:, :], x_d[r0:r0 + 128, :], writes=[B.xtok])
                        for k in range(8):
                            pb = P[k // 4]
                            K.op("pe", lambda e: e.transpose(out=pb.t[:, (k % 4) * 128:(k % 4 + 1) * 128],
                                                             in_=B.xtok.t[:, k * 128:(k + 1) * 128],
                                                             identity=ident_f.t[:, :]),
                                 reads=[B.xtok, ident_f], writes=[pb])
                        for k in range(8):
                            K.cp(xT[:, k, s * 128:(s + 1) * 128], P[k // 4][:, (k % 4) * 128:(k % 4 + 1) * 128],
                                 eng=("act" if k % 2 else "dve"))
                else:
                    K.dma("sp", xT.t[:, :, :],
                          xs_d[:, tt * TT:(tt + 1) * TT].rearrange("(k p) t -> p k t", p=128),
                          reads=[xs_trk[tt]], writes=[xT])
                for w in range(2):
                    if ('ffn1' if w == 0 else 'ffn2') in stages:
                        with ExitStack() as pes:
                            aT = K.sb("aT", [128, NJ, TT], BF16, es=pes)
                            alloc_ffn_bufs(pes)
                            ffn(l, w, aT)
                            K.barrier()
                    if w == 0 and 'mixer' in stages:
                        with ExitStack() as pes:
                            norm_mod(l, 1)
                            mixer(l, tt, pes)
                            K.barrier()
                if l < nlayers - 1:
                    K.dma("sp", xs_d[:, tt * TT:(tt + 1) * TT].rearrange("(k p) t -> p k t", p=128), xT.t[:, :, :],
                          reads=[xT], writes=[xs_trk[tt]])
                else:
                    for k in range(8):
                        K.act(hT[:, k, :], xT[:, k, :], AF.Square)
                    for k in range(8):
                        K.mm(P[7][:, :], ones128[:, :], hT[:, k, :], start=(k == 0), stop=(k == 7), last=(k == 7))
                    K.rsqrt(rstd[:, :], P[7][:, :])
                    for k in range(8):
                        K.stt(xT[:, k, :], xT[:, k, :], fgT[:, k:k + 1], rstd[:, :], ALU.mult, ALU.mult)
                    for s in range(4):
                        for k in range(8):
                            pb = P[k // 4]
                            K.op("pe", lambda e: e.transpose(out=pb.t[:, (k % 4) * 128:(k % 4 + 1) * 128],
                                                             in_=xT.t[:, k, s * 128:(s + 1) * 128],
                                                             identity=ident_f.t[:, :]),
                                 reads=[xT, ident_f], writes=[pb])
                        for hf in range(2):
                            K.cp(B.xtok[:, hf * 512:(hf + 1) * 512], P[hf][:, :], eng=("act" if hf else "dve"))
                        r0 = tt * TT + s * 128
                        K.dma("sp", out_d[r0:r0 + 128, :], B.xtok.t[:, :], reads=[B.xtok], writes=[out_trk])
        SP = K.eng["sp"]
        dq = K.dq["sp"]
        K._wait(SP, {s: c for s, c in zip(dq["sems"], dq["cnt"]) if c > 0})
    return nc


_NC = None
_NCORES = 8


def kernel(x, c, w_ada, b_ada, norm_g, ffn1_in, ffn1_out, w_in, gla_a2, gla_a_bias, gla_norm_g,
           nsa_pe_k, nsa_pe_v, nsa_w1_k, nsa_w2_k, nsa_w1_v, nsa_w2_v, nsa_gate_bias, ret_norm_g,
           w_out, ffn2_in, ffn2_out, final_norm_g):
    global _NC
    f = lambda a: np.ascontiguousarray(np.asarray(a, dtype=np.float32))
    x = f(x)
    B = x.shape[0]
    shared = {
        "w_ada": f(w_ada).reshape(L * D, 9 * D),
        "b_adaT": f(np.asarray(b_ada).reshape(L, 72, 128).transpose(2, 0, 1).reshape(128, L * 72)),
        "norm_gT": f(np.asarray(norm_g).reshape(L, 3, 8, 128).transpose(3, 0, 1, 2).reshape(128, L * 24)),
        "final_gT": f(np.asarray(final_norm_g).reshape(8, 128).T),
        "ffn1_in": f(ffn1_in).reshape(L * D, 2 * FF),
        "ffn2_in": f(ffn2_in).reshape(L * D, 2 * FF),
        "ffn1_out": f(ffn1_out).reshape(L * FF, D),
        "ffn2_out": f(ffn2_out).reshape(L * FF, D),
        "w_in_ext": f(np.asarray(w_in)[:, :, _colidx()]).reshape(L * D, NEXT),
        "a2b": f(np.concatenate([np.asarray(gla_a2), np.asarray(gla_a_bias)[:, None, :]], axis=1)).reshape(L * 17, 256),
        "gla_gT": f(np.asarray(gla_norm_g).T),
        "ret_gT": f(np.asarray(ret_norm_g).T),
        "pekT": f(np.tile(np.asarray(nsa_pe_k).transpose(2, 0, 1).reshape(64, L * 32), (2, 1))),
        "pevT": f(np.tile(np.asarray(nsa_pe_v).transpose(2, 0, 1).reshape(64, L * 32), (2, 1))),
        "w1k": f(nsa_w1_k).reshape(L * 2048, 128),
        "w1v": f(nsa_w1_v).reshape(L * 2048, 128),
        "w2k": f(nsa_w2_k).reshape(L * 128, 64),
        "w2v": f(nsa_w2_v).reshape(L * 128, 64),
        "gbias": f(np.tile(np.asarray(nsa_gate_bias).reshape(1, L * 24), (128, 1))),
        "w_out": f(w_out).reshape(L * D, D),
    }
    for k, v in _consts().items():
        shared["c_" + k] = f(v)
    if _NC is None:
        _NC = build()
    in_maps = []
    for core in range(8):
        b = core % B
        m = dict(shared)
        m["x"] = f(x[b])
        m["cT"] = f(np.asarray(c)[b].reshape(8, 128).T)
        in_maps.append(m)
    res = run_bass_kernel_spmd(_NC, in_maps[:_NCORES], core_ids=list(range(_NCORES)))
    out = np.stack([res.results[b % _NCORES]["out"] for b in range(B)], axis=0)
    return out.astype(np.float32)
```

```python
import math
from contextlib import ExitStack

import numpy as np
import concourse.bass as bass
import concourse.mybir as mybir
from concourse.bass_utils import run_bass_kernel_spmd

F32 = mybir.dt.float32
BF16 = mybir.dt.bfloat16
AF = mybir.ActivationFunctionType
ALU = mybir.AluOpType

D = 1024
T = 4096
L = 4
TT = 512
NTILE = T // TT
FF = 2816
NJ = FF // 128
EPS = 1e-6
NEG = -30000.0
NFM = 35
TMA1 = NFM * 128
TMA2 = TMA1 + 256
TMB = TMA2 + 256
NEXT = TMB + 280
FW = 2176


class V:
    def __init__(self, tl, ap):
        self.tl = tl
        self.ap = ap


class Tl:
    def __init__(self, t=None):
        self.t = t
        self.w = None
        self.r = {}

    def __getitem__(self, idx):
        return V(self, self.t[idx])


class Eng:
    def __init__(self, name, h):
        self.name = name
        self.h = h
        self.sem = None
        self.cnt = 0
        self.waited = {}
        self.pending = []


class KB:
    LIMIT = 20000

    def __init__(self, nc, es):
        self.nc = nc
        self.es = es
        self.sems = []
        self.eng = {
            "pe": Eng("pe", nc.tensor),
            "act": Eng("act", nc.scalar),
            "dve": Eng("dve", nc.vector),
            "pool": Eng("pool", nc.gpsimd),
            "sp": Eng("sp", nc.sync),
        }
        self.dq = {}
        for q in ("sp", "pool"):
            self.dq[q] = {"i": 0, "sems": [self.newsem("d%s%d" % (q, i)) for i in range(12)], "cnt": [0] * 12}
        self.nuniq = 0

    def newsem(self, name):
        s = self.es.enter_context(self.nc.semaphore(name))
        self.sems.append(s)
        return len(self.sems) - 1

    def sb(self, name, shape, dt, es=None):
        self.nuniq += 1
        t = (es or self.es).enter_context(self.nc.sbuf_tensor("%s_%d" % (name, self.nuniq), list(shape), dt))
        return Tl(t)

    def ps(self, name, shape, dt=F32):
        t = self.es.enter_context(self.nc.psum_tensor(name, list(shape), dt))
        return Tl(t)

    def _deps(self, reads, writes):
        deps = {}

        def add(ev):
            if ev is not None:
                if deps.get(ev[0], 0) < ev[1]:
                    deps[ev[0]] = ev[1]

        for t in reads:
            add(t.w)
        for t in writes:
            add(t.w)
            for s, v in t.r.items():
                add((s, v))
        return deps

    def _wait(self, E, deps):
        for s, v in deps.items():
            if E.waited.get(s, 0) < v:
                E.h.wait_ge(self.sems[s], v)
                E.waited[s] = v

    def op(self, eng, fn, reads=(), writes=(), last=True):
        E = self.eng[eng]
        reads = [x.tl if isinstance(x, V) else x for x in reads]
        writes = [x.tl if isinstance(x, V) else x for x in writes]
        if E.sem is None or (E.cnt >= self.LIMIT and not E.pending):
            E.sem = self.newsem("e%s%d" % (eng, len(self.sems)))
            E.cnt = 0
        self._wait(E, self._deps(reads, writes))
        ins = fn(E.h)
        E.pending.append((reads, writes))
        if last:
            E.cnt += 1
            ins.then_inc(self.sems[E.sem], 1)
            ev = (E.sem, E.cnt)
            for rd, wr in E.pending:
                for t in rd:
                    if t.r.get(ev[0], 0) < ev[1]:
                        t.r[ev[0]] = ev[1]
                for t in wr:
                    t.w = ev
                    t.r = {}
            E.pending = []
        return ins

    def dma(self, q, out, in_, reads=(), writes=()):
        Q = self.eng[q]
        reads = [x.tl if isinstance(x, V) else x for x in reads]
        writes = [x.tl if isinstance(x, V) else x for x in writes]
        deps = self._deps(reads, writes)
        pool = self.dq[q]
        i = pool["i"] % len(pool["sems"])
        pool["i"] += 1
        sem, cnt = pool["sems"][i], pool["cnt"][i]
        if cnt > 0 and deps.get(sem, 0) < cnt:
            deps[sem] = cnt
        self._wait(Q, deps)
        Q.h.dma_start(out=out, in_=in_).then_inc(self.sems[sem], 16)
        pool["cnt"][i] = cnt + 16
        ev = (sem, cnt + 16)
        for t in reads:
            if t.r.get(ev[0], 0) < ev[1]:
                t.r[ev[0]] = ev[1]
        for t in writes:
            t.w = ev
            t.r = {}
        return ev

    def barrier(self, names=("pe", "act", "dve", "sp", "pool")):
        for a in names:
            A = self.eng[a]
            deps = {}
            for b in names:
                B = self.eng[b]
                if B.sem is not None and B.cnt > 0 and not (b == a and b in ('sp', 'pool')):
                    deps[B.sem] = B.cnt
            self._wait(A, deps)

    def mm(self, out, lhsT, rhs, start=True, stop=True, last=True, sgc=False):
        return self.op("pe", lambda e: e.matmul(out.ap, lhsT.ap, rhs.ap, start=start, stop=stop,
                                                skip_group_check=sgc),
                       reads=[lhsT, rhs], writes=[out], last=last)

    def act(self, out, in_, func, bias=None, scale=None, extra=()):
        kw = {}
        rd = [in_] + list(extra)
        if bias is not None:
            if isinstance(bias, V):
                kw["bias"] = bias.ap
                rd.append(bias)
            else:
                kw["bias"] = bias
        if scale is not None:
            if isinstance(scale, V):
                kw["scale"] = scale.ap
                rd.append(scale)
            else:
                kw["scale"] = scale
        return self.op("act", lambda e: e.activation(out=out.ap, in_=in_.ap, func=func, **kw),
                       reads=rd, writes=[out])

    def tt(self, out, in0, in1, op, eng="dve"):
        return self.op(eng, lambda e: e.tensor_tensor(out=out.ap, in0=in0.ap, in1=in1.ap, op=op),
                       reads=[in0, in1], writes=[out])

    def ts(self, out, in0, s1, s2, op0, op1=None, eng="dve"):
        rd = [in0]
        a1 = s1
        a2 = s2
        if isinstance(s1, V):
            rd.append(s1)
            a1 = s1.ap
        if isinstance(s2, V):
            rd.append(s2)
            a2 = s2.ap
        if op1 is None:
            return self.op(eng, lambda e: e.tensor_scalar(out=out.ap, in0=in0.ap, scalar1=a1, scalar2=None, op0=op0),
                           reads=rd, writes=[out])
        return self.op(eng, lambda e: e.tensor_scalar(out=out.ap, in0=in0.ap, scalar1=a1, scalar2=a2, op0=op0, op1=op1),
                       reads=rd, writes=[out])

    def stt(self, out, in0, scalar, in1, op0, op1, eng="dve"):
        rd = [in0, in1]
        a = scalar
        if isinstance(scalar, V):
            rd.append(scalar)
            a = scalar.ap
        return self.op(eng, lambda e: e.scalar_tensor_tensor(out=out.ap, in0=in0.ap, scalar=a, in1=in1.ap, op0=op0, op1=op1),
                       reads=rd, writes=[out])

    def rsqrt(self, out, in_):
        self.act(out, in_, AF.Sqrt, bias=EPS)
        return self.op("dve", lambda e: e.reciprocal(out=out.ap, in_=out.ap), reads=[out], writes=[out])

    def sigmoid(self, out, in_):
        self.act(out, in_, AF.Exp, scale=-1.0)
        self.ts(out, out, 1.0, None, ALU.add)
        return self.op("dve", lambda e: e.reciprocal(out=out.ap, in_=out.ap), reads=[out], writes=[out])

    def cp(self, out, in_, eng="dve"):
        if eng == "act":
            return self.op(eng, lambda e: e.activation(out=out.ap, in_=in_.ap, func=AF.Copy), reads=[in_], writes=[out])
        return self.op(eng, lambda e: e.tensor_copy(out=out.ap, in_=in_.ap), reads=[in_], writes=[out])

    def memset(self, out, val, eng="dve"):
        return self.op(eng, lambda e: e.memset(out.ap, val), reads=[], writes=[out])


def _colidx():
    def rng(a, n):
        return list(range(a, a + n))

    def swp(a, n):
        o = []
        for h in range(n // 64):
            o += rng(a + 64 * h + 32, 32) + rng(a + 64 * h, 32)
        return o

    b = []
    b.append(rng(0, 128)); b.append(rng(128, 128))
    b.append(rng(256, 128)); b.append(rng(384, 128))
    b.append(rng(768, 128)); b.append(rng(896, 128))
    b.append(rng(1024, 16) + [1024] * 112)
    for p in range(4):
        b.append(rng(1040 + 128 * p, 128))
    for p in range(4):
        b.append(swp(1040 + 128 * p, 128))
    b.append(rng(1552, 128)); b.append(rng(1680, 128))
    for g in range(2):
        b.append(rng(1808 + 64 * g, 64) * 2)
    for g in range(2):
        b.append(swp(1808 + 64 * g, 64) * 2)
    for g in range(2):
        b.append(rng(2064 + 64 * g, 64) * 2)
    for g in range(2):
        b.append(swp(2064 + 64 * g, 64) * 2)
    for p in range(2):
        b.append(rng(2344 + 128 * p, 128))
    for p in range(2):
        b.append(swp(2344 + 128 * p, 128))
    for p in range(2):
        b.append(rng(2600 + 128 * p, 128))
    for p in range(2):
        b.append(swp(2600 + 128 * p, 128))
    b.append(rng(3112, 128)); b.append(rng(3240, 128))
    assert len(b) == NFM
    idx = []
    for x in b:
        assert len(x) == 128
        idx += x
    idx += rng(512, 256) + rng(2856, 256)
    idx += rng(1936, 128) + rng(2192, 128) + rng(2320, 24)
    assert len(idx) == NEXT
    return np.array(idx, dtype=np.int64)


def _consts():
    c = {}
    p = np.arange(128)
    t = np.arange(T)
    invf = (10000.0 ** (-np.arange(32, dtype=np.float64) / 32.0))
    ang = t[None, :].astype(np.float64) * invf[(p % 32)][:, None]
    cos = np.cos(ang)
    sin = np.sin(ang)
    sgn = np.where((p % 64) < 32, -1.0, 1.0)[:, None]
    c["cosk"] = cos.astype(np.float32)
    c["sink"] = (sin * sgn).astype(np.float32)
    c["cosq"] = (cos * 0.125).astype(np.float32)
    c["sinq"] = (sin * sgn * 0.125).astype(np.float32)
    lg = np.log1p(-np.exp2(-5.0 - np.arange(4, dtype=np.float64)))
    i = np.arange(128, dtype=np.float64)
    decq = np.zeros((128, 2, 128)); deck = np.zeros((128, 2, 128)); rets = np.zeros((128, 2))
    for pair in range(2):
        for half in range(2):
            h = pair * 2 + half
            dq = np.exp(lg[h] * (i + 1.0))
            dk = np.exp(-lg[h] * (i + 1.0)) * 0.125
            decq[half * 64:(half + 1) * 64, pair, :] = dq[None, :]
            deck[half * 64:(half + 1) * 64, pair, :] = dk[None, :]
            rets[half * 64:(half + 1) * 64, pair] = np.exp(lg[h] * 128.0)
    c["decq"] = decq.reshape(128, 256).astype(np.float32)
    c["deck"] = deck.reshape(128, 256).astype(np.float32)
    c["rets"] = rets.astype(np.float32)
    j = np.arange(128)[:, None]
    ii = np.arange(128)[None, :]
    c["ident"] = np.eye(128, dtype=np.float32)
    c["ident4"] = np.tile(np.eye(128, dtype=np.float32), (1, 4))
    c["negc4"] = np.tile(np.where(j > ii, NEG, 0.0).astype(np.float32), (1, 4))
    c["negw4"] = np.tile(np.where(j <= ii, NEG, 0.0).astype(np.float32), (1, 4))
    c["caus4"] = np.tile((j <= ii).astype(np.float32), (1, 4))
    c["tri"] = np.where(j <= ii, -1.0 / 16.0, 0.0).astype(np.float32)
    y = np.arange(FW)[None, :]
    c["fneg"] = np.where(16 * j + 15 <= y, 0.0, NEG).astype(np.float32)
    x = np.arange(126)[None, :]
    m = x - 62
    cur = (np.arange(128)[:, None] >= 64).astype(np.int64)
    c["mulu"] = (m < cur - 1).astype(np.float32)
    addu = np.zeros((128, 126), dtype=np.float32)
    addu[np.broadcast_to(m == cur - 1, addu.shape)] = 1.2e4
    addu[np.broadcast_to(m == cur, addu.shape)] = 1.1e4
    addu[np.broadcast_to(m > cur, addu.shape)] = -1.0
    c["addu"] = addu
    ova = np.zeros((128, 2, 65), dtype=np.float32)
    for slot in range(1, 256):
        cc, sl = divmod(slot, 128)
        ova[sl, cc, 0] = 1.0
        for s in range(64):
            if 4 * s <= slot <= 4 * s + 4:
                ova[sl, cc, 1 + s] = 1.0
    c["ovaug"] = ova.reshape(128, 130)
    return c


_CONST_SHAPES = None


def _const_shapes():
    global _CONST_SHAPES
    if _CONST_SHAPES is None:
        _CONST_SHAPES = {k: v.shape for k, v in _consts().items()}
    return _CONST_SHAPES


def build(nlayers=L, ntiles=NTILE, stages=('ffn1', 'mixer', 'pall', 'lin', 'nsa', 'wout', 'ffn2')):
    nc = bass.Bass("TRN2", target_bir_lowering=False)
    dr = {}

    def din(name, shape):
        dr[name] = nc.dram_tensor(name, list(shape), F32, kind="ExternalInput").ap()
        return dr[name]

    x_d = din("x", [T, D])
    cT_d = din("cT", [128, 8])
    wada_d = din("w_ada", [L * D, 9 * D])
    bada_d = din("b_adaT", [128, L * 72])
    ng_d = din("norm_gT", [128, L * 24])
    fg_d = din("final_gT", [128, 8])
    fin_d = [din("ffn1_in", [L * D, 2 * FF]), din("ffn2_in", [L * D, 2 * FF])]
    fout_d = [din("ffn1_out", [L * FF, D]), din("ffn2_out", [L * FF, D])]
    winx_d = din("w_in_ext", [L * D, NEXT])
    a2b_d = din("a2b", [L * 17, 256])
    glag_d = din("gla_gT", [64, L])
    retg_d = din("ret_gT", [64, L])
    pek_d = din("pekT", [128, L * 32])
    pev_d = din("pevT", [128, L * 32])
    w1k_d = din("w1k", [L * 2048, 128])
    w1v_d = din("w1v", [L * 2048, 128])
    w2k_d = din("w2k", [L * 128, 64])
    w2v_d = din("w2v", [L * 128, 64])
    gb_d = din("gbias", [128, L * 24])
    wout_d = din("w_out", [L * D, D])
    cd = {k: din("c_" + k, list(s)) for k, s in _const_shapes().items()}
    out_d = nc.dram_tensor("out", [T, D], F32, kind="ExternalOutput").ap()
    xs_d = nc.dram_tensor("xscr", [D, T], F32, kind="Internal").ap()

    with ExitStack() as es:
        K = KB(nc, es)
        P = [K.ps("ps%d" % i, [128, 512]) for i in range(8)]
        xs_trk = [Tl() for _ in range(NTILE)]
        out_trk = Tl()

        def cload(name, dt, q=None):
            shp = _const_shapes()[name]
            t = K.sb("c_" + name, shp, dt)
            K.dma("pool" if dt == BF16 else "sp", t.t[:], cd[name][:, :], writes=[t])
            return t

        ident_bf = cload("ident", BF16)
        ident_f = cload("ident", F32)
        ident4 = cload("ident4", BF16)
        negc4 = cload("negc4", BF16)
        negw4 = cload("negw4", BF16)
        caus4 = cload("caus4", BF16)
        tri = cload("tri", F32)
        fneg = cload("fneg", BF16)
        mulu = cload("mulu", F32)
        addu = cload("addu", F32)
        rets = cload("rets", F32)
        decq = cload("decq", F32)
        deck = cload("deck", F32)
        ones128 = K.sb("ones128", [128, 128], BF16)
        K.memset(ones128[:, :], 1.0 / 1024.0)
        ones64 = K.sb("ones64", [64, 64], BF16)
        K.memset(ones64[:, :], 1.0 / 64.0)

        xT = K.sb("xT", [128, 8, TT], F32)
        hT = K.sb("hT", [128, 8, TT], BF16)
        rstd = K.sb("rstd", [128, TT], F32)
        tmp = [K.sb("tmp%d" % i, [128, TT], F32) for i in range(3)]
        tmpi = [0]

        def ntmp():
            tmpi[0] += 1
            return tmp[tmpi[0] % 3]

        modall = K.sb("modall", [128, L * 72], F32)
        Amod = K.sb("Amod", [128, L * 24], F32)
        Gmod = K.sb("Gmod", [128, L * 24], F32)
        ngT = K.sb("ngT", [128, L * 24], F32)
        K.dma("sp", ngT.t[:], ng_d[:, :], writes=[ngT])
        fgT = K.sb("fgT", [128, 8], F32)
        K.dma("sp", fgT.t[:], fg_d[:, :], writes=[fgT])
        glag = K.sb("glag", [64, L], F32)
        K.dma("sp", glag.t[:], glag_d[:, :], writes=[glag])
        retg = K.sb("retg", [64, L], F32)
        K.dma("sp", retg.t[:], retg_d[:, :], writes=[retg])
        gbrow = K.sb("gbrow", [1, L * 24], BF16)
        K.dma("pool", gbrow.t[:], gb_d[0:1, :], writes=[gbrow])
        onesrow = K.sb("onesrow", [1, 128], BF16)
        K.memset(onesrow[:, :], 1.0)

        NFB = 3
        fwi = [0]
        NOB = 4
        foi = [0]
        NWB = 4
        wii = [0]
        wti = [0]

        class NS:
            pass

        B = NS()

        def alloc_ffn_bufs(pes):
            B.fwg = [K.sb("fwg%d" % i, [128, 8, 512], BF16, es=pes) for i in range(2)]
            B.fwu = [K.sb("fwu%d" % i, [128, 8, 512], BF16, es=pes) for i in range(2)]
            B.fob = [K.sb("fob%d" % i, [128, 1024], BF16, es=pes) for i in range(3)]
            B.su = [K.sb("su%d" % i, [128, 8, 512], F32, es=pes) for i in range(2)]
            B.so = [K.sb("so%d" % i, [128, 1024], F32, es=pes) for i in range(2)]

        a2b = K.sb("a2b", [17, 256], BF16)
        w2k = K.sb("w2k", [128, 128], BF16)
        w2v = K.sb("w2v", [128, 64], BF16)
        pek = K.sb("pek", [128, 32], BF16)
        pev = K.sb("pev", [128, 32], BF16)
        pebk = K.sb("pebk", [128, 1], F32)
        pebv = K.sb("pebv", [128, 1], F32)

        ksc = K.sb("ksc", [128, 2, T], BF16)
        kwc = K.sb("kwc", [128, 2, 1024], BF16)
        vsc = K.sb("vsc", [128, 32, 2, 80], BF16)
        vwc = K.sb("vwc", [128, 8, 2, 80], BF16)
        kcc = K.sb("kcc", [128, 2, 256], BF16)
        vca = K.sb("vca", [128, 2, 2, 136], BF16)
        kcb = K.sb("kcb", [128, 16 + TT], BF16)
        vcb = K.sb("vcb", [128, 16 + TT], BF16)
        hvp = K.sb("hvp", [128, 2, 128], BF16)
        K.memset(ksc[:, :, :], 0.0)
        K.memset(kwc[:, :, :], 0.0)
        K.memset(vsc[:, :, :, :], 1.0)
        K.memset(vwc[:, :, :, :], 1.0)
        K.memset(kcc[:, :, :], 0.0)
        K.memset(vca[:, :, :, :], 0.0)
        K.memset(kcb[:, :], 0.0)
        K.memset(vcb[:, :], 0.0)
        K.memset(hvp[:, :, :], 0.0)
        for g in range(2):
            for cc in range(2):
                K.dma("pool", vca.t[:, g, cc, 64:129], cd["ovaug"][:, cc * 65:(cc + 1) * 65], writes=[vca])

        S_f = {"g": K.sb("Sg", [128, 2, 128], F32), "r": K.sb("Sr", [128, 2, 128], F32)}
        S_b = {"g": K.sb("Sgb", [128, 2, 128], BF16), "r": K.sb("Srb", [128, 2, 128], BF16)}

        cact = K.sb("cact", [128, 8], BF16)
        ctmp = K.sb("ctmp", [128, 8], F32)
        K.dma("sp", ctmp.t[:], cT_d[:, :], writes=[ctmp])
        K.act(cact[:, :], ctmp[:, :], AF.Silu)
        badaT = K.sb("badaT", [128, L * 72], F32)
        K.dma("sp", badaT.t[:], bada_d[:, :], writes=[badaT])
        PM = P[7]
        pes0 = ExitStack()
        alloc_ffn_bufs(pes0)
        for l in range(nlayers):
            for cg in range(18):
                buf = (B.fwg + B.fwu)[fwi[0] % 4]
                fwi[0] += 1
                K.dma("pool", buf.t[:, :, :],
                      wada_d[l * D:(l + 1) * D, cg * 512:(cg + 1) * 512].rearrange("(k p) c -> p k c", p=128),
                      writes=[buf])
                for jj in range(4):
                    col = (l * 72 + cg * 4 + jj) % 512
                    for k in range(8):
                        K.mm(PM[:, col:col + 1], buf[:, k, jj * 128:(jj + 1) * 128], cact[:, k:k + 1],
                             start=(k == 0), stop=(k == 7), last=(k == 7))
            K.tt(modall[:, l * 72:(l + 1) * 72], PM[:, (l * 72) % 512:(l * 72) % 512 + 72],
                 badaT[:, l * 72:(l + 1) * 72], ALU.add)
            for i in range(3):
                sc = modall[:, l * 72 + (3 * i + 1) * 8: l * 72 + (3 * i + 1) * 8 + 8]
                gt = modall[:, l * 72 + (3 * i + 2) * 8: l * 72 + (3 * i + 2) * 8 + 8]
                K.stt(Amod[:, (l * 3 + i) * 8:(l * 3 + i) * 8 + 8], sc, 1.0,
                      ngT[:, (l * 3 + i) * 8:(l * 3 + i) * 8 + 8], ALU.add, ALU.mult)
                K.ts(Gmod[:, (l * 3 + i) * 8:(l * 3 + i) * 8 + 8], gt, 1.0 if i == 1 else 0.5, None, ALU.mult)

        K.barrier()
        pes0.close()

        def Bmod(l, i, k):
            c0 = l * 72 + (3 * i) * 8 + k
            return modall[:, c0:c0 + 1]

        def norm_mod(l, i):
            for k in range(8):
                K.act(hT[:, k, :], xT[:, k, :], AF.Square)
            for k in range(8):
                K.mm(P[7][:, :], ones128[:, :], hT[:, k, :], start=(k == 0), stop=(k == 7), last=(k == 7))
            K.rsqrt(rstd[:, :], P[7][:, :])
            for k in range(8):
                t1 = ntmp()
                K.tt(t1[:, :], xT[:, k, :], rstd[:, :], ALU.mult)
                c0 = (l * 3 + i) * 8 + k
                K.act(hT[:, k, :], t1[:, :], AF.Identity, bias=Bmod(l, i, k), scale=Amod[:, c0:c0 + 1])

        def ffn(l, w, aT):
            i = 0 if w == 0 else 2
            norm_mod(l, i)
            win = fin_d[w]
            wout = fout_d[w]
            for jg in range(6):
                j0 = jg * 4
                nj = min(4, NJ - j0)
                n = nj * 128
                gb_, ub_ = B.fwg[jg % 2], B.fwu[jg % 2]
                K.dma("pool", gb_.t[:, :, 0:n],
                      win[l * D:(l + 1) * D, j0 * 128:j0 * 128 + n].rearrange("(k p) c -> p k c", p=128), writes=[gb_])
                su = B.su[jg % 2]
                K.dma("sp", su.t[:, :, 0:n],
                      win[l * D:(l + 1) * D, FF + j0 * 128:FF + j0 * 128 + n].rearrange("(k p) c -> p k c", p=128),
                      writes=[su])
                K.cp(ub_[:, :, 0:n], su[:, :, 0:n])
                for jj in range(nj):
                    j = j0 + jj
                    pg = P[(j % 2) * 2]
                    pu = P[(j % 2) * 2 + 1]
                    for k in range(8):
                        K.mm(pg[:, :], gb_[:, k, jj * 128:(jj + 1) * 128], hT[:, k, :], start=(k == 0), stop=(k == 7),
                             last=(k == 7))
                    for k in range(8):
                        K.mm(pu[:, :], ub_[:, k, jj * 128:(jj + 1) * 128], hT[:, k, :], start=(k == 0), stop=(k == 7),
                             last=(k == 7))
                    t1 = ntmp()
                    K.act(t1[:, :], pg[:, :], AF.Silu)
                    K.tt(aT[:, j, :], t1[:, :], pu[:, :], ALU.mult)
            for j in range(NJ):
                ob = B.fob[foi[0] % 3]
                foi[0] += 1
                if j % 2 == 0:
                    K.dma("pool", ob.t[:, :], wout[l * FF + j * 128: l * FF + (j + 1) * 128, :], writes=[ob])
                else:
                    so = B.so[(j // 2) % 2]
                    K.dma("sp", so.t[:, :], wout[l * FF + j * 128: l * FF + (j + 1) * 128, :], writes=[so])
                    K.cp(ob[:, :], so[:, :], eng="act")
                for m in range(8):
                    K.mm(P[m][:, :], ob[:, m * 128:(m + 1) * 128], aT[:, j, :], start=(j == 0), stop=(j == NJ - 1))
            for k in range(8):
                c0 = (l * 3 + i) * 8 + k
                K.stt(xT[:, k, :], P[k][:, :], Gmod[:, c0:c0 + 1], xT[:, k, :], ALU.mult, ALU.add)

        def load_w1(l, pl):
            B.w1k = K.sb("w1k", [128, 32, 128], BF16, es=pl)
            B.w1v = K.sb("w1v", [128, 32, 128], BF16, es=pl)
            for half in range(2):
                K.dma("pool", B.w1k.t[half * 64:(half + 1) * 64, :, :],
                      w1k_d[l * 2048:(l + 1) * 2048, :].rearrange("(l d) h -> d l h", d=64), writes=[B.w1k])
                K.dma("pool", B.w1v.t[half * 64:(half + 1) * 64, :, :],
                      w1v_d[l * 2048:(l + 1) * 2048, :].rearrange("(l d) h -> d l h", d=64), writes=[B.w1v])

        def layer_setup(l):
            pl = ExitStack()
            load_w1(l, pl)
            K.dma("pool", a2b.t[:, :], a2b_d[l * 17:(l + 1) * 17, :], writes=[a2b])
            for half in range(2):
                K.dma("pool", w2k.t[:, half * 64:(half + 1) * 64], w2k_d[l * 128:(l + 1) * 128, :], writes=[w2k])
            K.dma("pool", w2v.t[:, :], w2v_d[l * 128:(l + 1) * 128, :], writes=[w2v])
            K.dma("pool", pek.t[:, :], pek_d[:, l * 32:(l + 1) * 32], writes=[pek])
            K.dma("pool", pev.t[:, :], pev_d[:, l * 32:(l + 1) * 32], writes=[pev])
            for (w1, pe, peb) in ((B.w1k, pek, pebk), (B.w1v, pev, pebv)):
                for ll in range(32):
                    K.mm(P[6][:, 0:1], w1[0:64, ll, :], pe[0:64, ll:ll + 1], start=(ll == 0), stop=(ll == 31),
                         last=(ll == 31))
                K.cp(peb[:, :], P[6][:, 0:1])
            for kind in ("g", "r"):
                K.memset(S_f[kind][:, :, :], 0.0)
                K.memset(S_b[kind][:, :, :], 0.0)
            K.memset(hvp[:, :, :], 0.0)
            K.barrier()
            pl.close()

        def mixer(l, tt, pes):
            cur = [pes]

            def sbl(name, shape, dt):
                return K.sb(name, shape, dt, es=cur[0])

            gqT = sbl("gqT", [128, 2, TT], BF16)
            gkT = sbl("gkT", [128, 2, TT], BF16)
            ggT = sbl("ggT", [64, 4, TT], BF16)
            glrT = sbl("glrT", [17, TT], BF16)
            nqr = sbl("nqr", [128, 4, TT], BF16)
            nqo = sbl("nqo", [128, 4, TT], BF16)
            rqT = sbl("rqT", [128, 2, TT], BF16)
            rkT = sbl("rkT", [128, 2, TT], BF16)
            rgT = sbl("rgT", [64, 4, TT], BF16)
            gv = sbl("gv", [128, 4, 256], BF16)
            rv = sbl("rv", [128, 4, 256], BF16)
            sig = sbl("sig", [128, 4, 24], F32)
            ogT = sbl("ogT", [64, 4, TT], BF16)
            orT = sbl("orT", [64, 4, TT], BF16)
            onT = sbl("onT", [128, 4, TT], BF16)
            pa = ExitStack()
            cur[0] = pa
            B.wib = [sbl("wib%d" % i, [128, 8, 512], BF16) for i in range(3)]
            B.swi = sbl("swi", [128, 8, 512], F32)
            B.wtb = [sbl("wtb%d" % i, [128, 8, 280], BF16) for i in range(2)]
            rot = {}
            for nm in ("cosq", "sinq", "cosk", "sink"):
                rot[nm] = sbl(nm, [128, TT], F32)
                K.dma("sp", rot[nm].t[:, :], cd[nm][:, tt * TT:(tt + 1) * TT], writes=[rot[nm]])
            K.memset(glrT[:, :], 1.0)

            pcur = [0]

            def fm(b, M=128, col0=0, cache={}):
                gid = b // 4
                if "g" not in cache:
                    cache["g"] = {}
                    cache["lru"] = []
                    cache["free"] = list(B.wib)
                if gid not in cache["g"]:
                    if cache["free"]:
                        buf = cache["free"].pop(0)
                    else:
                        old = cache["lru"].pop(0)
                        buf = cache["g"].pop(old)
                    nb = min(4, NFM - gid * 4)
                    src = winx_d[l * D:(l + 1) * D, gid * 512:gid * 512 + nb * 128].rearrange("(k p) c -> p k c", p=128)
                    if gid % 2 == 0:
                        K.dma("pool", buf.t[:, :, 0:nb * 128], src, writes=[buf])
                    else:
                        K.dma("sp", B.swi.t[:, :, 0:nb * 128], src, writes=[B.swi])
                        K.cp(buf[:, :, 0:nb * 128], B.swi[:, :, 0:nb * 128], eng="act")
                    cache["g"][gid] = buf
                if gid in cache["lru"]:
                    cache["lru"].remove(gid)
                cache["lru"].append(gid)
                buf = cache["g"][gid]
                o = (b % 4) * 128 + col0
                ps = P[pcur[0] % 4]
                pcur[0] += 1
                for k in range(8):
                    K.mm(ps[0:M, :], buf[:, k, o:o + M], hT[:, k, :], start=(k == 0), stop=(k == 7),
                         last=(k == 7))
                return ps

            fmc = {}
            G = lambda nm, n: (n if (nm in stages or 'pall' in stages) else 0)
            for p in range(G('pA', 2)):
                K.cp(gqT[:, p, :], fm(0 + p, cache=fmc)[:, :], eng="act")
                K.cp(gkT[:, p, :], fm(2 + p, cache=fmc)[:, :], eng="act")
            for p in range(G('pB', 2)):
                for hh in range(2):
                    ps = fm(4 + p, 64, hh * 64, cache=fmc)
                    K.act(ggT[:, 2 * p + hh, :], ps[0:64, :], AF.Silu)
            for _ in range(G('pC', 1)):
                ps = fm(6, 16, 0, cache=fmc)
                K.cp(glrT[0:16, :], ps[0:16, :], eng="act")
            for p in range(G('pD', 4)):
                K.act(nqr[:, p, :], fm(7 + p, cache=fmc)[:, :], AF.Copy, scale=0.125)

            def rotj(braw, bsw, cosn, sinn, dest, dec=None):
                t1 = ntmp()
                K.tt(t1[:, :], fm(braw, cache=fmc)[:, :], rot[cosn][:, :], ALU.mult)
                t2 = ntmp()
                K.tt(t2[:, :], fm(bsw, cache=fmc)[:, :], rot[sinn][:, :], ALU.mult)
                if dec is None:
                    K.tt(dest, t1[:, :], t2[:, :], ALU.add)
                else:
                    K.tt(t1[:, :], t1[:, :], t2[:, :], ALU.add)
                    dtile, dp, dtab = dec
                    for s4 in range(4):
                        K.tt(dtile[:, dp, s4 * 128:(s4 + 1) * 128], t1[:, s4 * 128:(s4 + 1) * 128],
                             dtab[:, dp * 128:(dp + 1) * 128], ALU.mult)

            for p in range(G('pE', 4)):
                rotj(7 + p, 11 + p, "cosq", "sinq", nqo[:, p, :])
            for _ in range(G('pF', 1)):
                K.cp(kcb[:, 0:16], kcb[:, TT:TT + 16])
                K.cp(vcb[:, 0:16], vcb[:, TT:TT + 16])
                K.cp(kcb[:, 16:16 + TT], fm(15, cache=fmc)[:, :], eng="act")
                K.cp(vcb[:, 16:16 + TT], fm(16, cache=fmc)[:, :], eng="act")
            for g in range(G('pG', 2)):
                rotj(17 + g, 19 + g, "cosk", "sink", ksc[:, g, tt * TT:(tt + 1) * TT])
                w0 = (tt % 2) * TT
                rotj(21 + g, 23 + g, "cosk", "sink", kwc[:, g, w0:w0 + TT])
            for p in range(G('pH', 2)):
                rotj(25 + p, 27 + p, "cosk", "sink", None, dec=(rqT, p, decq))
                rotj(29 + p, 31 + p, "cosk", "sink", None, dec=(rkT, p, deck))

            for p in range(G('pB', 2)):
                for hh in range(2):
                    ps = fm(33 + p, 64, hh * 64, cache=fmc)
                    K.act(rgT[:, 2 * p + hh, :], ps[0:64, :], AF.Silu)
            for (c0, n, kindtm) in ((TMA1, 256, "gv"), (TMA2, 256, "rv"), (TMB, 280, "b"))[:max(G('pT', 3), 2 if 'pT2' in stages else 0)]:
                buf = B.wtb[wti[0] % 2]
                wti[0] += 1
                K.dma("pool", buf.t[:, :, 0:n],
                      winx_d[l * D:(l + 1) * D, c0:c0 + n].rearrange("(k p) c -> p k c", p=128), writes=[buf])
                for s in range(4):
                    ps = P[pcur[0] % 4]
                    pcur[0] += 1
                    for k in range(8):
                        K.mm(ps[:, 0:n], hT[:, k, s * 128:(s + 1) * 128], buf[:, k, 0:n], start=(k == 0),
                             stop=(k == 7), last=(k == 7 and kindtm != "b"), sgc=(kindtm == "b"))
                    if kindtm == "b":
                        K.mm(ps[:, 256:280], onesrow[0:1, :], gbrow[0:1, l * 24:(l + 1) * 24], start=False, stop=True,
                             sgc=True)
                    if kindtm == "gv":
                        K.cp(gv[:, s, :], ps[:, 0:256], eng="act")
                    elif kindtm == "rv":
                        K.cp(rv[:, s, :], ps[:, 0:256], eng="act")
                    else:
                        ca = tt * 4 + s
                        for g in range(0 if 'nob2' in stages else 2):
                            K.cp(vsc[:, ca, g, 0:64], ps[:, g * 64:(g + 1) * 64], eng="act")
                            K.cp(vwc[:, ca % 8, g, 0:64], ps[:, 128 + g * 64:128 + (g + 1) * 64], eng="act")
                        if 'nob3' not in stages:
                            K.sigmoid(sig[:, s, :], ps[:, 256:280])

            K.barrier()
            pa.close()
            pb_ = ExitStack()
            cur[0] = pb_
            lt = {}
            for nm, shp, dt in (("L1", [128, 256], F32), ("eb", [128, 256], F32), ("enb", [128, 256], F32),
                                ("qd", [128, 2, 128], BF16), ("kd", [128, 2, 128], BF16),
                                ("am", [128, 512], BF16), ("ktok", [128, 256], BF16), ("tS", [128, 128], F32),
                                ("sq", [64, 512], BF16), ("osb", [64, 512], F32), ("obf", [64, 512], BF16),
                                ("rs", [64, 512], F32), ("xc", [64, 512], F32)):
                lt[nm] = sbl("lt_" + nm, shp, dt)

            def linattn(kind, s):
                PA, PK, PO, PV, PN, PB = P[0], P[1], P[2], P[3], P[4], P[5]
                cs = slice(s * 128, (s + 1) * 128)
                if kind == "g":
                    K.mm(PB[:, 0:256], glrT[0:17, cs], a2b[0:17, :])
                    K.act(lt["L1"][:, :], PB[:, 0:256], AF.Exp, scale=-1.0)
                    K.act(lt["L1"][:, :], lt["L1"][:, :], AF.Ln, bias=1.0)
                    for p in range(2):
                        K.mm(PB[:, 256 + p * 128:256 + (p + 1) * 128], lt["L1"][:, p * 128:(p + 1) * 128], tri[:, :])
                    K.act(lt["eb"][:, :], PB[:, 256:512], AF.Exp)
                    K.act(lt["enb"][:, :], PB[:, 256:512], AF.Exp, scale=-1.0, bias=math.log(0.125))
                    for p in range(2):
                        K.tt(lt["qd"][:, p, :], gqT[:, p, cs], lt["eb"][:, p * 128:(p + 1) * 128], ALU.mult)
                        K.tt(lt["kd"][:, p, :], gkT[:, p, cs], lt["enb"][:, p * 128:(p + 1) * 128], ALU.mult)
                    qd = lambda p, a, b: lt["qd"][a:b, p, :]
                    kd = lambda p, a, b: lt["kd"][a:b, p, :]
                    dec = lambda p: lt["eb"][:, p * 128 + 127:p * 128 + 128]
                    vt = gv
                    gate = ggT
                    dst = ogT
                    gn = glag
                else:
                    qd = lambda p, a, b: rqT[a:b, p, cs]
                    kd = lambda p, a, b: rkT[a:b, p, cs]
                    dec = lambda p: rets[:, p:p + 1]
                    vt = rv
                    gate = rgT
                    dst = orT
                    gn = retg
                Sf, Sb = S_f[kind], S_b[kind]
                for h in range(4):
                    p, hb = h // 2, (h % 2) * 64
                    K.mm(PA[:, h * 128:(h + 1) * 128], kd(p, hb, hb + 64), qd(p, hb, hb + 64))
                K.tt(lt["am"][:, :], PA[:, :], caus4[:, :], ALU.mult)
                for p in range(2):
                    K.mm(PK[:, p * 128:(p + 1) * 128], kd(p, 0, 128), ident_bf[:, :])
                K.cp(lt["ktok"][:, :], PK[:, 0:256], eng="act")
                for h in range(4):
                    p, hb = h // 2, (h % 2) * 64
                    K.mm(PO[0:64, h * 128:(h + 1) * 128], vt[:, s, h * 64:(h + 1) * 64],
                         lt["am"][:, h * 128:(h + 1) * 128], start=True, stop=False, last=False)
                    K.mm(PO[0:64, h * 128:(h + 1) * 128], Sb[hb:hb + 64, p, hb:hb + 64], qd(p, hb, hb + 64),
                         start=False, stop=True)
                for p in range(2):
                    K.mm(PV[:, p * 128:(p + 1) * 128], lt["ktok"][:, p * 128:(p + 1) * 128],
                         vt[:, s, p * 128:(p + 1) * 128])
                for p in range(2):
                    K.ts(lt["tS"][:, :], PV[:, p * 128:(p + 1) * 128], dec(p), None, ALU.mult)
                    K.stt(Sf[:, p, :], Sf[:, p, :], dec(p), lt["tS"][:, :], ALU.mult, ALU.add)
                    K.cp(Sb[:, p, :], Sf[:, p, :], eng="act")
                if kind == "g":
                    K.act(lt["sq"][:, :], PO[0:64, :], AF.Square)
                    K.mm(PN[0:64, :], ones64[:, :], lt["sq"][:, :])
                    K.rsqrt(lt["rs"][:, :], PN[0:64, :])
                    K.tt(lt["xc"][:, :], PO[0:64, :], lt["rs"][:, :], ALU.mult)
                else:
                    K.cp(lt["osb"][:, :], PO[0:64, :], eng="act")
                    K.cp(lt["obf"][:, :], PO[0:64, :], eng="act")
                    K.mm(PN[0:64, :], ones64[:, :], lt["obf"][:, :])
                    K.tt(lt["xc"][:, :], lt["osb"][:, :], PN[0:64, :], ALU.subtract)
                    K.act(lt["sq"][:, :], lt["xc"][:, :], AF.Square)
                    K.mm(PN[0:64, :], ones64[:, :], lt["sq"][:, :])
                    K.rsqrt(lt["rs"][:, :], PN[0:64, :])
                    K.tt(lt["xc"][:, :], lt["xc"][:, :], lt["rs"][:, :], ALU.mult)
                for h in range(4):
                    K.stt(dst[:, h, cs], lt["xc"][:, h * 128:(h + 1) * 128], gn[:, l:l + 1], gate[:, h, cs],
                          ALU.mult, ALU.mult)

            for s in range(4 if 'lin' in stages else 0):
                linattn("g", s)
                linattn("r", s)

            K.barrier()
            pb_.close()
            pc_ = ExitStack()
            cur[0] = pc_
            load_w1(l, pc_)
            hk = sbl("hk", [128, 32], F32)
            hs = sbl("hs", [128, 32], F32)
            hkb = sbl("hkb", [128, 32], BF16)
            m0 = 1 if tt == 0 else 0
            nm_ = 32 - m0
            cchunk = (32 * tt) // 128
            soff = (32 * tt) % 128
            for g in range(2 if 'nsa' in stages else 0):
                gb = g * 64
                for (w1, buf, peb, isk) in ((B.w1k, kcb, pebk, True), (B.w1v, vcb, pebv, False)):
                    for ll in range(32):
                        K.mm(P[6][:, 0:nm_], w1[gb:gb + 64, ll, :],
                             V(buf, buf.t[gb:gb + 64, 16 * m0 + ll: 16 * m0 + ll + 16 * (nm_ - 1) + 1: 16]),
                             start=(ll == 0), stop=(ll == 31), last=(ll == 31))
                    K.act(hk[:, 0:nm_], P[6][:, 0:nm_], AF.Identity, bias=peb[:, 0:1])
                    K.sigmoid(hs[:, 0:nm_], hk[:, 0:nm_])
                    if isk:
                        K.tt(hkb[:, 0:nm_], hk[:, 0:nm_], hs[:, 0:nm_], ALU.mult)
                        K.mm(P[6][:, 64:64 + nm_], w2k[:, :], hkb[:, 0:nm_])
                        K.cp(kcc[:, g, 32 * tt + m0: 32 * tt + 32], P[6][:, 64:64 + nm_], eng="act")
                    else:
                        K.tt(hvp[:, g, soff + m0: soff + 32], hk[:, 0:nm_], hs[:, 0:nm_], ALU.mult)
                        K.mm(P[6][:, 128:192], hvp[:, g, :], w2v[:, :])
                        K.cp(vca[:, g, cchunk, 0:64], P[6][:, 128:192], eng="act")
            if soff == 96:
                K.memset(hvp[:, :, :], 0.0)

            nt = {}
            for nm, shp, dt in (("e0", [128, 512], BF16), ("e1", [128, 512], BF16), ("e2", [128, 512], BF16),
                                ("zt", [128, 12], F32), ("cf", [128, 12], F32), ("imp", [128, 64], F32),
                                ("imp2", [128, 64], F32), ("w1", [128, 64], F32), ("w2", [128, 64], F32),
                                ("m8", [128, 8], F32), ("nsb", [128, 64], BF16), ("nse", [128, 64, 64], BF16),
                                ("ont", [128, 512], BF16)):
                nt[nm] = sbl("nt_" + nm, shp, dt)
            ei = [0]

            def branch(kq, kcache_fn, vfn, chunks, acc_fn, maskfn, g, s, first_r=(0,)):
                cs = slice(s * 128, (s + 1) * 128)
                for ci, c in enumerate(chunks):
                    ps = P[ei[0] % 2]
                    ms = maskfn(c)
                    for r in range(4):
                        pr, hb = 2 * g + r // 2, (r % 2) * 64
                        K.mm(ps[:, r * 128:(r + 1) * 128], kcache_fn(c, hb), kq[hb:hb + 64, pr, cs],
                             start=(r == 0), stop=(len(ms) == 0), sgc=True)
                    for mi, (ml, mr, full) in enumerate(ms):
                        if full:
                            K.mm(ps[:, :], ml, mr, start=False, stop=(mi == len(ms) - 1), sgc=True)
                        else:
                            for r in range(4):
                                K.mm(ps[:, r * 128:(r + 1) * 128], ml, mr, start=False, stop=(mi == len(ms) - 1), sgc=True)
                    e = nt["e%d" % (ei[0] % 3)]
                    ei[0] += 1
                    K.act(e[:, :], ps[:, :], AF.Exp)
                    for r in range(4):
                        K.mm(acc_fn(r), e[:, r * 128:(r + 1) * 128], vfn(c), start=(ci == 0 and r in first_r),
                             stop=(ci == len(chunks) - 1), sgc=True)

            for s in range(4 if 'nsa' in stages else 0):
                qa = tt * 4 + s
                for g in range(2):
                    cch = [0] if qa < 16 else [0, 1]

                    def cmask(c):
                        u = qa - 16 * c
                        if u > 16:
                            return []
                        return [(ident_bf[:, :], fneg[:, 128 * u:128 * u + 128], False)]

                    branch(nqr, lambda c, hb: kcc[hb:hb + 64, g, c * 128:(c + 1) * 128],
                           lambda c: vca[:, g, c, 0:130], cch,
                           lambda r: P[2 + r // 2][:, (r % 2) * 136:(r % 2) * 136 + 130], cmask, g, s, first_r=(0, 2))
                    wch = list(range(max(0, qa - 4), qa + 1))

                    def wmask(c):
                        m = []
                        if c == qa:
                            m.append((ident_bf[:, :], negc4[:, :], True))
                        if c == qa - 4:
                            m.append((ident_bf[:, :], negw4[:, :], True))
                        return m

                    branch(nqo, lambda c, hb: kwc[hb:hb + 64, g, (c % 8) * 128:(c % 8 + 1) * 128],
                           lambda c: vwc[:, c % 8, g, 0:66], wch,
                           lambda r: P[5][:, r * 72:r * 72 + 66], wmask, g, s)
                    zt, cf = nt["zt"], nt["cf"]
                    for bk in range(2):
                        K.cp(V(zt, zt.t[:, 6 * bk:6 * bk + 6:3]), V(P[2 + bk], P[2 + bk].t[:, 64:64 + 272:136]))
                    K.ts(zt[:, 0:12:3], zt[:, 0:12:3], 1e-30, None, ALU.add)
                    K.op("dve", lambda e: e.reciprocal(out=cf.t[:, 0:12:3], in_=zt.t[:, 0:12:3]), reads=[zt], writes=[cf])
                    for r in range(4):
                        src = P[2 + r // 2][:, (r % 2) * 136 + 65:(r % 2) * 136 + 129]
                        if r == 0:
                            K.ts(nt["imp"][:, :], src, cf[:, 0:1], None, ALU.mult)
                        else:
                            K.stt(nt["imp"][:, :], src, cf[:, 3 * r:3 * r + 1], nt["imp"][:, :], ALU.mult, ALU.add)
                    x0 = 62 - 2 * qa
                    K.tt(nt["imp2"][:, :], nt["imp"][:, :], mulu[:, x0:x0 + 64], ALU.mult)
                    K.tt(nt["imp2"][:, :], nt["imp2"][:, :], addu[:, x0:x0 + 64], ALU.add)
                    K.memset(nt["imp2"][:, 0:1], 1.0e4)
                    K.op("dve", lambda e: e.max(out=nt["m8"].t[:, :], in_=nt["imp2"].t[:, :]),
                         reads=[nt["imp2"]], writes=[nt["m8"]])
                    K.op("dve", lambda e: e.match_replace(out=nt["w1"].t[:, :], in_to_replace=nt["m8"].t[:, :],
                                                          in_values=nt["imp2"].t[:, :], imm_value=-1.0e9),
                         reads=[nt["m8"], nt["imp2"]], writes=[nt["w1"]])
                    K.op("dve", lambda e: e.max(out=nt["m8"].t[:, :], in_=nt["w1"].t[:, :]),
                         reads=[nt["w1"]], writes=[nt["m8"]])
                    K.op("dve", lambda e: e.match_replace(out=nt["w2"].t[:, :], in_to_replace=nt["m8"].t[:, :],
                                                          in_values=nt["w1"].t[:, :], imm_value=-1.0e9),
                         reads=[nt["m8"], nt["w1"]], writes=[nt["w2"]])
                    K.tt(nt["w1"][:, :], nt["imp2"][:, :], nt["w2"][:, :], ALU.is_gt)
                    K.ts(nt["nsb"][:, :], nt["w1"][:, :], -1.0, -NEG, ALU.add, ALU.mult)
                    K.op("dve", lambda e: e.tensor_copy(
                        out=nt["nse"].t[:, :, :],
                        in_=nt["nsb"].t[:, :].unsqueeze(2).to_broadcast([128, 64, 64])),
                        reads=[nt["nsb"]], writes=[nt["nse"]])
                    sch = list(range(0, qa + 1))

                    def smask(c):
                        m = [(V(nt["nse"], nt["nse"].t[:, 2 * c:2 * c + 2, :]), ident4[:, :], True)]
                        if c == qa:
                            m.append((ident_bf[:, :], negc4[:, :], True))
                        return m

                    branch(nqo, lambda c, hb: ksc[hb:hb + 64, g, c * 128:(c + 1) * 128],
                           lambda c: vsc[:, c, g, 0:66], sch,
                           lambda r: P[4][:, r * 72:r * 72 + 66], smask, g, s)
                    K.cp(V(zt, zt.t[:, 1:12:3]), V(P[4], P[4].t[:, 64:288:72]))
                    K.cp(V(zt, zt.t[:, 2:12:3]), V(P[5], P[5].t[:, 64:288:72]))
                    K.ts(zt[:, :], zt[:, :], 1e-30, None, ALU.add)
                    K.op("dve", lambda e: e.reciprocal(out=cf.t[:, :], in_=zt.t[:, :]), reads=[zt], writes=[cf])
                    K.tt(cf[:, :], cf[:, :], sig[:, s, 12 * g:12 * g + 12], ALU.mult)
                    for r in range(4):
                        dst = nt["ont"][:, (4 * g + r) * 64:(4 * g + r + 1) * 64]
                        K.ts(dst, P[2 + r // 2][:, (r % 2) * 136:(r % 2) * 136 + 64], cf[:, 3 * r:3 * r + 1], None, ALU.mult)
                        K.stt(dst, P[4][:, r * 72:r * 72 + 64], cf[:, 3 * r + 1:3 * r + 2], dst, ALU.mult, ALU.add)
                        K.stt(dst, P[5][:, r * 72:r * 72 + 64], cf[:, 3 * r + 2:3 * r + 3], dst, ALU.mult, ALU.add)
                for p in range(4):
                    K.mm(P[6][:, p * 128:(p + 1) * 128], nt["ont"][:, p * 128:(p + 1) * 128], ident_bf[:, :])
                for p in range(4):
                    K.cp(onT[:, p, s * 128:(s + 1) * 128], P[6][:, p * 128:(p + 1) * 128], eng="act")

            K.barrier()
            pc_.close()
            cur[0] = pes
            B.wogr = sbl("wogr", [64, 8, 512], BF16)
            B.wons = sbl("wons", [128, 4, 512], BF16)
            for mg in range(2 if 'wout' in stages else 0):
                K.dma("pool", B.wogr.t[:, 0:4, :],
                      wout_d[l * D:l * D + 256, mg * 512:(mg + 1) * 512].rearrange("(h p) c -> p h c", p=64),
                      writes=[B.wogr])
                K.dma("pool", B.wogr.t[:, 4:8, :],
                      wout_d[l * D + 768:l * D + 1024, mg * 512:(mg + 1) * 512].rearrange("(h p) c -> p h c", p=64),
                      writes=[B.wogr])
                K.dma("pool", B.wons.t[:, :, :],
                      wout_d[l * D + 256:l * D + 768, mg * 512:(mg + 1) * 512].rearrange("(h p) c -> p h c", p=128),
                      writes=[B.wons])
                for m in range(4):
                    ps = P[m % 4]
                    ms = slice(m * 128, (m + 1) * 128)
                    for h in range(4):
                        K.mm(ps[:, :], B.wogr[0:64, h, ms], ogT[0:64, h, :], start=(h == 0), stop=False, last=False)
                    for h in range(4):
                        K.mm(ps[:, :], B.wogr[0:64, 4 + h, ms], orT[0:64, h, :], start=False, stop=False, last=False)
                    for p in range(4):
                        K.mm(ps[:, :], B.wons[:, p, ms], onT[:, p, :], start=False, stop=(p == 3), last=(p == 3))
                    k = mg * 4 + m
                    c0 = (l * 3 + 1) * 8 + k
                    K.stt(xT[:, k, :], ps[:, :], Gmod[:, c0:c0 + 1], xT[:, k, :], ALU.mult, ALU.add)

        B.xtok = K.sb("xtok", [128, D], F32)
        for l in range(nlayers):
            layer_setup(l)
            for tt in range(ntiles):
                if l == 0:
                    for s in range(4):
                        r0 = tt * TT + s * 128
                        K.dma("sp", B.xtok.t[:, :], x_d[r0:r0 + 128, :], writes=[B.xtok])
                        for k in range(8):
                            pb = P[k // 4]
                            K.op("pe", lambda e: e.transpose(out=pb.t[:, (k % 4) * 128:(k % 4 + 1) * 128],
                                                             in_=B.xtok.t[:, k * 128:(k + 1) * 128],
                                                             identity=ident_f.t[:, :]),
                                 reads=[B.xtok, ident_f], writes=[pb])
                        for k in range(8):
                            K.cp(xT[:, k, s * 128:(s + 1) * 128], P[k // 4][:, (k % 4) * 128:(k % 4 + 1) * 128],
                                 eng=("act" if k % 2 else "dve"))
                else:
                    K.dma("sp", xT.t[:, :, :],
                          xs_d[:, tt * TT:(tt + 1) * TT].rearrange("(k p) t -> p k t", p=128),
                          reads=[xs_trk[tt]], writes=[xT])
                for w in range(2):
                    if ('ffn1' if w == 0 else 'ffn2') in stages:
                        with ExitStack() as pes:
                            aT = K.sb("aT", [128, NJ, TT], BF16, es=pes)
                            alloc_ffn_bufs(pes)
                            ffn(l, w, aT)
                            K.barrier()
                    if w == 0 and 'mixer' in stages:
                        with ExitStack() as pes:
                            norm_mod(l, 1)
                            mixer(l, tt, pes)
                            K.barrier()
                if l < nlayers - 1:
                    K.dma("sp", xs_d[:, tt * TT:(tt + 1) * TT].rearrange("(k p) t -> p k t", p=128), xT.t[:, :, :],
                          reads=[xT], writes=[xs_trk[tt]])
                else:
                    for k in range(8):
                        K.act(hT[:, k, :], xT[:, k, :], AF.Square)
                    for k in range(8):
                        K.mm(P[7][:, :], ones128[:, :], hT[:, k, :], start=(k == 0), stop=(k == 7), last=(k == 7))
                    K.rsqrt(rstd[:, :], P[7][:, :])
                    for k in range(8):
                        K.stt(xT[:, k, :], xT[:, k, :], fgT[:, k:k + 1], rstd[:, :], ALU.mult, ALU.mult)
                    for s in range(4):
                        for k in range(8):
                            pb = P[k // 4]
                            K.op("pe", lambda e: e.transpose(out=pb.t[:, (k % 4) * 128:(k % 4 + 1) * 128],
                                                             in_=xT.t[:, k, s * 128:(s + 1) * 128],
                                                             identity=ident_f.t[:, :]),
                                 reads=[xT, ident_f], writes=[pb])
                        for hf in range(2):
                            K.cp(B.xtok[:, hf * 512:(hf + 1) * 512], P[hf][:, :], eng=("act" if hf else "dve"))
                        r0 = tt * TT + s * 128
                        K.dma("sp", out_d[r0:r0 + 128, :], B.xtok.t[:, :], reads=[B.xtok], writes=[out_trk])
        SP = K.eng["sp"]
        dq = K.dq["sp"]
        K._wait(SP, {s: c for s, c in zip(dq["sems"], dq["cnt"]) if c > 0})
    return nc


_NC = None
_NCORES = 8


def kernel(x, c, w_ada, b_ada, norm_g, ffn1_in, ffn1_out, w_in, gla_a2, gla_a_bias, gla_norm_g,
           nsa_pe_k, nsa_pe_v, nsa_w1_k, nsa_w2_k, nsa_w1_v, nsa_w2_v, nsa_gate_bias, ret_norm_g,
           w_out, ffn2_in, ffn2_out, final_norm_g):
    global _NC
    f = lambda a: np.ascontiguousarray(np.asarray(a, dtype=np.float32))
    x = f(x)
    B = x.shape[0]
    shared = {
        "w_ada": f(w_ada).reshape(L * D, 9 * D),
        "b_adaT": f(np.asarray(b_ada).reshape(L, 72, 128).transpose(2, 0, 1).reshape(128, L * 72)),
        "norm_gT": f(np.asarray(norm_g).reshape(L, 3, 8, 128).transpose(3, 0, 1, 2).reshape(128, L * 24)),
        "final_gT": f(np.asarray(final_norm_g).reshape(8, 128).T),
        "ffn1_in": f(ffn1_in).reshape(L * D, 2 * FF),
        "ffn2_in": f(ffn2_in).reshape(L * D, 2 * FF),
        "ffn1_out": f(ffn1_out).reshape(L * FF, D),
        "ffn2_out": f(ffn2_out).reshape(L * FF, D),
        "w_in_ext": f(np.asarray(w_in)[:, :, _colidx()]).reshape(L * D, NEXT),
        "a2b": f(np.concatenate([np.asarray(gla_a2), np.asarray(gla_a_bias)[:, None, :]], axis=1)).reshape(L * 17, 256),
        "gla_gT": f(np.asarray(gla_norm_g).T),
        "ret_gT": f(np.asarray(ret_norm_g).T),
        "pekT": f(np.tile(np.asarray(nsa_pe_k).transpose(2, 0, 1).reshape(64, L * 32), (2, 1))),
        "pevT": f(np.tile(np.asarray(nsa_pe_v).transpose(2, 0, 1).reshape(64, L * 32), (2, 1))),
        "w1k": f(nsa_w1_k).reshape(L * 2048, 128),
        "w1v": f(nsa_w1_v).reshape(L * 2048, 128),
        "w2k": f(nsa_w2_k).reshape(L * 128, 64),
        "w2v": f(nsa_w2_v).reshape(L * 128, 64),
        "gbias": f(np.tile(np.asarray(nsa_gate_bias).reshape(1, L * 24), (128, 1))),
        "w_out": f(w_out).reshape(L * D, D),
    }
    for k, v in _consts().items():
        shared["c_" + k] = f(v)
    if _NC is None:
        _NC = build()
    in_maps = []
    for core in range(8):
        b = core % B
        m = dict(shared)
        m["x"] = f(x[b])
        m["cT"] = f(np.asarray(c)[b].reshape(8, 128).T)
        in_maps.append(m)
    res = run_bass_kernel_spmd(_NC, in_maps[:_NCORES], core_ids=list(range(_NCORES)))
    out = np.stack([res.results[b % _NCORES]["out"] for b in range(B)], axis=0)
    return out.astype(np.float32)
```

```python
import math
from contextlib import ExitStack

import numpy as np
import concourse.bass as bass
import concourse.mybir as mybir
from concourse.bass_utils import run_bass_kernel_spmd

F32 = mybir.dt.float32
BF16 = mybir.dt.bfloat16
AF = mybir.ActivationFunctionType
ALU = mybir.AluOpType

D = 1024
T = 4096
L = 4
TT = 512
NTILE = T // TT
FF = 2816
NJ = FF // 128
EPS = 1e-6
NEG = -30000.0
NFM = 35
TMA1 = NFM * 128
TMA2 = TMA1 + 256
TMB = TMA2 + 256
NEXT = TMB + 280
FW = 2176


class V:
    def __init__(self, tl, ap):
        self.tl = tl
        self.ap = ap


class Tl:
    def __init__(self, t=None):
        self.t = t
        self.w = None
        self.r = {}

    def __getitem__(self, idx):
        return V(self, self.t[idx])


class Eng:
    def __init__(self, name, h):
        self.name = name
        self.h = h
        self.sem = None
        self.cnt = 0
        self.waited = {}
        self.pending = []


class KB:
    LIMIT = 20000

    def __init__(self, nc, es):
        self.nc = nc
        self.es = es
        self.sems = []
        self.eng = {
            "pe": Eng("pe", nc.tensor),
            "act": Eng("act", nc.scalar),
            "dve": Eng("dve", nc.vector),
            "pool": Eng("pool", nc.gpsimd),
            "sp": Eng("sp", nc.sync),
        }
        self.dq = {}
        for q in ("sp", "pool"):
            self.dq[q] = {"i": 0, "sems": [self.newsem("d%s%d" % (q, i)) for i in range(12)], "cnt": [0] * 12}
        self.nuniq = 0

    def newsem(self, name):
        s = self.es.enter_context(self.nc.semaphore(name))
        self.sems.append(s)
        return len(self.sems) - 1

    def sb(self, name, shape, dt, es=None):
        self.nuniq += 1
        t = (es or self.es).enter_context(self.nc.sbuf_tensor("%s_%d" % (name, self.nuniq), list(shape), dt))
        return Tl(t)

    def ps(self, name, shape, dt=F32):
        t = self.es.enter_context(self.nc.psum_tensor(name, list(shape), dt))
        return Tl(t)

    def _deps(self, reads, writes):
        deps = {}

        def add(ev):
            if ev is not None:
                if deps.get(ev[0], 0) < ev[1]:
                    deps[ev[0]] = ev[1]

        for t in reads:
            add(t.w)
        for t in writes:
            add(t.w)
            for s, v in t.r.items():
                add((s, v))
        return deps

    def _wait(self, E, deps):
        for s, v in deps.items():
            if E.waited.get(s, 0) < v:
                E.h.wait_ge(self.sems[s], v)
                E.waited[s] = v

    def op(self, eng, fn, reads=(), writes=(), last=True):
        E = self.eng[eng]
        reads = [x.tl if isinstance(x, V) else x for x in reads]
        writes = [x.tl if isinstance(x, V) else x for x in writes]
        if E.sem is None or (E.cnt >= self.LIMIT and not E.pending):
            E.sem = self.newsem("e%s%d" % (eng, len(self.sems)))
            E.cnt = 0
        self._wait(E, self._deps(reads, writes))
        ins = fn(E.h)
        E.pending.append((reads, writes))
        if last:
            E.cnt += 1
            ins.then_inc(self.sems[E.sem], 1)
            ev = (E.sem, E.cnt)
            for rd, wr in E.pending:
                for t in rd:
                    if t.r.get(ev[0], 0) < ev[1]:
                        t.r[ev[0]] = ev[1]
                for t in wr:
                    t.w = ev
                    t.r = {}
            E.pending = []
        return ins

    def dma(self, q, out, in_, reads=(), writes=()):
        Q = self.eng[q]
        reads = [x.tl if isinstance(x, V) else x for x in reads]
        writes = [x.tl if isinstance(x, V) else x for x in writes]
        deps = self._deps(reads, writes)
        pool = self.dq[q]
        i = pool["i"] % len(pool["sems"])
        pool["i"] += 1
        sem, cnt = pool["sems"][i], pool["cnt"][i]
        if cnt > 0 and deps.get(sem, 0) < cnt:
            deps[sem] = cnt
        self._wait(Q, deps)
        Q.h.dma_start(out=out, in_=in_).then_inc(self.sems[sem], 16)
        pool["cnt"][i] = cnt + 16
        ev = (sem, cnt + 16)
        for t in reads:
            if t.r.get(ev[0], 0) < ev[1]:
                t.r[ev[0]] = ev[1]
        for t in writes:
            t.w = ev
            t.r = {}
        return ev

    def barrier(self, names=("pe", "act", "dve", "sp", "pool")):
        for a in names:
            A = self.eng[a]
            deps = {}
            for b in names:
                B = self.eng[b]
                if B.sem is not None and B.cnt > 0 and not (b == a and b in ('sp', 'pool')):
                    deps[B.sem] = B.cnt
            self._wait(A, deps)

    def mm(self, out, lhsT, rhs, start=True, stop=True, last=True, sgc=False):
        return self.op("pe", lambda e: e.matmul(out.ap, lhsT.ap, rhs.ap, start=start, stop=stop,
                                                skip_group_check=sgc),
                       reads=[lhsT, rhs], writes=[out], last=last)

    def act(self, out, in_, func, bias=None, scale=None, extra=()):
        kw = {}
        rd = [in_] + list(extra)
        if bias is not None:
            if isinstance(bias, V):
                kw["bias"] = bias.ap
                rd.append(bias)
            else:
                kw["bias"] = bias
        if scale is not None:
            if isinstance(scale, V):
                kw["scale"] = scale.ap
                rd.append(scale)
            else:
                kw["scale"] = scale
        return self.op("act", lambda e: e.activation(out=out.ap, in_=in_.ap, func=func, **kw),
                       reads=rd, writes=[out])

    def tt(self, out, in0, in1, op, eng="dve"):
        return self.op(eng, lambda e: e.tensor_tensor(out=out.ap, in0=in0.ap, in1=in1.ap, op=op),
                       reads=[in0, in1], writes=[out])

    def ts(self, out, in0, s1, s2, op0, op1=None, eng="dve"):
        rd = [in0]
        a1 = s1
        a2 = s2
        if isinstance(s1, V):
            rd.append(s1)
            a1 = s1.ap
        if isinstance(s2, V):
            rd.append(s2)
            a2 = s2.ap
        if op1 is None:
            return self.op(eng, lambda e: e.tensor_scalar(out=out.ap, in0=in0.ap, scalar1=a1, scalar2=None, op0=op0),
                           reads=rd, writes=[out])
        return self.op(eng, lambda e: e.tensor_scalar(out=out.ap, in0=in0.ap, scalar1=a1, scalar2=a2, op0=op0, op1=op1),
                       reads=rd, writes=[out])

    def stt(self, out, in0, scalar, in1, op0, op1, eng="dve"):
        rd = [in0, in1]
        a = scalar
        if isinstance(scalar, V):
            rd.append(scalar)
            a = scalar.ap
        return self.op(eng, lambda e: e.scalar_tensor_tensor(out=out.ap, in0=in0.ap, scalar=a, in1=in1.ap, op0=op0, op1=op1),
                       reads=rd, writes=[out])

    def rsqrt(self, out, in_):
        self.act(out, in_, AF.Sqrt, bias=EPS)
        return self.op("dve", lambda e: e.reciprocal(out=out.ap, in_=out.ap), reads=[out], writes=[out])

    def sigmoid(self, out, in_):
        self.act(out, in_, AF.Exp, scale=-1.0)
        self.ts(out, out, 1.0, None, ALU.add)
        return self.op("dve", lambda e: e.reciprocal(out=out.ap, in_=out.ap), reads=[out], writes=[out])

    def cp(self, out, in_, eng="dve"):
        if eng == "act":
            return self.op(eng, lambda e: e.activation(out=out.ap, in_=in_.ap, func=AF.Copy), reads=[in_], writes=[out])
        return self.op(eng, lambda e: e.tensor_copy(out=out.ap, in_=in_.ap), reads=[in_], writes=[out])

    def memset(self, out, val, eng="dve"):
        return self.op(eng, lambda e: e.memset(out.ap, val), reads=[], writes=[out])


def _colidx():
    def rng(a, n):
        return list(range(a, a + n))

    def swp(a, n):
        o = []
        for h in range(n // 64):
            o += rng(a + 64 * h + 32, 32) + rng(a + 64 * h, 32)
        return o

    b = []
    b.append(rng(0, 128)); b.append(rng(128, 128))
    b.append(rng(256, 128)); b.append(rng(384, 128))
    b.append(rng(768, 128)); b.append(rng(896, 128))
    b.append(rng(1024, 16) + [1024] * 112)
    for p in range(4):
        b.append(rng(1040 + 128 * p, 128))
    for p in range(4):
        b.append(swp(1040 + 128 * p, 128))
    b.append(rng(1552, 128)); b.append(rng(1680, 128))
    for g in range(2):
        b.append(rng(1808 + 64 * g, 64) * 2)
    for g in range(2):
        b.append(swp(1808 + 64 * g, 64) * 2)
    for g in range(2):
        b.append(rng(2064 + 64 * g, 64) * 2)
    for g in range(2):
        b.append(swp(2064 + 64 * g, 64) * 2)
    for p in range(2):
        b.append(rng(2344 + 128 * p, 128))
    for p in range(2):
        b.append(swp(2344 + 128 * p, 128))
    for p in range(2):
        b.append(rng(2600 + 128 * p, 128))
    for p in range(2):
        b.append(swp(2600 + 128 * p, 128))
    b.append(rng(3112, 128)); b.append(rng(3240, 128))
    assert len(b) == NFM
    idx = []
    for x in b:
        assert len(x) == 128
        idx += x
    idx += rng(512, 256) + rng(2856, 256)
    idx += rng(1936, 128) + rng(2192, 128) + rng(2320, 24)
    assert len(idx) == NEXT
    return np.array(idx, dtype=np.int64)


def _consts():
    c = {}
    p = np.arange(128)
    t = np.arange(T)
    invf = (10000.0 ** (-np.arange(32, dtype=np.float64) / 32.0))
    ang = t[None, :].astype(np.float64) * invf[(p % 32)][:, None]
    cos = np.cos(ang)
    sin = np.sin(ang)
    sgn = np.where((p % 64) < 32, -1.0, 1.0)[:, None]
    c["cosk"] = cos.astype(np.float32)
    c["sink"] = (sin * sgn).astype(np.float32)
    c["cosq"] = (cos * 0.125).astype(np.float32)
    c["sinq"] = (sin * sgn * 0.125).astype(np.float32)
    lg = np.log1p(-np.exp2(-5.0 - np.arange(4, dtype=np.float64)))
    i = np.arange(128, dtype=np.float64)
    decq = np.zeros((128, 2, 128)); deck = np.zeros((128, 2, 128)); rets = np.zeros((128, 2))
    for pair in range(2):
        for half in range(2):
            h = pair * 2 + half
            dq = np.exp(lg[h] * (i + 1.0))
            dk = np.exp(-lg[h] * (i + 1.0)) * 0.125
            decq[half * 64:(half + 1) * 64, pair, :] = dq[None, :]
            deck[half * 64:(half + 1) * 64, pair, :] = dk[None, :]
            rets[half * 64:(half + 1) * 64, pair] = np.exp(lg[h] * 128.0)
    c["decq"] = decq.reshape(128, 256).astype(np.float32)
    c["deck"] = deck.reshape(128, 256).astype(np.float32)
    c["rets"] = rets.astype(np.float32)
    j = np.arange(128)[:, None]
    ii = np.arange(128)[None, :]
    c["ident"] = np.eye(128, dtype=np.float32)
    c["ident4"] = np.tile(np.eye(128, dtype=np.float32), (1, 4))
    c["negc4"] = np.tile(np.where(j > ii, NEG, 0.0).astype(np.float32), (1, 4))
    c["negw4"] = np.tile(np.where(j <= ii, NEG, 0.0).astype(np.float32), (1, 4))
    c["caus4"] = np.tile((j <= ii).astype(np.float32), (1, 4))
    c["tri"] = np.where(j <= ii, -1.0 / 16.0, 0.0).astype(np.float32)
    y = np.arange(FW)[None, :]
    c["fneg"] = np.where(16 * j + 15 <= y, 0.0, NEG).astype(np.float32)
    x = np.arange(126)[None, :]
    m = x - 62
    cur = (np.arange(128)[:, None] >= 64).astype(np.int64)
    c["mulu"] = (m < cur - 1).astype(np.float32)
    addu = np.zeros((128, 126), dtype=np.float32)
    addu[np.broadcast_to(m == cur - 1, addu.shape)] = 1.2e4
    addu[np.broadcast_to(m == cur, addu.shape)] = 1.1e4
    addu[np.broadcast_to(m > cur, addu.shape)] = -1.0
    c["addu"] = addu
    ova = np.zeros((128, 2, 65), dtype=np.float32)
    for slot in range(1, 256):
        cc, sl = divmod(slot, 128)
        ova[sl, cc, 0] = 1.0
        for s in range(64):
            if 4 * s <= slot <= 4 * s + 4:
                ova[sl, cc, 1 + s] = 1.0
    c["ovaug"] = ova.reshape(128, 130)
    return c


_CONST_SHAPES = None


def _const_shapes():
    global _CONST_SHAPES
    if _CONST_SHAPES is None:
        _CONST_SHAPES = {k: v.shape for k, v in _consts().items()}
    return _CONST_SHAPES


def build(nlayers=L, ntiles=NTILE, stages=('ffn1', 'mixer', 'pall', 'lin', 'nsa', 'wout', 'ffn2')):
    nc = bass.Bass("TRN2", target_bir_lowering=False)
    dr = {}

    def din(name, shape):
        dr[name] = nc.dram_tensor(name, list(shape), F32, kind="ExternalInput").ap()
        return dr[name]

    x_d = din("x", [T, D])
    cT_d = din("cT", [128, 8])
    wada_d = din("w_ada", [L * D, 9 * D])
    bada_d = din("b_adaT", [128, L * 72])
    ng_d = din("norm_gT", [128, L * 24])
    fg_d = din("final_gT", [128, 8])
    fin_d = [din("ffn1_in", [L * D, 2 * FF]), din("ffn2_in", [L * D, 2 * FF])]
    fout_d = [din("ffn1_out", [L * FF, D]), din("ffn2_out", [L * FF, D])]
    winx_d = din("w_in_ext", [L * D, NEXT])
    a2b_d = din("a2b", [L * 17, 256])
    glag_d = din("gla_gT", [64, L])
    retg_d = din("ret_gT", [64, L])
    pek_d = din("pekT", [128, L * 32])
    pev_d = din("pevT", [128, L * 32])
    w1k_d = din("w1k", [L * 2048, 128])
    w1v_d = din("w1v", [L * 2048, 128])
    w2k_d = din("w2k", [L * 128, 64])
    w2v_d = din("w2v", [L * 128, 64])
    gb_d = din("gbias", [128, L * 24])
    wout_d = din("w_out", [L * D, D])
    cd = {k: din("c_" + k, list(s)) for k, s in _const_shapes().items()}
    out_d = nc.dram_tensor("out", [T, D], F32, kind="ExternalOutput").ap()
    xs_d = nc.dram_tensor("xscr", [D, T], F32, kind="Internal").ap()

    with ExitStack() as es:
        K = KB(nc, es)
        P = [K.ps("ps%d" % i, [128, 512]) for i in range(8)]
        xs_trk = [Tl() for _ in range(NTILE)]
        out_trk = Tl()

        def cload(name, dt, q=None):
            shp = _const_shapes()[name]
            t = K.sb("c_" + name, shp, dt)
            K.dma("pool" if dt == BF16 else "sp", t.t[:], cd[name][:, :], writes=[t])
            return t

        ident_bf = cload("ident", BF16)
        ident_f = cload("ident", F32)
        ident4 = cload("ident4", BF16)
        negc4 = cload("negc4", BF16)
        negw4 = cload("negw4", BF16)
        caus4 = cload("caus4", BF16)
        tri = cload("tri", F32)
        fneg = cload("fneg", BF16)
        mulu = cload("mulu", F32)
        addu = cload("addu", F32)
        rets = cload("rets", F32)
        decq = cload("decq", F32)
        deck = cload("deck", F32)
        ones128 = K.sb("ones128", [128, 128], BF16)
        K.memset(ones128[:, :], 1.0 / 1024.0)
        ones64 = K.sb("ones64", [64, 64], BF16)
        K.memset(ones64[:, :], 1.0 / 64.0)

        xT = K.sb("xT", [128, 8, TT], F32)
        hT = K.sb("hT", [128, 8, TT], BF16)
        rstd = K.sb("rstd", [128, TT], F32)
        tmp = [K.sb("tmp%d" % i, [128, TT], F32) for i in range(3)]
        tmpi = [0]

        def ntmp():
            tmpi[0] += 1
            return tmp[tmpi[0] % 3]

        modall = K.sb("modall", [128, L * 72], F32)
        Amod = K.sb("Amod", [128, L * 24], F32)
        Gmod = K.sb("Gmod", [128, L * 24], F32)
        ngT = K.sb("ngT", [128, L * 24], F32)
        K.dma("sp", ngT.t[:], ng_d[:, :], writes=[ngT])
        fgT = K.sb("fgT", [128, 8], F32)
        K.dma("sp", fgT.t[:], fg_d[:, :], writes=[fgT])
        glag = K.sb("glag", [64, L], F32)
        K.dma("sp", glag.t[:], glag_d[:, :], writes=[glag])
        retg = K.sb("retg", [64, L], F32)
        K.dma("sp", retg.t[:], retg_d[:, :], writes=[retg])
        gbrow = K.sb("gbrow", [1, L * 24], BF16)
        K.dma("pool", gbrow.t[:], gb_d[0:1, :], writes=[gbrow])
        onesrow = K.sb("onesrow", [1, 128], BF16)
        K.memset(onesrow[:, :], 1.0)

        NFB = 3
        fwi = [0]
        NOB = 4
        foi = [0]
        NWB = 4
        wii = [0]
        wti = [0]

        class NS:
            pass

        B = NS()

        def alloc_ffn_bufs(pes):
            B.fwg = [K.sb("fwg%d" % i, [128, 8, 512], BF16, es=pes) for i in range(2)]
            B.fwu = [K.sb("fwu%d" % i, [128, 8, 512], BF16, es=pes) for i in range(2)]
            B.fob = [K.sb("fob%d" % i, [128, 1024], BF16, es=pes) for i in range(3)]
            B.su = [K.sb("su%d" % i, [128, 8, 512], F32, es=pes) for i in range(2)]
            B.so = [K.sb("so%d" % i, [128, 1024], F32, es=pes) for i in range(2)]

        a2b = K.sb("a2b", [17, 256], BF16)
        w2k = K.sb("w2k", [128, 128], BF16)
        w2v = K.sb("w2v", [128, 64], BF16)
        pek = K.sb("pek", [128, 32], BF16)
        pev = K.sb("pev", [128, 32], BF16)
        pebk = K.sb("pebk", [128, 1], F32)
        pebv = K.sb("pebv", [128, 1], F32)

        ksc = K.sb("ksc", [128, 2, T], BF16)
        kwc = K.sb("kwc", [128, 2, 1024], BF16)
        vsc = K.sb("vsc", [128, 32, 2, 80], BF16)
        vwc = K.sb("vwc", [128, 8, 2, 80], BF16)
        kcc = K.sb("kcc", [128, 2, 256], BF16)
        vca = K.sb("vca", [128, 2, 2, 136], BF16)
        kcb = K.sb("kcb", [128, 16 + TT], BF16)
        vcb = K.sb("vcb", [128, 16 + TT], BF16)
        hvp = K.sb("hvp", [128, 2, 128], BF16)
        K.memset(ksc[:, :, :], 0.0)
        K.memset(kwc[:, :, :], 0.0)
        K.memset(vsc[:, :, :, :], 1.0)
        K.memset(vwc[:, :, :, :], 1.0)
        K.memset(kcc[:, :, :], 0.0)
        K.memset(vca[:, :, :, :], 0.0)
        K.memset(kcb[:, :], 0.0)
        K.memset(vcb[:, :], 0.0)
        K.memset(hvp[:, :, :], 0.0)
        for g in range(2):
            for cc in range(2):
                K.dma("pool", vca.t[:, g, cc, 64:129], cd["ovaug"][:, cc * 65:(cc + 1) * 65], writes=[vca])

        S_f = {"g": K.sb("Sg", [128, 2, 128], F32), "r": K.sb("Sr", [128, 2, 128], F32)}
        S_b = {"g": K.sb("Sgb", [128, 2, 128], BF16), "r": K.sb("Srb", [128, 2, 128], BF16)}

        cact = K.sb("cact", [128, 8], BF16)
        ctmp = K.sb("ctmp", [128, 8], F32)
        K.dma("sp", ctmp.t[:], cT_d[:, :], writes=[ctmp])
        K.act(cact[:, :], ctmp[:, :], AF.Silu)
        badaT = K.sb("badaT", [128, L * 72], F32)
        K.dma("sp", badaT.t[:], bada_d[:, :], writes=[badaT])
        PM = P[7]
        pes0 = ExitStack()
        alloc_ffn_bufs(pes0)
        for l in range(nlayers):
            for cg in range(18):
                buf = (B.fwg + B.fwu)[fwi[0] % 4]
                fwi[0] += 1
                K.dma("pool", buf.t[:, :, :],
                      wada_d[l * D:(l + 1) * D, cg * 512:(cg + 1) * 512].rearrange("(k p) c -> p k c", p=128),
                      writes=[buf])
                for jj in range(4):
                    col = (l * 72 + cg * 4 + jj) % 512
                    for k in range(8):
                        K.mm(PM[:, col:col + 1], buf[:, k, jj * 128:(jj + 1) * 128], cact[:, k:k + 1],
                             start=(k == 0), stop=(k == 7), last=(k == 7))
            K.tt(modall[:, l * 72:(l + 1) * 72], PM[:, (l * 72) % 512:(l * 72) % 512 + 72],
                 badaT[:, l * 72:(l + 1) * 72], ALU.add)
            for i in range(3):
                sc = modall[:, l * 72 + (3 * i + 1) * 8: l * 72 + (3 * i + 1) * 8 + 8]
                gt = modall[:, l * 72 + (3 * i + 2) * 8: l * 72 + (3 * i + 2) * 8 + 8]
                K.stt(Amod[:, (l * 3 + i) * 8:(l * 3 + i) * 8 + 8], sc, 1.0,
                      ngT[:, (l * 3 + i) * 8:(l * 3 + i) * 8 + 8], ALU.add, ALU.mult)
                K.ts(Gmod[:, (l * 3 + i) * 8:(l * 3 + i) * 8 + 8], gt, 1.0 if i == 1 else 0.5, None, ALU.mult)

        K.barrier()
        pes0.close()

        def Bmod(l, i, k):
            c0 = l * 72 + (3 * i) * 8 + k
            return modall[:, c0:c0 + 1]

        def norm_mod(l, i):
            for k in range(8):
                K.act(hT[:, k, :], xT[:, k, :], AF.Square)
            for k in range(8):
                K.mm(P[7][:, :], ones128[:, :], hT[:, k, :], start=(k == 0), stop=(k == 7), last=(k == 7))
            K.rsqrt(rstd[:, :], P[7][:, :])
            for k in range(8):
                t1 = ntmp()
                K.tt(t1[:, :], xT[:, k, :], rstd[:, :], ALU.mult)
                c0 = (l * 3 + i) * 8 + k
                K.act(hT[:, k, :], t1[:, :], AF.Identity, bias=Bmod(l, i, k), scale=Amod[:, c0:c0 + 1])

        def ffn(l, w, aT):
            i = 0 if w == 0 else 2
            norm_mod(l, i)
            win = fin_d[w]
            wout = fout_d[w]
            for jg in range(6):
                j0 = jg * 4
                nj = min(4, NJ - j0)
                n = nj * 128
                gb_, ub_ = B.fwg[jg % 2], B.fwu[jg % 2]
                K.dma("pool", gb_.t[:, :, 0:n],
                      win[l * D:(l + 1) * D, j0 * 128:j0 * 128 + n].rearrange("(k p) c -> p k c", p=128), writes=[gb_])
                su = B.su[jg % 2]
                K.dma("sp", su.t[:, :, 0:n],
                      win[l * D:(l + 1) * D, FF + j0 * 128:FF + j0 * 128 + n].rearrange("(k p) c -> p k c", p=128),
                      writes=[su])
                K.cp(ub_[:, :, 0:n], su[:, :, 0:n])
                for jj in range(nj):
                    j = j0 + jj
                    pg = P[(j % 2) * 2]
                    pu = P[(j % 2) * 2 + 1]
                    for k in range(8):
                        K.mm(pg[:, :], gb_[:, k, jj * 128:(jj + 1) * 128], hT[:, k, :], start=(k == 0), stop=(k == 7),
                             last=(k == 7))
                    for k in range(8):
                        K.mm(pu[:, :], ub_[:, k, jj * 128:(jj + 1) * 128], hT[:, k, :], start=(k == 0), stop=(k == 7),
                             last=(k == 7))
                    t1 = ntmp()
                    K.act(t1[:, :], pg[:, :], AF.Silu)
                    K.tt(aT[:, j, :], t1[:, :], pu[:, :], ALU.mult)
            for j in range(NJ):
                ob = B.fob[foi[0] % 3]
                foi[0] += 1
                if j % 2 == 0:
                    K.dma("pool", ob.t[:, :], wout[l * FF + j * 128: l * FF + (j + 1) * 128, :], writes=[ob])
                else:
                    so = B.so[(j // 2) % 2]
                    K.dma("sp", so.t[:, :], wout[l * FF + j * 128: l * FF + (j + 1) * 128, :], writes=[so])
                    K.cp(ob[:, :], so[:, :], eng="act")
                for m in range(8):
                    K.mm(P[m][:, :], ob[:, m * 128:(m + 1) * 128], aT[:, j, :], start=(j == 0), stop=(j == NJ - 1))
            for k in range(8):
                c0 = (l * 3 + i) * 8 + k
                K.stt(xT[:, k, :], P[k][:, :], Gmod[:, c0:c0 + 1], xT[:, k, :], ALU.mult, ALU.add)

        def load_w1(l, pl):
            B.w1k = K.sb("w1k", [128, 32, 128], BF16, es=pl)
            B.w1v = K.sb("w1v", [128, 32, 128], BF16, es=pl)
            for half in range(2):
                K.dma("pool", B.w1k.t[half * 64:(half + 1) * 64, :, :],
                      w1k_d[l * 2048:(l + 1) * 2048, :].rearrange("(l d) h -> d l h", d=64), writes=[B.w1k])
                K.dma("pool", B.w1v.t[half * 64:(half + 1) * 64, :, :],
                      w1v_d[l * 2048:(l + 1) * 2048, :].rearrange("(l d) h -> d l h", d=64), writes=[B.w1v])

        def layer_setup(l):
            pl = ExitStack()
            load_w1(l, pl)
            K.dma("pool", a2b.t[:, :], a2b_d[l * 17:(l + 1) * 17, :], writes=[a2b])
            for half in range(2):
                K.dma("pool", w2k.t[:, half * 64:(half + 1) * 64], w2k_d[l * 128:(l + 1) * 128, :], writes=[w2k])
            K.dma("pool", w2v.t[:, :], w2v_d[l * 128:(l + 1) * 128, :], writes=[w2v])
            K.dma("pool", pek.t[:, :], pek_d[:, l * 32:(l + 1) * 32], writes=[pek])
            K.dma("pool", pev.t[:, :], pev_d[:, l * 32:(l + 1) * 32], writes=[pev])
            for (w1, pe, peb) in ((B.w1k, pek, pebk), (B.w1v, pev, pebv)):
                for ll in range(32):
                    K.mm(P[6][:, 0:1], w1[0:64, ll, :], pe[0:64, ll:ll + 1], start=(ll == 0), stop=(ll == 31),
                         last=(ll == 31))
                K.cp(peb[:, :], P[6][:, 0:1])
            for kind in ("g", "r"):
                K.memset(S_f[kind][:, :, :], 0.0)
                K.memset(S_b[kind][:, :, :], 0.0)
            K.memset(hvp[:, :, :], 0.0)
            K.barrier()
            pl.close()

        def mixer(l, tt, pes):
            cur = [pes]

            def sbl(name, shape, dt):
                return K.sb(name, shape, dt, es=cur[0])

            gqT = sbl("gqT", [128, 2, TT], BF16)
            gkT = sbl("gkT", [128, 2, TT], BF16)
            ggT = sbl("ggT", [64, 4, TT], BF16)
            glrT = sbl("glrT", [17, TT], BF16)
            nqr = sbl("nqr", [128, 4, 4, 256], BF16)
            nqo = sbl("nqo", [128, 4, 4, 256], BF16)
            K.memset(nqr[:, :, :, :], 0.0)
            K.memset(nqo[:, :, :, :], 0.0)
            rqT = sbl("rqT", [128, 2, TT], BF16)
            rkT = sbl("rkT", [128, 2, TT], BF16)
            rgT = sbl("rgT", [64, 4, TT], BF16)
            gv = sbl("gv", [128, 4, 256], BF16)
            rv = sbl("rv", [128, 4, 256], BF16)
            sig = sbl("sig", [128, 4, 24], F32)
            ogT = sbl("ogT", [64, 4, TT], BF16)
            orT = sbl("orT", [64, 4, TT], BF16)
            onT = sbl("onT", [128, 4, TT], BF16)
            pa = ExitStack()
            cur[0] = pa
            B.wib = [sbl("wib%d" % i, [128, 8, 512], BF16) for i in range(3)]
            B.swi = sbl("swi", [128, 8, 512], F32)
            B.wtb = [sbl("wtb%d" % i, [128, 8, 280], BF16) for i in range(2)]
            rot = {}
            for nm in ("cosq", "sinq", "cosk", "sink"):
                rot[nm] = sbl(nm, [128, TT], F32)
                K.dma("sp", rot[nm].t[:, :], cd[nm][:, tt * TT:(tt + 1) * TT], writes=[rot[nm]])
            K.memset(glrT[:, :], 1.0)

            pcur = [0]

            def fm(b, M=128, col0=0, cache={}):
                gid = b // 4
                if "g" not in cache:
                    cache["g"] = {}
                    cache["lru"] = []
                    cache["free"] = list(B.wib)
                if gid not in cache["g"]:
                    if cache["free"]:
                        buf = cache["free"].pop(0)
                    else:
                        old = cache["lru"].pop(0)
                        buf = cache["g"].pop(old)
                    nb = min(4, NFM - gid * 4)
                    src = winx_d[l * D:(l + 1) * D, gid * 512:gid * 512 + nb * 128].rearrange("(k p) c -> p k c", p=128)
                    if gid % 2 == 0:
                        K.dma("pool", buf.t[:, :, 0:nb * 128], src, writes=[buf])
                    else:
                        K.dma("sp", B.swi.t[:, :, 0:nb * 128], src, writes=[B.swi])
                        K.cp(buf[:, :, 0:nb * 128], B.swi[:, :, 0:nb * 128], eng="act")
                    cache["g"][gid] = buf
                if gid in cache["lru"]:
                    cache["lru"].remove(gid)
                cache["lru"].append(gid)
                buf = cache["g"][gid]
                o = (b % 4) * 128 + col0
                ps = P[pcur[0] % 4]
                pcur[0] += 1
                for k in range(8):
                    K.mm(ps[0:M, :], buf[:, k, o:o + M], hT[:, k, :], start=(k == 0), stop=(k == 7),
                         last=(k == 7))
                return ps

            fmc = {}
            G = lambda nm, n: (n if (nm in stages or 'pall' in stages) else 0)
            for p in range(G('pA', 2)):
                K.cp(gqT[:, p, :], fm(0 + p, cache=fmc)[:, :], eng="act")
                K.cp(gkT[:, p, :], fm(2 + p, cache=fmc)[:, :], eng="act")
            for p in range(G('pB', 2)):
                for hh in range(2):
                    ps = fm(4 + p, 64, hh * 64, cache=fmc)
                    K.act(ggT[:, 2 * p + hh, :], ps[0:64, :], AF.Silu)
            for _ in range(G('pC', 1)):
                ps = fm(6, 16, 0, cache=fmc)
                K.cp(glrT[0:16, :], ps[0:16, :], eng="act")
            for p in range(G('pD', 4)):
                psq = fm(7 + p, cache=fmc)
                for hb_ in (0, 64):
                    K.op("act", lambda e: e.activation(
                        out=nqr.t[hb_:hb_ + 64, p, :, hb_ * 2:hb_ * 2 + 128],
                        in_=psq.t[hb_:hb_ + 64, :].rearrange("p (s t) -> p s t", s=4),
                        func=AF.Copy, scale=0.125), reads=[psq], writes=[nqr])

            def rotj(braw, bsw, cosn, sinn, dest, dec=None, bd=None):
                t1 = ntmp()
                K.tt(t1[:, :], fm(braw, cache=fmc)[:, :], rot[cosn][:, :], ALU.mult)
                t2 = ntmp()
                K.tt(t2[:, :], fm(bsw, cache=fmc)[:, :], rot[sinn][:, :], ALU.mult)
                if bd is not None:
                    qt_, qp_ = bd
                    for hb_ in (0, 64):
                        K.op("dve", lambda e: e.tensor_tensor(
                            out=qt_.t[hb_:hb_ + 64, qp_, :, hb_ * 2:hb_ * 2 + 128],
                            in0=t1.t[hb_:hb_ + 64, :].rearrange("p (s t) -> p s t", s=4),
                            in1=t2.t[hb_:hb_ + 64, :].rearrange("p (s t) -> p s t", s=4), op=ALU.add),
                            reads=[t1, t2], writes=[qt_])
                elif dec is None:
                    K.tt(dest, t1[:, :], t2[:, :], ALU.add)
                else:
                    K.tt(t1[:, :], t1[:, :], t2[:, :], ALU.add)
                    dtile, dp, dtab = dec
                    for s4 in range(4):
                        K.tt(dtile[:, dp, s4 * 128:(s4 + 1) * 128], t1[:, s4 * 128:(s4 + 1) * 128],
                             dtab[:, dp * 128:(dp + 1) * 128], ALU.mult)

            for p in range(G('pE', 4)):
                rotj(7 + p, 11 + p, "cosq", "sinq", None, bd=(nqo, p))
            for _ in range(G('pF', 1)):
                K.cp(kcb[:, 0:16], kcb[:, TT:TT + 16])
                K.cp(vcb[:, 0:16], vcb[:, TT:TT + 16])
                K.cp(kcb[:, 16:16 + TT], fm(15, cache=fmc)[:, :], eng="act")
                K.cp(vcb[:, 16:16 + TT], fm(16, cache=fmc)[:, :], eng="act")
            for g in range(G('pG', 2)):
                rotj(17 + g, 19 + g, "cosk", "sink", ksc[:, g, tt * TT:(tt + 1) * TT])
                w0 = (tt % 2) * TT
                rotj(21 + g, 23 + g, "cosk", "sink", kwc[:, g, w0:w0 + TT])
            for p in range(G('pH', 2)):
                rotj(25 + p, 27 + p, "cosk", "sink", None, dec=(rqT, p, decq))
                rotj(29 + p, 31 + p, "cosk", "sink", None, dec=(rkT, p, deck))

            for p in range(G('pB', 2)):
                for hh in range(2):
                    ps = fm(33 + p, 64, hh * 64, cache=fmc)
                    K.act(rgT[:, 2 * p + hh, :], ps[0:64, :], AF.Silu)
            for (c0, n, kindtm) in ((TMA1, 256, "gv"), (TMA2, 256, "rv"), (TMB, 280, "b"))[:max(G('pT', 3), 2 if 'pT2' in stages else 0)]:
                buf = B.wtb[wti[0] % 2]
                wti[0] += 1
                K.dma("pool", buf.t[:, :, 0:n],
                      winx_d[l * D:(l + 1) * D, c0:c0 + n].rearrange("(k p) c -> p k c", p=128), writes=[buf])
                for s in range(4):
                    ps = P[pcur[0] % 4]
                    pcur[0] += 1
                    for k in range(8):
                        K.mm(ps[:, 0:n], hT[:, k, s * 128:(s + 1) * 128], buf[:, k, 0:n], start=(k == 0),
                             stop=(k == 7), last=(k == 7 and kindtm != "b"), sgc=(kindtm == "b"))
                    if kindtm == "b":
                        K.mm(ps[:, 256:280], onesrow[0:1, :], gbrow[0:1, l * 24:(l + 1) * 24], start=False, stop=True,
                             sgc=True)
                    if kindtm == "gv":
                        K.cp(gv[:, s, :], ps[:, 0:256], eng="act")
                    elif kindtm == "rv":
                        K.cp(rv[:, s, :], ps[:, 0:256], eng="act")
                    else:
                        ca = tt * 4 + s
                        for g in range(0 if 'nob2' in stages else 2):
                            K.cp(vsc[:, ca, g, 0:64], ps[:, g * 64:(g + 1) * 64], eng="act")
                            K.cp(vwc[:, ca % 8, g, 0:64], ps[:, 128 + g * 64:128 + (g + 1) * 64], eng="act")
                        if 'nob3' not in stages:
                            K.sigmoid(sig[:, s, :], ps[:, 256:280])

            K.barrier()
            pa.close()
            pb_ = ExitStack()
            cur[0] = pb_
            lt = {}
            for nm, shp, dt in (("L1", [128, 256], F32), ("eb", [128, 256], F32), ("enb", [128, 256], F32),
                                ("qd", [128, 2, 128], BF16), ("kd", [128, 2, 128], BF16),
                                ("am", [128, 512], BF16), ("ktok", [128, 256], BF16), ("tS", [128, 128], F32),
                                ("sq", [64, 512], BF16), ("osb", [64, 512], F32), ("obf", [64, 512], BF16),
                                ("rs", [64, 512], F32), ("xc", [64, 512], F32)):
                lt[nm] = sbl("lt_" + nm, shp, dt)

            def linattn(kind, s):
                PA, PK, PO, PV, PN, PB = P[0], P[1], P[2], P[3], P[4], P[5]
                cs = slice(s * 128, (s + 1) * 128)
                if kind == "g":
                    K.mm(PB[:, 0:256], glrT[0:17, cs], a2b[0:17, :])
                    K.act(lt["L1"][:, :], PB[:, 0:256], AF.Exp, scale=-1.0)
                    K.act(lt["L1"][:, :], lt["L1"][:, :], AF.Ln, bias=1.0)
                    for p in range(2):
                        K.mm(PB[:, 256 + p * 128:256 + (p + 1) * 128], lt["L1"][:, p * 128:(p + 1) * 128], tri[:, :])
                    K.act(lt["eb"][:, :], PB[:, 256:512], AF.Exp)
                    K.act(lt["enb"][:, :], PB[:, 256:512], AF.Exp, scale=-1.0, bias=math.log(0.125))
                    for p in range(2):
                        K.tt(lt["qd"][:, p, :], gqT[:, p, cs], lt["eb"][:, p * 128:(p + 1) * 128], ALU.mult)
                        K.tt(lt["kd"][:, p, :], gkT[:, p, cs], lt["enb"][:, p * 128:(p + 1) * 128], ALU.mult)
                    qd = lambda p, a, b: lt["qd"][a:b, p, :]
                    kd = lambda p, a, b: lt["kd"][a:b, p, :]
                    dec = lambda p: lt["eb"][:, p * 128 + 127:p * 128 + 128]
                    vt = gv
                    gate = ggT
                    dst = ogT
                    gn = glag
                else:
                    qd = lambda p, a, b: rqT[a:b, p, cs]
                    kd = lambda p, a, b: rkT[a:b, p, cs]
                    dec = lambda p: rets[:, p:p + 1]
                    vt = rv
                    gate = rgT
                    dst = orT
                    gn = retg
                Sf, Sb = S_f[kind], S_b[kind]
                for h in range(4):
                    p, hb = h // 2, (h % 2) * 64
                    K.mm(PA[:, h * 128:(h + 1) * 128], kd(p, hb, hb + 64), qd(p, hb, hb + 64))
                K.tt(lt["am"][:, :], PA[:, :], caus4[:, :], ALU.mult)
                for p in range(2):
                    K.mm(PK[:, p * 128:(p + 1) * 128], kd(p, 0, 128), ident_bf[:, :])
                K.cp(lt["ktok"][:, :], PK[:, 0:256], eng="act")
                for h in range(4):
                    p, hb = h // 2, (h % 2) * 64
                    K.mm(PO[0:64, h * 128:(h + 1) * 128], vt[:, s, h * 64:(h + 1) * 64],
                         lt["am"][:, h * 128:(h + 1) * 128], start=True, stop=False, last=False)
                    K.mm(PO[0:64, h * 128:(h + 1) * 128], Sb[hb:hb + 64, p, hb:hb + 64], qd(p, hb, hb + 64),
                         start=False, stop=True)
                for p in range(2):
                    K.mm(PV[:, p * 128:(p + 1) * 128], lt["ktok"][:, p * 128:(p + 1) * 128],
                         vt[:, s, p * 128:(p + 1) * 128])
                for p in range(2):
                    K.ts(lt["tS"][:, :], PV[:, p * 128:(p + 1) * 128], dec(p), None, ALU.mult)
                    K.stt(Sf[:, p, :], Sf[:, p, :], dec(p), lt["tS"][:, :], ALU.mult, ALU.add)
                    K.cp(Sb[:, p, :], Sf[:, p, :], eng="act")
                if kind == "g":
                    K.act(lt["sq"][:, :], PO[0:64, :], AF.Square)
                    K.mm(PN[0:64, :], ones64[:, :], lt["sq"][:, :])
                    K.rsqrt(lt["rs"][:, :], PN[0:64, :])
                    K.tt(lt["xc"][:, :], PO[0:64, :], lt["rs"][:, :], ALU.mult)
                else:
                    K.cp(lt["osb"][:, :], PO[0:64, :], eng="act")
                    K.cp(lt["obf"][:, :], PO[0:64, :], eng="act")
                    K.mm(PN[0:64, :], ones64[:, :], lt["obf"][:, :])
                    K.tt(lt["xc"][:, :], lt["osb"][:, :], PN[0:64, :], ALU.subtract)
                    K.act(lt["sq"][:, :], lt["xc"][:, :], AF.Square)
                    K.mm(PN[0:64, :], ones64[:, :], lt["sq"][:, :])
                    K.rsqrt(lt["rs"][:, :], PN[0:64, :])
                    K.tt(lt["xc"][:, :], lt["xc"][:, :], lt["rs"][:, :], ALU.mult)
                for h in range(4):
                    K.stt(dst[:, h, cs], lt["xc"][:, h * 128:(h + 1) * 128], gn[:, l:l + 1], gate[:, h, cs],
                          ALU.mult, ALU.mult)

            for s in range(4 if 'lin' in stages else 0):
                linattn("g", s)
                linattn("r", s)

            K.barrier()
            pb_.close()
            pc_ = ExitStack()
            cur[0] = pc_
            load_w1(l, pc_)
            hk = sbl("hk", [128, 32], F32)
            hs = sbl("hs", [128, 32], F32)
            hkb = sbl("hkb", [128, 32], BF16)
            m0 = 1 if tt == 0 else 0
            nm_ = 32 - m0
            cchunk = (32 * tt) // 128
            soff = (32 * tt) % 128
            for g in range(2 if 'nsa' in stages else 0):
                gb = g * 64
                for (w1, buf, peb, isk) in ((B.w1k, kcb, pebk, True), (B.w1v, vcb, pebv, False)):
                    for ll in range(32):
                        K.mm(P[6][:, 0:nm_], w1[gb:gb + 64, ll, :],
                             V(buf, buf.t[gb:gb + 64, 16 * m0 + ll: 16 * m0 + ll + 16 * (nm_ - 1) + 1: 16]),
                             start=(ll == 0), stop=(ll == 31), last=(ll == 31))
                    K.act(hk[:, 0:nm_], P[6][:, 0:nm_], AF.Identity, bias=peb[:, 0:1])
                    K.sigmoid(hs[:, 0:nm_], hk[:, 0:nm_])
                    if isk:
                        K.tt(hkb[:, 0:nm_], hk[:, 0:nm_], hs[:, 0:nm_], ALU.mult)
                        K.mm(P[6][:, 64:64 + nm_], w2k[:, :], hkb[:, 0:nm_])
                        K.cp(kcc[:, g, 32 * tt + m0: 32 * tt + 32], P[6][:, 64:64 + nm_], eng="act")
                    else:
                        K.tt(hvp[:, g, soff + m0: soff + 32], hk[:, 0:nm_], hs[:, 0:nm_], ALU.mult)
                        K.mm(P[6][:, 128:192], hvp[:, g, :], w2v[:, :])
                        K.cp(vca[:, g, cchunk, 0:64], P[6][:, 128:192], eng="act")
            if soff == 96:
                K.memset(hvp[:, :, :], 0.0)

            nt = {}
            for nm, shp, dt in (("e0", [128, 512], BF16), ("e1", [128, 512], BF16), ("e2", [128, 512], BF16),
                                ("zt", [128, 12], F32), ("cf", [128, 12], F32), ("imp", [128, 64], F32),
                                ("imp2", [128, 64], F32), ("w1", [128, 64], F32), ("w2", [128, 64], F32),
                                ("m8", [128, 8], F32), ("nsb", [128, 64], BF16), ("nse", [128, 64, 64], BF16),
                                ("ont", [128, 512], BF16)):
                nt[nm] = sbl("nt_" + nm, shp, dt)
            ei = [0]

            def branch(kq, kcache_fn, vfn, chunks, acc_fn, maskfn, g, s, first_r=(0,)):
                cs = slice(s * 128, (s + 1) * 128)
                for ci, c in enumerate(chunks):
                    ps = P[ei[0] % 2]
                    ms = maskfn(c)
                    for pp in range(2):
                        K.mm(ps[:, pp * 256:(pp + 1) * 256], kcache_fn(c, 0), kq[:, 2 * g + pp, s, :],
                             start=(pp == 0), stop=(len(ms) == 0), sgc=True)
                    for mi, (ml, mr, full) in enumerate(ms):
                        if full:
                            K.mm(ps[:, :], ml, mr, start=False, stop=(mi == len(ms) - 1), sgc=True)
                        else:
                            for r in range(4):
                                K.mm(ps[:, r * 128:(r + 1) * 128], ml, mr, start=False, stop=(mi == len(ms) - 1), sgc=True)
                    e = nt["e%d" % (ei[0] % 3)]
                    ei[0] += 1
                    K.act(e[:, :], ps[:, :], AF.Exp)
                    for r in range(4):
                        K.mm(acc_fn(r), e[:, r * 128:(r + 1) * 128], vfn(c), start=(ci == 0 and r in first_r),
                             stop=(ci == len(chunks) - 1), sgc=True)

            for s in range(4 if 'nsa' in stages else 0):
                qa = tt * 4 + s
                for g in range(2):
                    cch = [0] if qa < 16 else [0, 1]

                    def cmask(c):
                        u = qa - 16 * c
                        if u > 16:
                            return []
                        return [(ident_bf[:, :], fneg[:, 128 * u:128 * u + 128], False)]

                    branch(nqr, lambda c, hb: kcc[:, g, c * 128:(c + 1) * 128],
                           lambda c: vca[:, g, c, 0:130], cch,
                           lambda r: P[2 + r // 2][:, (r % 2) * 136:(r % 2) * 136 + 130], cmask, g, s, first_r=(0, 2))
                    wch = list(range(max(0, qa - 4), qa + 1))

                    def wmask(c):
                        m = []
                        if c == qa:
                            m.append((ident_bf[:, :], negc4[:, :], True))
                        if c == qa - 4:
                            m.append((ident_bf[:, :], negw4[:, :], True))
                        return m

                    branch(nqo, lambda c, hb: kwc[:, g, (c % 8) * 128:(c % 8 + 1) * 128],
                           lambda c: vwc[:, c % 8, g, 0:66], wch,
                           lambda r: P[5][:, r * 72:r * 72 + 66], wmask, g, s)
                    zt, cf = nt["zt"], nt["cf"]
                    for bk in range(2):
                        K.cp(V(zt, zt.t[:, 6 * bk:6 * bk + 6:3]), V(P[2 + bk], P[2 + bk].t[:, 64:64 + 272:136]))
                    K.ts(zt[:, 0:12:3], zt[:, 0:12:3], 1e-30, None, ALU.add)
                    K.op("dve", lambda e: e.reciprocal(out=cf.t[:, 0:12:3], in_=zt.t[:, 0:12:3]), reads=[zt], writes=[cf])
                    for r in range(4):
                        src = P[2 + r // 2][:, (r % 2) * 136 + 65:(r % 2) * 136 + 129]
                        if r == 0:
                            K.ts(nt["imp"][:, :], src, cf[:, 0:1], None, ALU.mult)
                        else:
                            K.stt(nt["imp"][:, :], src, cf[:, 3 * r:3 * r + 1], nt["imp"][:, :], ALU.mult, ALU.add)
                    x0 = 62 - 2 * qa
                    K.tt(nt["imp2"][:, :], nt["imp"][:, :], mulu[:, x0:x0 + 64], ALU.mult)
                    K.tt(nt["imp2"][:, :], nt["imp2"][:, :], addu[:, x0:x0 + 64], ALU.add)
                    K.memset(nt["imp2"][:, 0:1], 1.0e4)
                    K.op("dve", lambda e: e.max(out=nt["m8"].t[:, :], in_=nt["imp2"].t[:, :]),
                         reads=[nt["imp2"]], writes=[nt["m8"]])
                    K.op("dve", lambda e: e.match_replace(out=nt["w1"].t[:, :], in_to_replace=nt["m8"].t[:, :],
                                                          in_values=nt["imp2"].t[:, :], imm_value=-1.0e9),
                         reads=[nt["m8"], nt["imp2"]], writes=[nt["w1"]])
                    K.op("dve", lambda e: e.max(out=nt["m8"].t[:, :], in_=nt["w1"].t[:, :]),
                         reads=[nt["w1"]], writes=[nt["m8"]])
                    K.op("dve", lambda e: e.match_replace(out=nt["w2"].t[:, :], in_to_replace=nt["m8"].t[:, :],
                                                          in_values=nt["w1"].t[:, :], imm_value=-1.0e9),
                         reads=[nt["m8"], nt["w1"]], writes=[nt["w2"]])
                    K.tt(nt["w1"][:, :], nt["imp2"][:, :], nt["w2"][:, :], ALU.is_gt)
                    K.ts(nt["nsb"][:, :], nt["w1"][:, :], -1.0, -NEG, ALU.add, ALU.mult)
                    K.op("dve", lambda e: e.tensor_copy(
                        out=nt["nse"].t[:, :, :],
                        in_=nt["nsb"].t[:, :].unsqueeze(2).to_broadcast([128, 64, 64])),
                        reads=[nt["nsb"]], writes=[nt["nse"]])
                    sch = list(range(0, qa + 1))

                    def smask(c):
                        m = [(V(nt["nse"], nt["nse"].t[:, 2 * c:2 * c + 2, :]), ident4[:, :], True)]
                        if c == qa:
                            m.append((ident_bf[:, :], negc4[:, :], True))
                        return m

                    branch(nqo, lambda c, hb: ksc[:, g, c * 128:(c + 1) * 128],
                           lambda c: vsc[:, c, g, 0:66], sch,
                           lambda r: P[4][:, r * 72:r * 72 + 66], smask, g, s)
                    K.cp(V(zt, zt.t[:, 1:12:3]), V(P[4], P[4].t[:, 64:288:72]))
                    K.cp(V(zt, zt.t[:, 2:12:3]), V(P[5], P[5].t[:, 64:288:72]))
                    K.ts(zt[:, :], zt[:, :], 1e-30, None, ALU.add)
                    K.op("dve", lambda e: e.reciprocal(out=cf.t[:, :], in_=zt.t[:, :]), reads=[zt], writes=[cf])
                    K.tt(cf[:, :], cf[:, :], sig[:, s, 12 * g:12 * g + 12], ALU.mult)
                    for r in range(4):
                        dst = nt["ont"][:, (4 * g + r) * 64:(4 * g + r + 1) * 64]
                        K.ts(dst, P[2 + r // 2][:, (r % 2) * 136:(r % 2) * 136 + 64], cf[:, 3 * r:3 * r + 1], None, ALU.mult)
                        K.stt(dst, P[4][:, r * 72:r * 72 + 64], cf[:, 3 * r + 1:3 * r + 2], dst, ALU.mult, ALU.add)
                        K.stt(dst, P[5][:, r * 72:r * 72 + 64], cf[:, 3 * r + 2:3 * r + 3], dst, ALU.mult, ALU.add)
                for p in range(4):
                    K.mm(P[6][:, p * 128:(p + 1) * 128], nt["ont"][:, p * 128:(p + 1) * 128], ident_bf[:, :])
                for p in range(4):
                    K.cp(onT[:, p, s * 128:(s + 1) * 128], P[6][:, p * 128:(p + 1) * 128], eng="act")

            K.barrier()
            pc_.close()
            cur[0] = pes
            B.wogr = sbl("wogr", [64, 8, 512], BF16)
            B.wons = sbl("wons", [128, 4, 512], BF16)
            for mg in range(2 if 'wout' in stages else 0):
                K.dma("pool", B.wogr.t[:, 0:4, :],
                      wout_d[l * D:l * D + 256, mg * 512:(mg + 1) * 512].rearrange("(h p) c -> p h c", p=64),
                      writes=[B.wogr])
                K.dma("pool", B.wogr.t[:, 4:8, :],
                      wout_d[l * D + 768:l * D + 1024, mg * 512:(mg + 1) * 512].rearrange("(h p) c -> p h c", p=64),
                      writes=[B.wogr])
                K.dma("pool", B.wons.t[:, :, :],
                      wout_d[l * D + 256:l * D + 768, mg * 512:(mg + 1) * 512].rearrange("(h p) c -> p h c", p=128),
                      writes=[B.wons])
                for m in range(4):
                    ps = P[m % 4]
                    ms = slice(m * 128, (m + 1) * 128)
                    for h in range(4):
                        K.mm(ps[:, :], B.wogr[0:64, h, ms], ogT[0:64, h, :], start=(h == 0), stop=False, last=False)
                    for h in range(4):
                        K.mm(ps[:, :], B.wogr[0:64, 4 + h, ms], orT[0:64, h, :], start=False, stop=False, last=False)
                    for p in range(4):
                        K.mm(ps[:, :], B.wons[:, p, ms], onT[:, p, :], start=False, stop=(p == 3), last=(p == 3))
                    k = mg * 4 + m
                    c0 = (l * 3 + 1) * 8 + k
                    K.stt(xT[:, k, :], ps[:, :], Gmod[:, c0:c0 + 1], xT[:, k, :], ALU.mult, ALU.add)

        B.xtok = K.sb("xtok", [128, D], F32)
        for l in range(nlayers):
            layer_setup(l)
            for tt in range(ntiles):
                if l == 0:
                    for s in range(4):
                        r0 = tt * TT + s * 128
                        K.dma("sp", B.xtok.t[:, :], x_d[r0:r0 + 128, :], writes=[B.xtok])
                        for k in range(8):
                            pb = P[k // 4]
                            K.op("pe", lambda e: e.transpose(out=pb.t[:, (k % 4) * 128:(k % 4 + 1) * 128],
                                                             in_=B.xtok.t[:, k * 128:(k + 1) * 128],
                                                             identity=ident_f.t[:, :]),
                                 reads=[B.xtok, ident_f], writes=[pb])
                        for k in range(8):
                            K.cp(xT[:, k, s * 128:(s + 1) * 128], P[k // 4][:, (k % 4) * 128:(k % 4 + 1) * 128],
                                 eng=("act" if k % 2 else "dve"))
                else:
                    K.dma("sp", xT.t[:, :, :],
                          xs_d[:, tt * TT:(tt + 1) * TT].rearrange("(k p) t -> p k t", p=128),
                          reads=[xs_trk[tt]], writes=[xT])
                for w in range(2):
                    if ('ffn1' if w == 0 else 'ffn2') in stages:
                        with ExitStack() as pes:
                            aT = K.sb("aT", [128, NJ, TT], BF16, es=pes)
                            alloc_ffn_bufs(pes)
                            ffn(l, w, aT)
                            K.barrier()
                    if w == 0 and 'mixer' in stages:
                        with ExitStack() as pes:
                            norm_mod(l, 1)
                            mixer(l, tt, pes)
                            K.barrier()
                if l < nlayers - 1:
                    K.dma("sp", xs_d[:, tt * TT:(tt + 1) * TT].rearrange("(k p) t -> p k t", p=128), xT.t[:, :, :],
                          reads=[xT], writes=[xs_trk[tt]])
                else:
                    for k in range(8):
                        K.act(hT[:, k, :], xT[:, k, :], AF.Square)
                    for k in range(8):
                        K.mm(P[7][:, :], ones128[:, :], hT[:, k, :], start=(k == 0), stop=(k == 7), last=(k == 7))
                    K.rsqrt(rstd[:, :], P[7][:, :])
                    for k in range(8):
                        K.stt(xT[:, k, :], xT[:, k, :], fgT[:, k:k + 1], rstd[:, :], ALU.mult, ALU.mult)
                    for s in range(4):
                        for k in range(8):
                            pb = P[k // 4]
                            K.op("pe", lambda e: e.transpose(out=pb.t[:, (k % 4) * 128:(k % 4 + 1) * 128],
                                                             in_=xT.t[:, k, s * 128:(s + 1) * 128],
                                                             identity=ident_f.t[:, :]),
                                 reads=[xT, ident_f], writes=[pb])
                        for hf in range(2):
                            K.cp(B.xtok[:, hf * 512:(hf + 1) * 512], P[hf][:, :], eng=("act" if hf else "dve"))
                        r0 = tt * TT + s * 128
                        K.dma("sp", out_d[r0:r0 + 128, :], B.xtok.t[:, :], reads=[B.xtok], writes=[out_trk])
        SP = K.eng["sp"]
        dq = K.dq["sp"]
        K._wait(SP, {s: c for s, c in zip(dq["sems"], dq["cnt"]) if c > 0})
    return nc


_NC = None
_NCORES = 8


def kernel(x, c, w_ada, b_ada, norm_g, ffn1_in, ffn1_out, w_in, gla_a2, gla_a_bias, gla_norm_g,
           nsa_pe_k, nsa_pe_v, nsa_w1_k, nsa_w2_k, nsa_w1_v, nsa_w2_v, nsa_gate_bias, ret_norm_g,
           w_out, ffn2_in, ffn2_out, final_norm_g):
    global _NC
    f = lambda a: np.ascontiguousarray(np.asarray(a, dtype=np.float32))
    x = f(x)
    B = x.shape[0]
    shared = {
        "w_ada": f(w_ada).reshape(L * D, 9 * D),
        "b_adaT": f(np.asarray(b_ada).reshape(L, 72, 128).transpose(2, 0, 1).reshape(128, L * 72)),
        "norm_gT": f(np.asarray(norm_g).reshape(L, 3, 8, 128).transpose(3, 0, 1, 2).reshape(128, L * 24)),
        "final_gT": f(np.asarray(final_norm_g).reshape(8, 128).T),
        "ffn1_in": f(ffn1_in).reshape(L * D, 2 * FF),
        "ffn2_in": f(ffn2_in).reshape(L * D, 2 * FF),
        "ffn1_out": f(ffn1_out).reshape(L * FF, D),
        "ffn2_out": f(ffn2_out).reshape(L * FF, D),
        "w_in_ext": f(np.asarray(w_in)[:, :, _colidx()]).reshape(L * D, NEXT),
        "a2b": f(np.concatenate([np.asarray(gla_a2), np.asarray(gla_a_bias)[:, None, :]], axis=1)).reshape(L * 17, 256),
        "gla_gT": f(np.asarray(gla_norm_g).T),
        "ret_gT": f(np.asarray(ret_norm_g).T),
        "pekT": f(np.tile(np.asarray(nsa_pe_k).transpose(2, 0, 1).reshape(64, L * 32), (2, 1))),
        "pevT": f(np.tile(np.asarray(nsa_pe_v).transpose(2, 0, 1).reshape(64, L * 32), (2, 1))),
        "w1k": f(nsa_w1_k).reshape(L * 2048, 128),
        "w1v": f(nsa_w1_v).reshape(L * 2048, 128),
        "w2k": f(nsa_w2_k).reshape(L * 128, 64),
        "w2v": f(nsa_w2_v).reshape(L * 128, 64),
        "gbias": f(np.tile(np.asarray(nsa_gate_bias).reshape(1, L * 24), (128, 1))),
        "w_out": f(w_out).reshape(L * D, D),
    }
    for k, v in _consts().items():
        shared["c_" + k] = f(v)
    if _NC is None:
        _NC = build()
    in_maps = []
    for core in range(8):
        b = core % B
        m = dict(shared)
        m["x"] = f(x[b])
        m["cT"] = f(np.asarray(c)[b].reshape(8, 128).T)
        in_maps.append(m)
    res = run_bass_kernel_spmd(_NC, in_maps[:_NCORES], core_ids=list(range(_NCORES)))
    out = np.stack([res.results[b % _NCORES]["out"] for b in range(B)], axis=0)
    return out.astype(np.float32)
```

```python
import math
from contextlib import ExitStack

import numpy as np
import concourse.bass as bass
import concourse.mybir as mybir
from concourse.bass_utils import run_bass_kernel_spmd

F32 = mybir.dt.float32
BF16 = mybir.dt.bfloat16
AF = mybir.ActivationFunctionType
ALU = mybir.AluOpType

D = 1024
T = 4096
L = 4
TT = 512
NTILE = T // TT
FF = 2816
NJ = FF // 128
EPS = 1e-6
NEG = -30000.0
NFM = 35
TMA1 = NFM * 128
TMA2 = TMA1 + 256
TMB = TMA2 + 256
NEXT = TMB + 280
FW = 2176


class V:
    def __init__(self, tl, ap):
        self.tl = tl
        self.ap = ap


class Tl:
    def __init__(self, t=None):
        self.t = t
        self.w = None
        self.r = {}

    def __getitem__(self, idx):
        return V(self, self.t[idx])


class Eng:
    def __init__(self, name, h):
        self.name = name
        self.h = h
        self.sem = None
        self.cnt = 0
        self.waited = {}
        self.pending = []


class KB:
    LIMIT = 20000

    def __init__(self, nc, es):
        self.nc = nc
        self.es = es
        self.sems = []
        self.eng = {
            "pe": Eng("pe", nc.tensor),
            "act": Eng("act", nc.scalar),
            "dve": Eng("dve", nc.vector),
            "pool": Eng("pool", nc.gpsimd),
            "sp": Eng("sp", nc.sync),
        }
        self.dq = {}
        for q in ("sp", "pool"):
            self.dq[q] = {"i": 0, "sems": [self.newsem("d%s%d" % (q, i)) for i in range(12)], "cnt": [0] * 12}
        self.nuniq = 0

    def newsem(self, name):
        s = self.es.enter_context(self.nc.semaphore(name))
        self.sems.append(s)
        return len(self.sems) - 1

    def sb(self, name, shape, dt, es=None):
        self.nuniq += 1
        t = (es or self.es).enter_context(self.nc.sbuf_tensor("%s_%d" % (name, self.nuniq), list(shape), dt))
        return Tl(t)

    def ps(self, name, shape, dt=F32):
        t = self.es.enter_context(self.nc.psum_tensor(name, list(shape), dt))
        return Tl(t)

    def _deps(self, reads, writes):
        deps = {}

        def add(ev):
            if ev is not None:
                if deps.get(ev[0], 0) < ev[1]:
                    deps[ev[0]] = ev[1]

        for t in reads:
            add(t.w)
        for t in writes:
            add(t.w)
            for s, v in t.r.items():
                add((s, v))
        return deps

    def _wait(self, E, deps):
        for s, v in deps.items():
            if E.waited.get(s, 0) < v:
                E.h.wait_ge(self.sems[s], v)
                E.waited[s] = v

    def op(self, eng, fn, reads=(), writes=(), last=True):
        E = self.eng[eng]
        reads = [x.tl if isinstance(x, V) else x for x in reads]
        writes = [x.tl if isinstance(x, V) else x for x in writes]
        if E.sem is None or (E.cnt >= self.LIMIT and not E.pending):
            E.sem = self.newsem("e%s%d" % (eng, len(self.sems)))
            E.cnt = 0
        self._wait(E, self._deps(reads, writes))
        ins = fn(E.h)
        E.pending.append((reads, writes))
        if last:
            E.cnt += 1
            ins.then_inc(self.sems[E.sem], 1)
            ev = (E.sem, E.cnt)
            for rd, wr in E.pending:
                for t in rd:
                    if t.r.get(ev[0], 0) < ev[1]:
                        t.r[ev[0]] = ev[1]
                for t in wr:
                    t.w = ev
                    t.r = {}
            E.pending = []
        return ins

    def dma(self, q, out, in_, reads=(), writes=()):
        Q = self.eng[q]
        reads = [x.tl if isinstance(x, V) else x for x in reads]
        writes = [x.tl if isinstance(x, V) else x for x in writes]
        deps = self._deps(reads, writes)
        pool = self.dq[q]
        i = pool["i"] % len(pool["sems"])
        pool["i"] += 1
        sem, cnt = pool["sems"][i], pool["cnt"][i]
        if cnt > 0 and deps.get(sem, 0) < cnt:
            deps[sem] = cnt
        self._wait(Q, deps)
        Q.h.dma_start(out=out, in_=in_).then_inc(self.sems[sem], 16)
        pool["cnt"][i] = cnt + 16
        ev = (sem, cnt + 16)
        for t in reads:
            if t.r.get(ev[0], 0) < ev[1]:
                t.r[ev[0]] = ev[1]
        for t in writes:
            t.w = ev
            t.r = {}
        return ev

    def barrier(self, names=("pe", "act", "dve", "sp", "pool")):
        for a in names:
            A = self.eng[a]
            deps = {}
            for b in names:
                B = self.eng[b]
                if B.sem is not None and B.cnt > 0 and not (b == a and b in ('sp', 'pool')):
                    deps[B.sem] = B.cnt
            self._wait(A, deps)

    def mm(self, out, lhsT, rhs, start=True, stop=True, last=True, sgc=False):
        return self.op("pe", lambda e: e.matmul(out.ap, lhsT.ap, rhs.ap, start=start, stop=stop,
                                                skip_group_check=sgc),
                       reads=[lhsT, rhs], writes=[out], last=last)

    def act(self, out, in_, func, bias=None, scale=None, extra=()):
        kw = {}
        rd = [in_] + list(extra)
        if bias is not None:
            if isinstance(bias, V):
                kw["bias"] = bias.ap
                rd.append(bias)
            else:
                kw["bias"] = bias
        if scale is not None:
            if isinstance(scale, V):
                kw["scale"] = scale.ap
                rd.append(scale)
            else:
                kw["scale"] = scale
        return self.op("act", lambda e: e.activation(out=out.ap, in_=in_.ap, func=func, **kw),
                       reads=rd, writes=[out])

    def tt(self, out, in0, in1, op, eng="dve"):
        return self.op(eng, lambda e: e.tensor_tensor(out=out.ap, in0=in0.ap, in1=in1.ap, op=op),
                       reads=[in0, in1], writes=[out])

    def ts(self, out, in0, s1, s2, op0, op1=None, eng="dve"):
        rd = [in0]
        a1 = s1
        a2 = s2
        if isinstance(s1, V):
            rd.append(s1)
            a1 = s1.ap
        if isinstance(s2, V):
            rd.append(s2)
            a2 = s2.ap
        if op1 is None:
            return self.op(eng, lambda e: e.tensor_scalar(out=out.ap, in0=in0.ap, scalar1=a1, scalar2=None, op0=op0),
                           reads=rd, writes=[out])
        return self.op(eng, lambda e: e.tensor_scalar(out=out.ap, in0=in0.ap, scalar1=a1, scalar2=a2, op0=op0, op1=op1),
                       reads=rd, writes=[out])

    def stt(self, out, in0, scalar, in1, op0, op1, eng="dve"):
        rd = [in0, in1]
        a = scalar
        if isinstance(scalar, V):
            rd.append(scalar)
            a = scalar.ap
        return self.op(eng, lambda e: e.scalar_tensor_tensor(out=out.ap, in0=in0.ap, scalar=a, in1=in1.ap, op0=op0, op1=op1),
                       reads=rd, writes=[out])

    def rsqrt(self, out, in_):
        self.act(out, in_, AF.Sqrt, bias=EPS)
        return self.op("dve", lambda e: e.reciprocal(out=out.ap, in_=out.ap), reads=[out], writes=[out])

    def sigmoid(self, out, in_):
        self.act(out, in_, AF.Exp, scale=-1.0)
        self.ts(out, out, 1.0, None, ALU.add)
        return self.op("dve", lambda e: e.reciprocal(out=out.ap, in_=out.ap), reads=[out], writes=[out])

    def cp(self, out, in_, eng="dve"):
        if eng == "act":
            return self.op(eng, lambda e: e.activation(out=out.ap, in_=in_.ap, func=AF.Copy), reads=[in_], writes=[out])
        return self.op(eng, lambda e: e.tensor_copy(out=out.ap, in_=in_.ap), reads=[in_], writes=[out])

    def memset(self, out, val, eng="dve"):
        return self.op(eng, lambda e: e.memset(out.ap, val), reads=[], writes=[out])


def _colidx():
    def rng(a, n):
        return list(range(a, a + n))

    def swp(a, n):
        o = []
        for h in range(n // 64):
            o += rng(a + 64 * h + 32, 32) + rng(a + 64 * h, 32)
        return o

    b = []
    b.append(rng(0, 128)); b.append(rng(128, 128))
    b.append(rng(256, 128)); b.append(rng(384, 128))
    b.append(rng(768, 128)); b.append(rng(896, 128))
    b.append(rng(1024, 16) + [1024] * 112)
    for p in range(4):
        b.append(rng(1040 + 128 * p, 128))
    for p in range(4):
        b.append(swp(1040 + 128 * p, 128))
    b.append(rng(1552, 128)); b.append(rng(1680, 128))
    for g in range(2):
        b.append(rng(1808 + 64 * g, 64) * 2)
    for g in range(2):
        b.append(swp(1808 + 64 * g, 64) * 2)
    for g in range(2):
        b.append(rng(2064 + 64 * g, 64) * 2)
    for g in range(2):
        b.append(swp(2064 + 64 * g, 64) * 2)
    for p in range(2):
        b.append(rng(2344 + 128 * p, 128))
    for p in range(2):
        b.append(swp(2344 + 128 * p, 128))
    for p in range(2):
        b.append(rng(2600 + 128 * p, 128))
    for p in range(2):
        b.append(swp(2600 + 128 * p, 128))
    b.append(rng(3112, 128)); b.append(rng(3240, 128))
    assert len(b) == NFM
    idx = []
    for x in b:
        assert len(x) == 128
        idx += x
    idx += rng(512, 256) + rng(2856, 256)
    idx += rng(1936, 128) + rng(2192, 128) + rng(2320, 24)
    assert len(idx) == NEXT
    return np.array(idx, dtype=np.int64)


def _consts():
    c = {}
    p = np.arange(128)
    t = np.arange(T)
    invf = (10000.0 ** (-np.arange(32, dtype=np.float64) / 32.0))
    ang = t[None, :].astype(np.float64) * invf[(p % 32)][:, None]
    cos = np.cos(ang)
    sin = np.sin(ang)
    sgn = np.where((p % 64) < 32, -1.0, 1.0)[:, None]
    c["cosk"] = cos.astype(np.float32)
    c["sink"] = (sin * sgn).astype(np.float32)
    c["cosq"] = (cos * 0.125).astype(np.float32)
    c["sinq"] = (sin * sgn * 0.125).astype(np.float32)
    lg = np.log1p(-np.exp2(-5.0 - np.arange(4, dtype=np.float64)))
    i = np.arange(128, dtype=np.float64)
    decq = np.zeros((128, 2, 128)); deck = np.zeros((128, 2, 128)); rets = np.zeros((128, 2))
    for pair in range(2):
        for half in range(2):
            h = pair * 2 + half
            dq = np.exp(lg[h] * (i + 1.0))
            dk = np.exp(-lg[h] * (i + 1.0)) * 0.125
            decq[half * 64:(half + 1) * 64, pair, :] = dq[None, :]
            deck[half * 64:(half + 1) * 64, pair, :] = dk[None, :]
            rets[half * 64:(half + 1) * 64, pair] = np.exp(lg[h] * 128.0)
    c["decq"] = decq.reshape(128, 256).astype(np.float32)
    c["deck"] = deck.reshape(128, 256).astype(np.float32)
    c["rets"] = rets.astype(np.float32)
    j = np.arange(128)[:, None]
    ii = np.arange(128)[None, :]
    c["ident"] = np.eye(128, dtype=np.float32)
    c["ident4"] = np.tile(np.eye(128, dtype=np.float32), (1, 4))
    c["negc4"] = np.tile(np.where(j > ii, NEG, 0.0).astype(np.float32), (1, 4))
    c["negw4"] = np.tile(np.where(j <= ii, NEG, 0.0).astype(np.float32), (1, 4))
    c["caus4"] = np.tile((j <= ii).astype(np.float32), (1, 4))
    c["tri"] = np.where(j <= ii, -1.0 / 16.0, 0.0).astype(np.float32)
    y = np.arange(FW)[None, :]
    c["fneg"] = np.where(16 * j + 15 <= y, 0.0, NEG).astype(np.float32)
    x = np.arange(126)[None, :]
    m = x - 62
    cur = (np.arange(128)[:, None] >= 64).astype(np.int64)
    c["mulu"] = (m < cur - 1).astype(np.float32)
    addu = np.zeros((128, 126), dtype=np.float32)
    addu[np.broadcast_to(m == cur - 1, addu.shape)] = 1.2e4
    addu[np.broadcast_to(m == cur, addu.shape)] = 1.1e4
    addu[np.broadcast_to(m > cur, addu.shape)] = -1.0
    c["addu"] = addu
    ova = np.zeros((128, 2, 65), dtype=np.float32)
    for slot in range(1, 256):
        cc, sl = divmod(slot, 128)
        ova[sl, cc, 0] = 1.0
        for s in range(64):
            if 4 * s <= slot <= 4 * s + 4:
                ova[sl, cc, 1 + s] = 1.0
    c["ovaug"] = ova.reshape(128, 130)
    return c


_CONST_SHAPES = None


def _const_shapes():
    global _CONST_SHAPES
    if _CONST_SHAPES is None:
        _CONST_SHAPES = {k: v.shape for k, v in _consts().items()}
    return _CONST_SHAPES


def build(nlayers=L, ntiles=NTILE, stages=('ffn1', 'mixer', 'pall', 'lin', 'nsa', 'wout', 'ffn2')):
    nc = bass.Bass("TRN2", target_bir_lowering=False)
    dr = {}

    def din(name, shape):
        dr[name] = nc.dram_tensor(name, list(shape), F32, kind="ExternalInput").ap()
        return dr[name]

    x_d = din("x", [T, D])
    cT_d = din("cT", [128, 8])
    wada_d = din("w_ada", [L * D, 9 * D])
    bada_d = din("b_adaT", [128, L * 72])
    ng_d = din("norm_gT", [128, L * 24])
    fg_d = din("final_gT", [128, 8])
    fin_d = [din("ffn1_in", [L * D, 2 * FF]), din("ffn2_in", [L * D, 2 * FF])]
    fout_d = [din("ffn1_out", [L * FF, D]), din("ffn2_out", [L * FF, D])]
    winx_d = din("w_in_ext", [L * D, NEXT])
    a2b_d = din("a2b", [L * 17, 256])
    glag_d = din("gla_gT", [64, L])
    retg_d = din("ret_gT", [64, L])
    pek_d = din("pekT", [128, L * 32])
    pev_d = din("pevT", [128, L * 32])
    w1k_d = din("w1k", [L * 2048, 128])
    w1v_d = din("w1v", [L * 2048, 128])
    w2k_d = din("w2k", [L * 128, 64])
    w2v_d = din("w2v", [L * 128, 64])
    gb_d = din("gbias", [128, L * 24])
    wout_d = din("w_out", [L * D, D])
    cd = {k: din("c_" + k, list(s)) for k, s in _const_shapes().items()}
    out_d = nc.dram_tensor("out", [T, D], F32, kind="ExternalOutput").ap()
    xs_d = nc.dram_tensor("xscr", [D, T], F32, kind="Internal").ap()

    with ExitStack() as es:
        K = KB(nc, es)
        P = [K.ps("ps%d" % i, [128, 512]) for i in range(8)]
        xs_trk = [Tl() for _ in range(NTILE)]
        out_trk = Tl()

        def cload(name, dt, q=None):
            shp = _const_shapes()[name]
            t = K.sb("c_" + name, shp, dt)
            K.dma("pool" if dt == BF16 else "sp", t.t[:], cd[name][:, :], writes=[t])
            return t

        ident_bf = cload("ident", BF16)
        ident_f = cload("ident", F32)
        ident4 = cload("ident4", BF16)
        negc4 = cload("negc4", BF16)
        negw4 = cload("negw4", BF16)
        caus4 = cload("caus4", BF16)
        tri = cload("tri", F32)
        fneg = cload("fneg", BF16)
        mulu = cload("mulu", F32)
        addu = cload("addu", F32)
        rets = cload("rets", F32)
        decq = cload("decq", F32)
        deck = cload("deck", F32)
        ones128 = K.sb("ones128", [128, 128], BF16)
        K.memset(ones128[:, :], 1.0 / 1024.0)
        ones64 = K.sb("ones64", [64, 64], BF16)
        K.memset(ones64[:, :], 1.0 / 64.0)

        xT = K.sb("xT", [128, 8, TT], F32)
        hT = K.sb("hT", [128, 8, TT], BF16)
        rstd = K.sb("rstd", [128, TT], F32)
        tmp = [K.sb("tmp%d" % i, [128, TT], F32) for i in range(3)]
        tmpi = [0]

        def ntmp():
            tmpi[0] += 1
            return tmp[tmpi[0] % 3]

        modall = K.sb("modall", [128, L * 72], F32)
        Amod = K.sb("Amod", [128, L * 24], F32)
        Gmod = K.sb("Gmod", [128, L * 24], F32)
        ngT = K.sb("ngT", [128, L * 24], F32)
        K.dma("sp", ngT.t[:], ng_d[:, :], writes=[ngT])
        fgT = K.sb("fgT", [128, 8], F32)
        K.dma("sp", fgT.t[:], fg_d[:, :], writes=[fgT])
        glag = K.sb("glag", [64, L], F32)
        K.dma("sp", glag.t[:], glag_d[:, :], writes=[glag])
        retg = K.sb("retg", [64, L], F32)
        K.dma("sp", retg.t[:], retg_d[:, :], writes=[retg])
        gbrow = K.sb("gbrow", [1, L * 24], BF16)
        K.dma("pool", gbrow.t[:], gb_d[0:1, :], writes=[gbrow])
        onesrow = K.sb("onesrow", [1, 128], BF16)
        K.memset(onesrow[:, :], 1.0)

        NFB = 3
        fwi = [0]
        NOB = 4
        foi = [0]
        NWB = 4
        wii = [0]
        wti = [0]

        class NS:
            pass

        B = NS()

        def alloc_ffn_bufs(pes):
            B.fwg = [K.sb("fwg%d" % i, [128, 8, 512], BF16, es=pes) for i in range(2)]
            B.fwu = [K.sb("fwu%d" % i, [128, 8, 512], BF16, es=pes) for i in range(2)]
            B.fob = [K.sb("fob%d" % i, [128, 1024], BF16, es=pes) for i in range(3)]
            B.su = [K.sb("su%d" % i, [128, 8, 512], F32, es=pes) for i in range(2)]
            B.so = [K.sb("so%d" % i, [128, 1024], F32, es=pes) for i in range(2)]

        a2b = K.sb("a2b", [17, 256], BF16)
        w2k = K.sb("w2k", [128, 128], BF16)
        w2v = K.sb("w2v", [128, 64], BF16)
        pek = K.sb("pek", [128, 32], BF16)
        pev = K.sb("pev", [128, 32], BF16)
        pebk = K.sb("pebk", [128, 1], F32)
        pebv = K.sb("pebv", [128, 1], F32)

        ksc = K.sb("ksc", [128, 2, T], BF16)
        kwc = K.sb("kwc", [128, 2, 1024], BF16)
        vsc = K.sb("vsc", [128, 32, 2, 80], BF16)
        vwc = K.sb("vwc", [128, 8, 2, 80], BF16)
        kcc = K.sb("kcc", [128, 2, 256], BF16)
        vca = K.sb("vca", [128, 2, 2, 136], BF16)
        kcb = K.sb("kcb", [128, 16 + TT], BF16)
        vcb = K.sb("vcb", [128, 16 + TT], BF16)
        hvp = K.sb("hvp", [128, 2, 128], BF16)
        K.memset(ksc[:, :, :], 0.0)
        K.memset(kwc[:, :, :], 0.0)
        K.memset(vsc[:, :, :, :], 1.0)
        K.memset(vwc[:, :, :, :], 1.0)
        K.memset(kcc[:, :, :], 0.0)
        K.memset(vca[:, :, :, :], 0.0)
        K.memset(kcb[:, :], 0.0)
        K.memset(vcb[:, :], 0.0)
        K.memset(hvp[:, :, :], 0.0)
        for g in range(2):
            for cc in range(2):
                K.dma("pool", vca.t[:, g, cc, 64:129], cd["ovaug"][:, cc * 65:(cc + 1) * 65], writes=[vca])

        S_f = {"g": K.sb("Sg", [128, 2, 128], F32), "r": K.sb("Sr", [128, 2, 128], F32)}
        S_b = {"g": K.sb("Sgb", [128, 2, 128], BF16), "r": K.sb("Srb", [128, 2, 128], BF16)}

        cact = K.sb("cact", [128, 8], BF16)
        ctmp = K.sb("ctmp", [128, 8], F32)
        K.dma("sp", ctmp.t[:], cT_d[:, :], writes=[ctmp])
        K.act(cact[:, :], ctmp[:, :], AF.Silu)
        badaT = K.sb("badaT", [128, L * 72], F32)
        K.dma("sp", badaT.t[:], bada_d[:, :], writes=[badaT])
        PM = P[7]
        pes0 = ExitStack()
        alloc_ffn_bufs(pes0)
        for l in range(nlayers):
            for cg in range(18):
                buf = (B.fwg + B.fwu)[fwi[0] % 4]
                fwi[0] += 1
                K.dma("pool", buf.t[:, :, :],
                      wada_d[l * D:(l + 1) * D, cg * 512:(cg + 1) * 512].rearrange("(k p) c -> p k c", p=128),
                      writes=[buf])
                for jj in range(4):
                    col = (l * 72 + cg * 4 + jj) % 512
                    for k in range(8):
                        K.mm(PM[:, col:col + 1], buf[:, k, jj * 128:(jj + 1) * 128], cact[:, k:k + 1],
                             start=(k == 0), stop=(k == 7), last=(k == 7))
            K.tt(modall[:, l * 72:(l + 1) * 72], PM[:, (l * 72) % 512:(l * 72) % 512 + 72],
                 badaT[:, l * 72:(l + 1) * 72], ALU.add)
            for i in range(3):
                sc = modall[:, l * 72 + (3 * i + 1) * 8: l * 72 + (3 * i + 1) * 8 + 8]
                gt = modall[:, l * 72 + (3 * i + 2) * 8: l * 72 + (3 * i + 2) * 8 + 8]
                K.stt(Amod[:, (l * 3 + i) * 8:(l * 3 + i) * 8 + 8], sc, 1.0,
                      ngT[:, (l * 3 + i) * 8:(l * 3 + i) * 8 + 8], ALU.add, ALU.mult)
                K.ts(Gmod[:, (l * 3 + i) * 8:(l * 3 + i) * 8 + 8], gt, 1.0 if i == 1 else 0.5, None, ALU.mult)

        K.barrier()
        pes0.close()

        def Bmod(l, i, k):
            c0 = l * 72 + (3 * i) * 8 + k
            return modall[:, c0:c0 + 1]

        def norm_mod(l, i):
            for k in range(8):
                K.act(hT[:, k, :], xT[:, k, :], AF.Square)
            for k in range(8):
                K.mm(P[7][:, :], ones128[:, :], hT[:, k, :], start=(k == 0), stop=(k == 7), last=(k == 7))
            K.rsqrt(rstd[:, :], P[7][:, :])
            for k in range(8):
                t1 = ntmp()
                K.tt(t1[:, :], xT[:, k, :], rstd[:, :], ALU.mult)
                c0 = (l * 3 + i) * 8 + k
                K.act(hT[:, k, :], t1[:, :], AF.Identity, bias=Bmod(l, i, k), scale=Amod[:, c0:c0 + 1])

        def ffn(l, w, aT):
            i = 0 if w == 0 else 2
            norm_mod(l, i)
            win = fin_d[w]
            wout = fout_d[w]
            for jg in range(6):
                j0 = jg * 4
                nj = min(4, NJ - j0)
                n = nj * 128
                gb_, ub_ = B.fwg[jg % 2], B.fwu[jg % 2]
                K.dma("pool", gb_.t[:, :, 0:n],
                      win[l * D:(l + 1) * D, j0 * 128:j0 * 128 + n].rearrange("(k p) c -> p k c", p=128), writes=[gb_])
                su = B.su[jg % 2]
                K.dma("sp", su.t[:, :, 0:n],
                      win[l * D:(l + 1) * D, FF + j0 * 128:FF + j0 * 128 + n].rearrange("(k p) c -> p k c", p=128),
                      writes=[su])
                K.cp(ub_[:, :, 0:n], su[:, :, 0:n])
                for jj in range(nj):
                    j = j0 + jj
                    pg = P[(j % 2) * 2]
                    pu = P[(j % 2) * 2 + 1]
                    for k in range(8):
                        K.mm(pg[:, :], gb_[:, k, jj * 128:(jj + 1) * 128], hT[:, k, :], start=(k == 0), stop=(k == 7),
                             last=(k == 7))
                    for k in range(8):
                        K.mm(pu[:, :], ub_[:, k, jj * 128:(jj + 1) * 128], hT[:, k, :], start=(k == 0), stop=(k == 7),
                             last=(k == 7))
                    t1 = ntmp()
                    K.act(t1[:, :], pg[:, :], AF.Silu)
                    K.tt(aT[:, j, :], t1[:, :], pu[:, :], ALU.mult)
            for j in range(NJ):
                ob = B.fob[foi[0] % 3]
                foi[0] += 1
                if j % 2 == 0:
                    K.dma("pool", ob.t[:, :], wout[l * FF + j * 128: l * FF + (j + 1) * 128, :], writes=[ob])
                else:
                    so = B.so[(j // 2) % 2]
                    K.dma("sp", so.t[:, :], wout[l * FF + j * 128: l * FF + (j + 1) * 128, :], writes=[so])
                    K.cp(ob[:, :], so[:, :], eng="act")
                for m in range(8):
                    K.mm(P[m][:, :], ob[:, m * 128:(m + 1) * 128], aT[:, j, :], start=(j == 0), stop=(j == NJ - 1),
                         last=(m == 7))
            for k in range(8):
                c0 = (l * 3 + i) * 8 + k
                K.stt(xT[:, k, :], P[k][:, :], Gmod[:, c0:c0 + 1], xT[:, k, :], ALU.mult, ALU.add)

        def load_w1(l, pl):
            B.w1k = K.sb("w1k", [128, 32, 128], BF16, es=pl)
            B.w1v = K.sb("w1v", [128, 32, 128], BF16, es=pl)
            for half in range(2):
                K.dma("pool", B.w1k.t[half * 64:(half + 1) * 64, :, :],
                      w1k_d[l * 2048:(l + 1) * 2048, :].rearrange("(l d) h -> d l h", d=64), writes=[B.w1k])
                K.dma("pool", B.w1v.t[half * 64:(half + 1) * 64, :, :],
                      w1v_d[l * 2048:(l + 1) * 2048, :].rearrange("(l d) h -> d l h", d=64), writes=[B.w1v])

        def layer_setup(l):
            pl = ExitStack()
            load_w1(l, pl)
            K.dma("pool", a2b.t[:, :], a2b_d[l * 17:(l + 1) * 17, :], writes=[a2b])
            for half in range(2):
                K.dma("pool", w2k.t[:, half * 64:(half + 1) * 64], w2k_d[l * 128:(l + 1) * 128, :], writes=[w2k])
            K.dma("pool", w2v.t[:, :], w2v_d[l * 128:(l + 1) * 128, :], writes=[w2v])
            K.dma("pool", pek.t[:, :], pek_d[:, l * 32:(l + 1) * 32], writes=[pek])
            K.dma("pool", pev.t[:, :], pev_d[:, l * 32:(l + 1) * 32], writes=[pev])
            for (w1, pe, peb) in ((B.w1k, pek, pebk), (B.w1v, pev, pebv)):
                for ll in range(32):
                    K.mm(P[6][:, 0:1], w1[0:64, ll, :], pe[0:64, ll:ll + 1], start=(ll == 0), stop=(ll == 31),
                         last=(ll == 31))
                K.cp(peb[:, :], P[6][:, 0:1])
            for kind in ("g", "r"):
                K.memset(S_f[kind][:, :, :], 0.0)
                K.memset(S_b[kind][:, :, :], 0.0)
            K.memset(hvp[:, :, :], 0.0)
            K.barrier()
            pl.close()

        def mixer(l, tt, pes):
            cur = [pes]

            def sbl(name, shape, dt):
                return K.sb(name, shape, dt, es=cur[0])

            gqT = sbl("gqT", [128, 2, TT], BF16)
            gkT = sbl("gkT", [128, 2, TT], BF16)
            ggT = sbl("ggT", [64, 4, TT], BF16)
            glrT = sbl("glrT", [17, TT], BF16)
            nqr = sbl("nqr", [128, 4, 4, 256], BF16)
            nqo = sbl("nqo", [128, 4, 4, 256], BF16)
            K.memset(nqr[:, :, :, :], 0.0)
            K.memset(nqo[:, :, :, :], 0.0)
            rqT = sbl("rqT", [128, 2, TT], BF16)
            rkT = sbl("rkT", [128, 2, TT], BF16)
            rgT = sbl("rgT", [64, 4, TT], BF16)
            gv = sbl("gv", [128, 4, 256], BF16)
            rv = sbl("rv", [128, 4, 256], BF16)
            sig = sbl("sig", [128, 4, 24], F32)
            ogT = sbl("ogT", [64, 4, TT], BF16)
            orT = sbl("orT", [64, 4, TT], BF16)
            onT = sbl("onT", [128, 4, TT], BF16)
            pa = ExitStack()
            cur[0] = pa
            B.wib = [sbl("wib%d" % i, [128, 8, 512], BF16) for i in range(3)]
            B.swi = sbl("swi", [128, 8, 512], F32)
            B.wtb = [sbl("wtb%d" % i, [128, 8, 280], BF16) for i in range(2)]
            rot = {}
            for nm in ("cosq", "sinq", "cosk", "sink"):
                rot[nm] = sbl(nm, [128, TT], F32)
                K.dma("sp", rot[nm].t[:, :], cd[nm][:, tt * TT:(tt + 1) * TT], writes=[rot[nm]])
            K.memset(glrT[:, :], 1.0)

            pcur = [0]

            def fm(b, M=128, col0=0, cache={}):
                gid = b // 4
                if "g" not in cache:
                    cache["g"] = {}
                    cache["lru"] = []
                    cache["free"] = list(B.wib)
                if gid not in cache["g"]:
                    if cache["free"]:
                        buf = cache["free"].pop(0)
                    else:
                        old = cache["lru"].pop(0)
                        buf = cache["g"].pop(old)
                    nb = min(4, NFM - gid * 4)
                    src = winx_d[l * D:(l + 1) * D, gid * 512:gid * 512 + nb * 128].rearrange("(k p) c -> p k c", p=128)
                    if gid % 2 == 0:
                        K.dma("pool", buf.t[:, :, 0:nb * 128], src, writes=[buf])
                    else:
                        K.dma("sp", B.swi.t[:, :, 0:nb * 128], src, writes=[B.swi])
                        K.cp(buf[:, :, 0:nb * 128], B.swi[:, :, 0:nb * 128], eng="act")
                    cache["g"][gid] = buf
                if gid in cache["lru"]:
                    cache["lru"].remove(gid)
                cache["lru"].append(gid)
                buf = cache["g"][gid]
                o = (b % 4) * 128 + col0
                ps = P[pcur[0] % 4]
                pcur[0] += 1
                for k in range(8):
                    K.mm(ps[0:M, :], buf[:, k, o:o + M], hT[:, k, :], start=(k == 0), stop=(k == 7),
                         last=(k == 7))
                return ps

            fmc = {}
            G = lambda nm, n: (n if (nm in stages or 'pall' in stages) else 0)
            for p in range(G('pA', 2)):
                K.cp(gqT[:, p, :], fm(0 + p, cache=fmc)[:, :], eng="act")
                K.cp(gkT[:, p, :], fm(2 + p, cache=fmc)[:, :], eng="act")
            for p in range(G('pB', 2)):
                for hh in range(2):
                    ps = fm(4 + p, 64, hh * 64, cache=fmc)
                    K.act(ggT[:, 2 * p + hh, :], ps[0:64, :], AF.Silu)
            for _ in range(G('pC', 1)):
                ps = fm(6, 16, 0, cache=fmc)
                K.cp(glrT[0:16, :], ps[0:16, :], eng="act")
            for p in range(G('pD', 4)):
                psq = fm(7 + p, cache=fmc)
                for hb_ in (0, 64):
                    K.op("act", lambda e: e.activation(
                        out=nqr.t[hb_:hb_ + 64, p, :, hb_ * 2:hb_ * 2 + 128],
                        in_=psq.t[hb_:hb_ + 64, :].rearrange("p (s t) -> p s t", s=4),
                        func=AF.Copy, scale=0.125), reads=[psq], writes=[nqr])

            def rotj(braw, bsw, cosn, sinn, dest, dec=None, bd=None):
                t1 = ntmp()
                K.tt(t1[:, :], fm(braw, cache=fmc)[:, :], rot[cosn][:, :], ALU.mult)
                t2 = ntmp()
                K.tt(t2[:, :], fm(bsw, cache=fmc)[:, :], rot[sinn][:, :], ALU.mult)
                if bd is not None:
                    qt_, qp_ = bd
                    for hb_ in (0, 64):
                        K.op("dve", lambda e: e.tensor_tensor(
                            out=qt_.t[hb_:hb_ + 64, qp_, :, hb_ * 2:hb_ * 2 + 128],
                            in0=t1.t[hb_:hb_ + 64, :].rearrange("p (s t) -> p s t", s=4),
                            in1=t2.t[hb_:hb_ + 64, :].rearrange("p (s t) -> p s t", s=4), op=ALU.add),
                            reads=[t1, t2], writes=[qt_])
                elif dec is None:
                    K.tt(dest, t1[:, :], t2[:, :], ALU.add)
                else:
                    K.tt(t1[:, :], t1[:, :], t2[:, :], ALU.add)
                    dtile, dp, dtab = dec
                    for s4 in range(4):
                        K.tt(dtile[:, dp, s4 * 128:(s4 + 1) * 128], t1[:, s4 * 128:(s4 + 1) * 128],
                             dtab[:, dp * 128:(dp + 1) * 128], ALU.mult)

            for p in range(G('pE', 4)):
                rotj(7 + p, 11 + p, "cosq", "sinq", None, bd=(nqo, p))
            for _ in range(G('pF', 1)):
                K.cp(kcb[:, 0:16], kcb[:, TT:TT + 16])
                K.cp(vcb[:, 0:16], vcb[:, TT:TT + 16])
                K.cp(kcb[:, 16:16 + TT], fm(15, cache=fmc)[:, :], eng="act")
                K.cp(vcb[:, 16:16 + TT], fm(16, cache=fmc)[:, :], eng="act")
            for g in range(G('pG', 2)):
                rotj(17 + g, 19 + g, "cosk", "sink", ksc[:, g, tt * TT:(tt + 1) * TT])
                w0 = (tt % 2) * TT
                rotj(21 + g, 23 + g, "cosk", "sink", kwc[:, g, w0:w0 + TT])
            for p in range(G('pH', 2)):
                rotj(25 + p, 27 + p, "cosk", "sink", None, dec=(rqT, p, decq))
                rotj(29 + p, 31 + p, "cosk", "sink", None, dec=(rkT, p, deck))

            for p in range(G('pB', 2)):
                for hh in range(2):
                    ps = fm(33 + p, 64, hh * 64, cache=fmc)
                    K.act(rgT[:, 2 * p + hh, :], ps[0:64, :], AF.Silu)
            for (c0, n, kindtm) in ((TMA1, 256, "gv"), (TMA2, 256, "rv"), (TMB, 280, "b"))[:max(G('pT', 3), 2 if 'pT2' in stages else 0)]:
                buf = B.wtb[wti[0] % 2]
                wti[0] += 1
                K.dma("pool", buf.t[:, :, 0:n],
                      winx_d[l * D:(l + 1) * D, c0:c0 + n].rearrange("(k p) c -> p k c", p=128), writes=[buf])
                for s in range(4):
                    ps = P[pcur[0] % 4]
                    pcur[0] += 1
                    for k in range(8):
                        K.mm(ps[:, 0:n], hT[:, k, s * 128:(s + 1) * 128], buf[:, k, 0:n], start=(k == 0),
                             stop=(k == 7), last=(k == 7 and kindtm != "b"), sgc=(kindtm == "b"))
                    if kindtm == "b":
                        K.mm(ps[:, 256:280], onesrow[0:1, :], gbrow[0:1, l * 24:(l + 1) * 24], start=False, stop=True,
                             sgc=True)
                    if kindtm == "gv":
                        K.cp(gv[:, s, :], ps[:, 0:256], eng="act")
                    elif kindtm == "rv":
                        K.cp(rv[:, s, :], ps[:, 0:256], eng="act")
                    else:
                        ca = tt * 4 + s
                        for g in range(0 if 'nob2' in stages else 2):
                            K.cp(vsc[:, ca, g, 0:64], ps[:, g * 64:(g + 1) * 64], eng="act")
                            K.cp(vwc[:, ca % 8, g, 0:64], ps[:, 128 + g * 64:128 + (g + 1) * 64], eng="act")
                        if 'nob3' not in stages:
                            K.sigmoid(sig[:, s, :], ps[:, 256:280])

            K.barrier()
            pa.close()
            pb_ = ExitStack()
            cur[0] = pb_
            lt = {}
            for nm, shp, dt in (("L1", [128, 256], F32), ("eb", [128, 256], F32), ("enb", [128, 256], F32),
                                ("qd", [128, 2, 128], BF16), ("kd", [128, 2, 128], BF16),
                                ("am", [128, 512], BF16), ("ktok", [128, 256], BF16), ("tS", [128, 128], F32),
                                ("sq", [64, 512], BF16), ("osb", [64, 512], F32), ("obf", [64, 512], BF16),
                                ("rs", [64, 512], F32), ("xc", [64, 512], F32)):
                lt[nm] = sbl("lt_" + nm, shp, dt)

            def linattn(kind, s):
                PA, PK, PO, PV, PN, PB = P[0], P[1], P[2], P[3], P[4], P[5]
                cs = slice(s * 128, (s + 1) * 128)
                if kind == "g":
                    K.mm(PB[:, 0:256], glrT[0:17, cs], a2b[0:17, :])
                    K.act(lt["L1"][:, :], PB[:, 0:256], AF.Exp, scale=-1.0)
                    K.act(lt["L1"][:, :], lt["L1"][:, :], AF.Ln, bias=1.0)
                    for p in range(2):
                        K.mm(PB[:, 256 + p * 128:256 + (p + 1) * 128], lt["L1"][:, p * 128:(p + 1) * 128], tri[:, :])
                    K.act(lt["eb"][:, :], PB[:, 256:512], AF.Exp)
                    K.act(lt["enb"][:, :], PB[:, 256:512], AF.Exp, scale=-1.0, bias=math.log(0.125))
                    for p in range(2):
                        K.tt(lt["qd"][:, p, :], gqT[:, p, cs], lt["eb"][:, p * 128:(p + 1) * 128], ALU.mult)
                        K.tt(lt["kd"][:, p, :], gkT[:, p, cs], lt["enb"][:, p * 128:(p + 1) * 128], ALU.mult)
                    qd = lambda p, a, b: lt["qd"][a:b, p, :]
                    kd = lambda p, a, b: lt["kd"][a:b, p, :]
                    dec = lambda p: lt["eb"][:, p * 128 + 127:p * 128 + 128]
                    vt = gv
                    gate = ggT
                    dst = ogT
                    gn = glag
                else:
                    qd = lambda p, a, b: rqT[a:b, p, cs]
                    kd = lambda p, a, b: rkT[a:b, p, cs]
                    dec = lambda p: rets[:, p:p + 1]
                    vt = rv
                    gate = rgT
                    dst = orT
                    gn = retg
                Sf, Sb = S_f[kind], S_b[kind]
                for h in range(4):
                    p, hb = h // 2, (h % 2) * 64
                    K.mm(PA[:, h * 128:(h + 1) * 128], kd(p, hb, hb + 64), qd(p, hb, hb + 64))
                K.tt(lt["am"][:, :], PA[:, :], caus4[:, :], ALU.mult)
                for p in range(2):
                    K.mm(PK[:, p * 128:(p + 1) * 128], kd(p, 0, 128), ident_bf[:, :])
                K.cp(lt["ktok"][:, :], PK[:, 0:256], eng="act")
                for h in range(4):
                    p, hb = h // 2, (h % 2) * 64
                    K.mm(PO[0:64, h * 128:(h + 1) * 128], vt[:, s, h * 64:(h + 1) * 64],
                         lt["am"][:, h * 128:(h + 1) * 128], start=True, stop=False, last=False)
                    K.mm(PO[0:64, h * 128:(h + 1) * 128], Sb[hb:hb + 64, p, hb:hb + 64], qd(p, hb, hb + 64),
                         start=False, stop=True)
                for p in range(2):
                    K.mm(PV[:, p * 128:(p + 1) * 128], lt["ktok"][:, p * 128:(p + 1) * 128],
                         vt[:, s, p * 128:(p + 1) * 128])
                for p in range(2):
                    K.ts(lt["tS"][:, :], PV[:, p * 128:(p + 1) * 128], dec(p), None, ALU.mult)
                    K.stt(Sf[:, p, :], Sf[:, p, :], dec(p), lt["tS"][:, :], ALU.mult, ALU.add)
                    K.cp(Sb[:, p, :], Sf[:, p, :], eng="act")
                if kind == "g":
                    K.act(lt["sq"][:, :], PO[0:64, :], AF.Square)
                    K.mm(PN[0:64, :], ones64[:, :], lt["sq"][:, :])
                    K.rsqrt(lt["rs"][:, :], PN[0:64, :])
                    K.tt(lt["xc"][:, :], PO[0:64, :], lt["rs"][:, :], ALU.mult)
                else:
                    K.cp(lt["osb"][:, :], PO[0:64, :], eng="act")
                    K.cp(lt["obf"][:, :], PO[0:64, :], eng="act")
                    K.mm(PN[0:64, :], ones64[:, :], lt["obf"][:, :])
                    K.tt(lt["xc"][:, :], lt["osb"][:, :], PN[0:64, :], ALU.subtract)
                    K.act(lt["sq"][:, :], lt["xc"][:, :], AF.Square)
                    K.mm(PN[0:64, :], ones64[:, :], lt["sq"][:, :])
                    K.rsqrt(lt["rs"][:, :], PN[0:64, :])
                    K.tt(lt["xc"][:, :], lt["xc"][:, :], lt["rs"][:, :], ALU.mult)
                for h in range(4):
                    K.stt(dst[:, h, cs], lt["xc"][:, h * 128:(h + 1) * 128], gn[:, l:l + 1], gate[:, h, cs],
                          ALU.mult, ALU.mult)

            for s in range(4 if 'lin' in stages else 0):
                linattn("g", s)
                linattn("r", s)

            pc_ = pb_
            load_w1(l, pc_)
            hk = sbl("hk", [128, 32], F32)
            hs = sbl("hs", [128, 32], F32)
            hkb = sbl("hkb", [128, 32], BF16)
            m0 = 1 if tt == 0 else 0
            nm_ = 32 - m0
            cchunk = (32 * tt) // 128
            soff = (32 * tt) % 128
            for g in range(2 if 'nsa' in stages else 0):
                gb = g * 64
                for (w1, buf, peb, isk) in ((B.w1k, kcb, pebk, True), (B.w1v, vcb, pebv, False)):
                    for ll in range(32):
                        K.mm(P[6][:, 0:nm_], w1[gb:gb + 64, ll, :],
                             V(buf, buf.t[gb:gb + 64, 16 * m0 + ll: 16 * m0 + ll + 16 * (nm_ - 1) + 1: 16]),
                             start=(ll == 0), stop=(ll == 31), last=(ll == 31))
                    K.act(hk[:, 0:nm_], P[6][:, 0:nm_], AF.Identity, bias=peb[:, 0:1])
                    K.sigmoid(hs[:, 0:nm_], hk[:, 0:nm_])
                    if isk:
                        K.tt(hkb[:, 0:nm_], hk[:, 0:nm_], hs[:, 0:nm_], ALU.mult)
                        K.mm(P[6][:, 64:64 + nm_], w2k[:, :], hkb[:, 0:nm_])
                        K.cp(kcc[:, g, 32 * tt + m0: 32 * tt + 32], P[6][:, 64:64 + nm_], eng="act")
                    else:
                        K.tt(hvp[:, g, soff + m0: soff + 32], hk[:, 0:nm_], hs[:, 0:nm_], ALU.mult)
                        K.mm(P[6][:, 128:192], hvp[:, g, :], w2v[:, :])
                        K.cp(vca[:, g, cchunk, 0:64], P[6][:, 128:192], eng="act")
            if soff == 96:
                K.memset(hvp[:, :, :], 0.0)

            nt = {}
            for nm, shp, dt in (("e0", [128, 512], BF16), ("e1", [128, 512], BF16), ("e2", [128, 512], BF16),
                                ("zt", [128, 12], F32), ("cf", [128, 12], F32), ("imp", [128, 64], F32),
                                ("imp2", [128, 64], F32), ("w1", [128, 64], F32), ("w2", [128, 64], F32),
                                ("m8", [128, 8], F32), ("nsb", [128, 64], BF16), ("nse", [128, 64, 64], BF16),
                                ("ont", [128, 512], BF16)):
                nt[nm] = sbl("nt_" + nm, shp, dt)
            ei = [0]

            def branch(kq, kcache_fn, vfn, chunks, acc_fn, maskfn, g, s, first_r=(0,)):
                cs = slice(s * 128, (s + 1) * 128)
                for ci, c in enumerate(chunks):
                    ps = P[ei[0] % 2]
                    ms = maskfn(c)
                    for pp in range(2):
                        K.mm(ps[:, pp * 256:(pp + 1) * 256], kcache_fn(c, 0), kq[:, 2 * g + pp, s, :],
                             start=(pp == 0), stop=(len(ms) == 0), sgc=True, last=(len(ms) == 0 and pp == 1))
                    for mi, (ml, mr, full) in enumerate(ms):
                        if full:
                            K.mm(ps[:, :], ml, mr, start=False, stop=(mi == len(ms) - 1), sgc=True,
                                 last=(mi == len(ms) - 1))
                        else:
                            for r in range(4):
                                K.mm(ps[:, r * 128:(r + 1) * 128], ml, mr, start=False, stop=(mi == len(ms) - 1), sgc=True,
                                     last=(mi == len(ms) - 1 and r == 3))
                    e = nt["e%d" % (ei[0] % 3)]
                    ei[0] += 1
                    K.act(e[:, :], ps[:, :], AF.Exp)
                    for r in range(4):
                        K.mm(acc_fn(r), e[:, r * 128:(r + 1) * 128], vfn(c), start=(ci == 0 and r in first_r),
                             stop=(ci == len(chunks) - 1), sgc=True, last=(r == 3))

            for s in range(4 if 'nsa' in stages else 0):
                qa = tt * 4 + s
                for g in range(2):
                    cch = [0] if qa < 16 else [0, 1]

                    def cmask(c):
                        u = qa - 16 * c
                        if u > 16:
                            return []
                        return [(ident_bf[:, :], fneg[:, 128 * u:128 * u + 128], False)]

                    branch(nqr, lambda c, hb: kcc[:, g, c * 128:(c + 1) * 128],
                           lambda c: vca[:, g, c, 0:130], cch,
                           lambda r: P[2 + r // 2][:, (r % 2) * 136:(r % 2) * 136 + 130], cmask, g, s, first_r=(0, 2))
                    wch = list(range(max(0, qa - 4), qa + 1))

                    def wmask(c):
                        m = []
                        if c == qa:
                            m.append((ident_bf[:, :], negc4[:, :], True))
                        if c == qa - 4:
                            m.append((ident_bf[:, :], negw4[:, :], True))
                        return m

                    branch(nqo, lambda c, hb: kwc[:, g, (c % 8) * 128:(c % 8 + 1) * 128],
                           lambda c: vwc[:, c % 8, g, 0:66], wch,
                           lambda r: P[5][:, r * 72:r * 72 + 66], wmask, g, s)
                    zt, cf = nt["zt"], nt["cf"]
                    for bk in range(2):
                        K.cp(V(zt, zt.t[:, 6 * bk:6 * bk + 6:3]), V(P[2 + bk], P[2 + bk].t[:, 64:64 + 272:136]))
                    K.ts(zt[:, 0:12:3], zt[:, 0:12:3], 1e-30, None, ALU.add)
                    K.op("dve", lambda e: e.reciprocal(out=cf.t[:, 0:12:3], in_=zt.t[:, 0:12:3]), reads=[zt], writes=[cf])
                    for r in range(4):
                        src = P[2 + r // 2][:, (r % 2) * 136 + 65:(r % 2) * 136 + 129]
                        if r == 0:
                            K.ts(nt["imp"][:, :], src, cf[:, 0:1], None, ALU.mult)
                        else:
                            K.stt(nt["imp"][:, :], src, cf[:, 3 * r:3 * r + 1], nt["imp"][:, :], ALU.mult, ALU.add)
                    x0 = 62 - 2 * qa
                    K.tt(nt["imp2"][:, :], nt["imp"][:, :], mulu[:, x0:x0 + 64], ALU.mult)
                    K.tt(nt["imp2"][:, :], nt["imp2"][:, :], addu[:, x0:x0 + 64], ALU.add)
                    K.memset(nt["imp2"][:, 0:1], 1.0e4)
                    K.op("dve", lambda e: e.max(out=nt["m8"].t[:, :], in_=nt["imp2"].t[:, :]),
                         reads=[nt["imp2"]], writes=[nt["m8"]])
                    K.op("dve", lambda e: e.match_replace(out=nt["w1"].t[:, :], in_to_replace=nt["m8"].t[:, :],
                                                          in_values=nt["imp2"].t[:, :], imm_value=-1.0e9),
                         reads=[nt["m8"], nt["imp2"]], writes=[nt["w1"]])
                    K.op("dve", lambda e: e.max(out=nt["m8"].t[:, :], in_=nt["w1"].t[:, :]),
                         reads=[nt["w1"]], writes=[nt["m8"]])
                    K.op("dve", lambda e: e.match_replace(out=nt["w2"].t[:, :], in_to_replace=nt["m8"].t[:, :],
                                                          in_values=nt["w1"].t[:, :], imm_value=-1.0e9),
                         reads=[nt["m8"], nt["w1"]], writes=[nt["w2"]])
                    K.tt(nt["w1"][:, :], nt["imp2"][:, :], nt["w2"][:, :], ALU.is_gt)
                    K.ts(nt["nsb"][:, :], nt["w1"][:, :], -1.0, -NEG, ALU.add, ALU.mult)
                    K.op("dve", lambda e: e.tensor_copy(
                        out=nt["nse"].t[:, :, :],
                        in_=nt["nsb"].t[:, :].unsqueeze(2).to_broadcast([128, 64, 64])),
                        reads=[nt["nsb"]], writes=[nt["nse"]])
                    sch = list(range(0, qa + 1))

                    def smask(c):
                        m = [(V(nt["nse"], nt["nse"].t[:, 2 * c:2 * c + 2, :]), ident4[:, :], True)]
                        if c == qa:
                            m.append((ident_bf[:, :], negc4[:, :], True))
                        return m

                    branch(nqo, lambda c, hb: ksc[:, g, c * 128:(c + 1) * 128],
                           lambda c: vsc[:, c, g, 0:66], sch,
                           lambda r: P[4][:, r * 72:r * 72 + 66], smask, g, s)
                    K.cp(V(zt, zt.t[:, 1:12:3]), V(P[4], P[4].t[:, 64:288:72]))
                    K.cp(V(zt, zt.t[:, 2:12:3]), V(P[5], P[5].t[:, 64:288:72]))
                    K.ts(zt[:, :], zt[:, :], 1e-30, None, ALU.add)
                    K.op("dve", lambda e: e.reciprocal(out=cf.t[:, :], in_=zt.t[:, :]), reads=[zt], writes=[cf])
                    K.tt(cf[:, :], cf[:, :], sig[:, s, 12 * g:12 * g + 12], ALU.mult)
                    for r in range(4):
                        dst = nt["ont"][:, (4 * g + r) * 64:(4 * g + r + 1) * 64]
                        K.ts(dst, P[2 + r // 2][:, (r % 2) * 136:(r % 2) * 136 + 64], cf[:, 3 * r:3 * r + 1], None, ALU.mult)
                        K.stt(dst, P[4][:, r * 72:r * 72 + 64], cf[:, 3 * r + 1:3 * r + 2], dst, ALU.mult, ALU.add)
                        K.stt(dst, P[5][:, r * 72:r * 72 + 64], cf[:, 3 * r + 2:3 * r + 3], dst, ALU.mult, ALU.add)
                for p in range(4):
                    K.mm(P[6][:, p * 128:(p + 1) * 128], nt["ont"][:, p * 128:(p + 1) * 128], ident_bf[:, :])
                for p in range(4):
                    K.cp(onT[:, p, s * 128:(s + 1) * 128], P[6][:, p * 128:(p + 1) * 128], eng="act")

            B.wogr = sbl("wogr", [64, 8, 512], BF16)
            B.wons = sbl("wons", [128, 4, 512], BF16)
            for mg in range(2 if 'wout' in stages else 0):
                K.dma("pool", B.wogr.t[:, 0:4, :],
                      wout_d[l * D:l * D + 256, mg * 512:(mg + 1) * 512].rearrange("(h p) c -> p h c", p=64),
                      writes=[B.wogr])
                K.dma("pool", B.wogr.t[:, 4:8, :],
                      wout_d[l * D + 768:l * D + 1024, mg * 512:(mg + 1) * 512].rearrange("(h p) c -> p h c", p=64),
                      writes=[B.wogr])
                K.dma("pool", B.wons.t[:, :, :],
                      wout_d[l * D + 256:l * D + 768, mg * 512:(mg + 1) * 512].rearrange("(h p) c -> p h c", p=128),
                      writes=[B.wons])
                for m in range(4):
                    ps = P[m % 4]
                    ms = slice(m * 128, (m + 1) * 128)
                    for h in range(4):
                        K.mm(ps[:, :], B.wogr[0:64, h, ms], ogT[0:64, h, :], start=(h == 0), stop=False, last=False)
                    for h in range(4):
                        K.mm(ps[:, :], B.wogr[0:64, 4 + h, ms], orT[0:64, h, :], start=False, stop=False, last=False)
                    for p in range(4):
                        K.mm(ps[:, :], B.wons[:, p, ms], onT[:, p, :], start=False, stop=(p == 3), last=(p == 3))
                    k = mg * 4 + m
                    c0 = (l * 3 + 1) * 8 + k
                    K.stt(xT[:, k, :], ps[:, :], Gmod[:, c0:c0 + 1], xT[:, k, :], ALU.mult, ALU.add)
            K.barrier()
            pb_.close()

        B.xtok = K.sb("xtok", [128, D], F32)
        for l in range(nlayers):
            layer_setup(l)
            for tt in range(ntiles):
                if l == 0:
                    for s in range(4):
                        r0 = tt * TT + s * 128
                        K.dma("sp", B.xtok.t[:, :], x_d[r0:r0 + 128, :], writes=[B.xtok])
                        for k in range(8):
                            pb = P[k // 4]
                            K.op("pe", lambda e: e.transpose(out=pb.t[:, (k % 4) * 128:(k % 4 + 1) * 128],
                                                             in_=B.xtok.t[:, k * 128:(k + 1) * 128],
                                                             identity=ident_f.t[:, :]),
                                 reads=[B.xtok, ident_f], writes=[pb])
                        for k in range(8):
                            K.cp(xT[:, k, s * 128:(s + 1) * 128], P[k // 4][:, (k % 4) * 128:(k % 4 + 1) * 128],
                                 eng=("act" if k % 2 else "dve"))
                else:
                    K.dma("sp", xT.t[:, :, :],
                          xs_d[:, tt * TT:(tt + 1) * TT].rearrange("(k p) t -> p k t", p=128),
                          reads=[xs_trk[tt]], writes=[xT])
                for w in range(2):
                    if ('ffn1' if w == 0 else 'ffn2') in stages:
                        with ExitStack() as pes:
                            aT = K.sb("aT", [128, NJ, TT], BF16, es=pes)
                            alloc_ffn_bufs(pes)
                            ffn(l, w, aT)
                            K.barrier()
                    if w == 0 and 'mixer' in stages:
                        with ExitStack() as pes:
                            norm_mod(l, 1)
                            mixer(l, tt, pes)
                            K.barrier()
                if l < nlayers - 1:
                    K.dma("sp", xs_d[:, tt * TT:(tt + 1) * TT].rearrange("(k p) t -> p k t", p=128), xT.t[:, :, :],
                          reads=[xT], writes=[xs_trk[tt]])
                else:
                    for k in range(8):
                        K.act(hT[:, k, :], xT[:, k, :], AF.Square)
                    for k in range(8):
                        K.mm(P[7][:, :], ones128[:, :], hT[:, k, :], start=(k == 0), stop=(k == 7), last=(k == 7))
                    K.rsqrt(rstd[:, :], P[7][:, :])
                    for k in range(8):
                        K.stt(xT[:, k, :], xT[:, k, :], fgT[:, k:k + 1], rstd[:, :], ALU.mult, ALU.mult)
                    for s in range(4):
                        for k in range(8):
                            pb = P[k // 4]
                            K.op("pe", lambda e: e.transpose(out=pb.t[:, (k % 4) * 128:(k % 4 + 1) * 128],
                                                             in_=xT.t[:, k, s * 128:(s + 1) * 128],
                                                             identity=ident_f.t[:, :]),
                                 reads=[xT, ident_f], writes=[pb])
                        for hf in range(2):
                            K.cp(B.xtok[:, hf * 512:(hf + 1) * 512], P[hf][:, :], eng=("act" if hf else "dve"))
                        r0 = tt * TT + s * 128
                        K.dma("sp", out_d[r0:r0 + 128, :], B.xtok.t[:, :], reads=[B.xtok], writes=[out_trk])
        SP = K.eng["sp"]
        dq = K.dq["sp"]
        K._wait(SP, {s: c for s, c in zip(dq["sems"], dq["cnt"]) if c > 0})
    return nc


_NC = None
_NCORES = 8


def kernel(x, c, w_ada, b_ada, norm_g, ffn1_in, ffn1_out, w_in, gla_a2, gla_a_bias, gla_norm_g,
           nsa_pe_k, nsa_pe_v, nsa_w1_k, nsa_w2_k, nsa_w1_v, nsa_w2_v, nsa_gate_bias, ret_norm_g,
           w_out, ffn2_in, ffn2_out, final_norm_g):
    global _NC
    f = lambda a: np.ascontiguousarray(np.asarray(a, dtype=np.float32))
    x = f(x)
    B = x.shape[0]
    shared = {
        "w_ada": f(w_ada).reshape(L * D, 9 * D),
        "b_adaT": f(np.asarray(b_ada).reshape(L, 72, 128).transpose(2, 0, 1).reshape(128, L * 72)),
        "norm_gT": f(np.asarray(norm_g).reshape(L, 3, 8, 128).transpose(3, 0, 1, 2).reshape(128, L * 24)),
        "final_gT": f(np.asarray(final_norm_g).reshape(8, 128).T),
        "ffn1_in": f(ffn1_in).reshape(L * D, 2 * FF),
        "ffn2_in": f(ffn2_in).reshape(L * D, 2 * FF),
        "ffn1_out": f(ffn1_out).reshape(L * FF, D),
        "ffn2_out": f(ffn2_out).reshape(L * FF, D),
        "w_in_ext": f(np.asarray(w_in)[:, :, _colidx()]).reshape(L * D, NEXT),
        "a2b": f(np.concatenate([np.asarray(gla_a2), np.asarray(gla_a_bias)[:, None, :]], axis=1)).reshape(L * 17, 256),
        "gla_gT": f(np.asarray(gla_norm_g).T),
        "ret_gT": f(np.asarray(ret_norm_g).T),
        "pekT": f(np.tile(np.asarray(nsa_pe_k).transpose(2, 0, 1).reshape(64, L * 32), (2, 1))),
        "pevT": f(np.tile(np.asarray(nsa_pe_v).transpose(2, 0, 1).reshape(64, L * 32), (2, 1))),
        "w1k": f(nsa_w1_k).reshape(L * 2048, 128),
        "w1v": f(nsa_w1_v).reshape(L * 2048, 128),
        "w2k": f(nsa_w2_k).reshape(L * 128, 64),
        "w2v": f(nsa_w2_v).reshape(L * 128, 64),
        "gbias": f(np.tile(np.asarray(nsa_gate_bias).reshape(1, L * 24), (128, 1))),
        "w_out": f(w_out).reshape(L * D, D),
    }
    for k, v in _consts().items():
        shared["c_" + k] = f(v)
    if _NC is None:
        _NC = build()
    in_maps = []
    for core in range(8):
        b = core % B
        m = dict(shared)
        m["x"] = f(x[b])
        m["cT"] = f(np.asarray(c)[b].reshape(8, 128).T)
        in_maps.append(m)
    res = run_bass_kernel_spmd(_NC, in_maps[:_NCORES], core_ids=list(range(_NCORES)))
    out = np.stack([res.results[b % _NCORES]["out"] for b in range(B)], axis=0)
    return out.astype(np.float32)
```
